# Optimizing a Trainium2 kernel written in Bass

```python
import math
import jax, jax.numpy as jnp
from jax import lax
import numpy as np

D_MODEL = 2048
BATCH = 2
SEQ = 4096
DEPTH = 1

N_META = 16
ATTN_HEADS = 16
ATTN_KV_HEADS = 4
HEAD_DIM = 128
IDX_HEADS = 16
IDX_DIM = 64
TOPK_MAX = 256
Q_BLOCK = 128
ROPE_THETA = 10000.0
SSM_EXPAND = 2
D_INNER = SSM_EXPAND * D_MODEL
SSM_HEAD_DIM = 64
SSM_HEADS = D_INNER // SSM_HEAD_DIM
SSM_GROUPS = 8
D_STATE = 128
CONV_WIDTH = 4
CHUNK = 256
CONV_DIM = D_INNER + 2 * SSM_GROUPS * D_STATE
PEER_HEADS = 8
N_KEYS = 128
N_EXPERTS = N_KEYS * N_KEYS
PEER_KEY_DIM = 256
PEER_TOPK = 16
PEER_TOK_BLOCK = 128
NORM_EPS = 1e-6

W_Q = ATTN_HEADS * HEAD_DIM
W_KV = ATTN_KV_HEADS * HEAD_DIM
W_IQ = IDX_HEADS * IDX_DIM
W_IK = IDX_DIM
W_IW = IDX_HEADS
W_BC = SSM_GROUPS * D_STATE
W_GATE = 2 * D_MODEL
IN_WIDTH = W_Q + 2 * W_KV + W_IQ + W_IK + W_IW + 2 * D_INNER + 2 * W_BC + SSM_HEADS + W_GATE

kernel_name = 'hybrid_dsa_ssd_peer_block'


def rms_norm(x, w):
    xf = x.astype(jnp.float32)
    y = xf * lax.rsqrt(jnp.mean(xf * xf, axis=-1, keepdims=True) + NORM_EPS)
    return (y * w.astype(jnp.float32)).astype(x.dtype)


def rope(x, pos):
    d = x.shape[-1]
    half = d // 2
    inv = ROPE_THETA ** (-jnp.arange(half, dtype=jnp.float32) / half)
    ang = pos.astype(jnp.float32)[:, None] * inv[None, :]
    cos = jnp.cos(ang)[:, None, :]
    sin = jnp.sin(ang)[:, None, :]
    x1 = x[..., :half].astype(jnp.float32)
    x2 = x[..., half:].astype(jnp.float32)
    out = jnp.concatenate([x1 * cos - x2 * sin, x2 * cos + x1 * sin], axis=-1)
    return out.astype(x.dtype)


def dsa_attention(q, k, v, q_idx, k_idx, w_idx, topk):
    B, T, H, Dh = q.shape
    G = k.shape[2]
    rep = H // G
    n_blk = -(-T // Q_BLOCK)
    pad = n_blk * Q_BLOCK - T

    def to_blocks(a):
        a = jnp.pad(a, [(0, 0), (0, pad)] + [(0, 0)] * (a.ndim - 2))
        return a.reshape((B, n_blk, Q_BLOCK) + a.shape[2:]).swapaxes(0, 1)

    starts = jnp.arange(n_blk) * Q_BLOCK
    key_pos = jnp.arange(T)
    k_idx_f = k_idx.astype(jnp.float32)
    gather = jax.vmap(lambda a, i: a[i])

    def one_block(args):
        qb, qib, wib, start = args
        q_pos = start + jnp.arange(Q_BLOCK)
        logits = jnp.einsum('bqhd,bkd->bqhk', qib.astype(jnp.float32), k_idx_f) * (IDX_DIM ** -0.5)
        score = jnp.einsum('bqh,bqhk->bqk', wib.astype(jnp.float32) * (IDX_HEADS ** -0.5), jax.nn.relu(logits))
        admissible = key_pos[None, :] <= q_pos[:, None]
        score = jnp.where(admissible[None], score, -jnp.inf)
        _, sel = lax.top_k(score, topk)
        k_sel = gather(k, sel).astype(jnp.float32)
        v_sel = gather(v, sel).astype(jnp.float32)
        qg = qb.reshape(B, Q_BLOCK, G, rep, Dh).astype(jnp.float32)
        s = jnp.einsum('bqgrd,bqkgd->bqgrk', qg, k_sel) * (Dh ** -0.5)
        valid = sel <= q_pos[None, :, None]
        s = jnp.where(valid[:, :, None, None, :], s, -jnp.inf)
        p = jax.nn.softmax(s, axis=-1)
        o = jnp.einsum('bqgrk,bqkgd->bqgrd', p, v_sel)
        return o.reshape(B, Q_BLOCK, H * Dh).astype(q.dtype)

    out = lax.map(one_block, (to_blocks(q), to_blocks(q_idx), to_blocks(w_idx), starts))
    return out.swapaxes(0, 1).reshape(B, n_blk * Q_BLOCK, H * Dh)[:, :T]


def causal_dwconv(u, w, b):
    out = lax.conv_general_dilated(
        u, w[:, None, :].astype(u.dtype), window_strides=(1,), padding=[(CONV_WIDTH - 1, 0)],
        dimension_numbers=('NWC', 'WIO', 'NWC'), feature_group_count=u.shape[-1])
    return out + b.astype(u.dtype)


def segsum_exp(a_cs):
    l = a_cs.shape[-1]
    diff = a_cs[..., :, None] - a_cs[..., None, :]
    mask = jnp.tril(jnp.ones((l, l), dtype=bool))
    return jnp.exp(jnp.where(mask, diff, -jnp.inf))


def ssd_mixer(xs, z, Bm, Cm, dt_raw, conv_w, conv_b, dt_bias, a_log, d_skip, norm_w):
    Bsz, T = xs.shape[:2]
    xbc = jax.nn.silu(causal_dwconv(jnp.concatenate([xs, Bm, Cm], axis=-1), conv_w, conv_b))
    xs = xbc[..., :D_INNER]
    Bm = xbc[..., D_INNER:D_INNER + W_BC]
    Cm = xbc[..., D_INNER + W_BC:]
    dt = jax.nn.softplus(dt_raw.astype(jnp.float32) + dt_bias.astype(jnp.float32))
    A = -jnp.exp(a_log.astype(jnp.float32))
    lead = CHUNK - N_META
    n_real = T - N_META
    tail = (-(-n_real // CHUNK)) * CHUNK - n_real
    Tp = lead + T + tail
    nc = Tp // CHUNK
    hpg = SSM_HEADS // SSM_GROUPS

    def pad_t(a):
        return jnp.pad(a, [(0, 0), (lead, tail)] + [(0, 0)] * (a.ndim - 2))

    X = pad_t(xs).astype(jnp.float32).reshape(Bsz, nc, CHUNK, SSM_GROUPS, hpg, SSM_HEAD_DIM)
    dtp = pad_t(dt).reshape(Bsz, nc, CHUNK, SSM_GROUPS, hpg)
    Bc = pad_t(Bm).astype(jnp.float32).reshape(Bsz, nc, CHUNK, SSM_GROUPS, D_STATE)
    Cc = pad_t(Cm).astype(jnp.float32).reshape(Bsz, nc, CHUNK, SSM_GROUPS, D_STATE)
    a = jnp.moveaxis(dtp * A.reshape(SSM_GROUPS, hpg), 2, -1)
    a_cs = jnp.cumsum(a, axis=-1)
    Xdt = X * dtp[..., None]
    Lmat = segsum_exp(a_cs)
    cb = jnp.einsum('bclgn,bcsgn->bcgls', Cc, Bc)
    y_diag = jnp.einsum('bcgls,bcgjls,bcsgjp->bclgjp', cb, Lmat, Xdt)
    decay_states = jnp.exp(a_cs[..., -1:] - a_cs)
    states = jnp.einsum('bclgn,bcgjl,bclgjp->bcgjpn', Bc, decay_states, Xdt)
    chunk_decay = jnp.exp(a_cs[..., -1])

    def step(hc, inp):
        dec, st = inp
        return hc * dec[..., None, None] + st, hc

    h0 = jnp.zeros((Bsz, SSM_GROUPS, hpg, SSM_HEAD_DIM, D_STATE), jnp.float32)
    _, prev = lax.scan(step, h0, (jnp.moveaxis(chunk_decay, 1, 0), jnp.moveaxis(states, 1, 0)))
    prev = jnp.moveaxis(prev, 0, 1)
    y_off = jnp.einsum('bclgn,bcgjpn,bcgjl->bclgjp', Cc, prev, jnp.exp(a_cs))
    y = y_diag + y_off + X * d_skip.astype(jnp.float32).reshape(SSM_GROUPS, hpg)[..., None]
    y = y.reshape(Bsz, Tp, D_INNER)[:, lead:lead + T]
    y = y * jax.nn.silu(z.astype(jnp.float32))
    yg = y.reshape(Bsz, T, SSM_GROUPS, D_INNER // SSM_GROUPS)
    yg = yg * lax.rsqrt(jnp.mean(yg * yg, axis=-1, keepdims=True) + NORM_EPS)
    y = yg.reshape(Bsz, T, D_INNER) * norm_w.astype(jnp.float32)
    return y.astype(xs.dtype)


def peer(h, w_query, sub_keys, u, v):
    Bsz, T, D = h.shape
    n = Bsz * T
    n_blk = -(-n // PEER_TOK_BLOCK)
    hf = jnp.pad(h.reshape(n, D), [(0, n_blk * PEER_TOK_BLOCK - n), (0, 0)])
    blocks = hf.reshape(n_blk, PEER_TOK_BLOCK, D)
    half = PEER_KEY_DIM // 2
    keys_f = sub_keys.astype(jnp.float32)

    def one_block(xb):
        q = (xb @ w_query).astype(jnp.float32).reshape(PEER_TOK_BLOCK, PEER_HEADS, 2, half)
        s = jnp.einsum('thcd,ckd->thck', q, keys_f)
        top_s, top_i = lax.top_k(s, PEER_TOPK)
        cand_s = (top_s[:, :, 0, :, None] + top_s[:, :, 1, None, :]).reshape(PEER_TOK_BLOCK, PEER_HEADS, -1)
        cand_i = (top_i[:, :, 0, :, None] * N_KEYS + top_i[:, :, 1, None, :]).reshape(PEER_TOK_BLOCK, PEER_HEADS, -1)
        best_s, pos = lax.top_k(cand_s, PEER_TOPK)
        expert = jnp.take_along_axis(cand_i, pos, axis=-1)
        g = jax.nn.softmax(best_s, axis=-1)
        u_sel = u[expert].astype(jnp.float32)
        v_sel = v[expert].astype(jnp.float32)
        act = jax.nn.gelu(jnp.einsum('td,thkd->thk', xb.astype(jnp.float32), u_sel), approximate=False)
        out = jnp.einsum('thk,thkd->td', g * act, v_sel)
        return out.astype(h.dtype)

    out = lax.map(one_block, blocks).reshape(n_blk * PEER_TOK_BLOCK, D)[:n]
    return out.reshape(Bsz, T, D)


def setup_inputs(seed: int = 0) -> dict:
    key = jax.random.key(seed)
    ks = jax.random.split(key, 20)
    nrm = jax.random.normal
    dt0 = jnp.exp(jax.random.uniform(ks[6], (DEPTH, SSM_HEADS)) * (math.log(0.1) - math.log(0.001)) + math.log(0.001))
    return {
        'x': nrm(ks[0], (BATCH, SEQ, D_MODEL), jnp.float32),
        'meta_tokens': nrm(ks[1], (N_META, D_MODEL), jnp.float32),
        'norm_mix_w': 1.0 + 0.02 * nrm(ks[2], (DEPTH, D_MODEL), jnp.float32),
        'w_in': nrm(ks[3], (DEPTH, D_MODEL, IN_WIDTH), jnp.float32) * D_MODEL ** -0.5,
        'conv_w': nrm(ks[4], (DEPTH, CONV_WIDTH, CONV_DIM), jnp.float32) * CONV_WIDTH ** -0.5,
        'conv_b': 0.02 * nrm(ks[5], (DEPTH, CONV_DIM), jnp.float32),
        'dt_bias': dt0 + jnp.log(-jnp.expm1(-dt0)),
        'a_log': jnp.log(jax.random.uniform(ks[7], (DEPTH, SSM_HEADS), jnp.float32, 1.0, 16.0)),
        'd_skip': 1.0 + 0.1 * nrm(ks[8], (DEPTH, SSM_HEADS), jnp.float32),
        'ssm_norm_w': 1.0 + 0.02 * nrm(ks[9], (DEPTH, D_INNER), jnp.float32),
        'w_branch_attn': nrm(ks[10], (DEPTH, W_Q, D_MODEL), jnp.float32) * W_Q ** -0.5,
        'w_branch_ssm': nrm(ks[11], (DEPTH, D_INNER, D_MODEL), jnp.float32) * D_INNER ** -0.5,
        'w_out': nrm(ks[12], (DEPTH, D_MODEL, D_MODEL), jnp.float32) * D_MODEL ** -0.5,
        'norm_ffn_w': 1.0 + 0.02 * nrm(ks[13], (DEPTH, D_MODEL), jnp.float32),
        'peer_w_query': nrm(ks[14], (DEPTH, D_MODEL, PEER_HEADS * PEER_KEY_DIM), jnp.float32) * D_MODEL ** -0.5,
        'peer_sub_keys': nrm(ks[15], (DEPTH, 2, N_KEYS, PEER_KEY_DIM // 2), jnp.float32) * (PEER_KEY_DIM // 2) ** -0.5,
        'peer_u': nrm(ks[16], (DEPTH, N_EXPERTS, D_MODEL), jnp.float32) * D_MODEL ** -0.5,
        'peer_v': nrm(ks[17], (DEPTH, N_EXPERTS, D_MODEL), jnp.float32) * PEER_HEADS ** -0.5,
        'norm_final_w': 1.0 + 0.02 * nrm(ks[18], (D_MODEL,), jnp.float32),
    }


def reference(x, meta_tokens, norm_mix_w, w_in, conv_w, conv_b, dt_bias, a_log, d_skip, ssm_norm_w,
              w_branch_attn, w_branch_ssm, w_out, norm_ffn_w, peer_w_query, peer_sub_keys, peer_u, peer_v,
              norm_final_w):
    Bsz, S, _ = x.shape
    topk = min(TOPK_MAX, S // 4)
    meta = jnp.broadcast_to(meta_tokens.astype(x.dtype)[None], (Bsz, N_META, D_MODEL))
    h = jnp.concatenate([meta, x], axis=1)
    T = S + N_META
    pos = jnp.arange(T)
    widths = [W_Q, W_KV, W_KV, W_IQ, W_IK, W_IW, D_INNER, D_INNER, W_BC, W_BC, SSM_HEADS]
    split_at = []
    acc = 0
    for wd in widths:
        acc += wd
        split_at.append(acc)
    for l in range(DEPTH):
        hn = rms_norm(h, norm_mix_w[l])
        proj = hn @ w_in[l]
        q, k, v, qi, ki, wi, z, xs, Bm, Cm, dt_raw, gates = jnp.split(proj, split_at, axis=-1)
        q = rope(q.reshape(Bsz, T, ATTN_HEADS, HEAD_DIM), pos)
        k = rope(k.reshape(Bsz, T, ATTN_KV_HEADS, HEAD_DIM), pos)
        v = v.reshape(Bsz, T, ATTN_KV_HEADS, HEAD_DIM)
        qi = rope(qi.reshape(Bsz, T, IDX_HEADS, IDX_DIM), pos)
        ki = rope(ki[:, :, None, :], pos)[:, :, 0]
        attn = dsa_attention(q, k, v, qi, ki, wi, topk)
        ssm = ssd_mixer(xs, z, Bm, Cm, dt_raw, conv_w[l], conv_b[l], dt_bias[l], a_log[l], d_skip[l], ssm_norm_w[l])
        g = jax.nn.sigmoid(gates.astype(jnp.float32)).astype(h.dtype)
        g_attn = g[..., :D_MODEL]
        g_ssm = g[..., D_MODEL:]
        merged = g_attn * (attn @ w_branch_attn[l]) + g_ssm * (ssm @ w_branch_ssm[l])
        h = h + merged @ w_out[l]
        h = h + peer(rms_norm(h, norm_ffn_w[l]), peer_w_query[l], peer_sub_keys[l], peer_u[l], peer_v[l])
    h = rms_norm(h, norm_final_w)
    return h[:, N_META:]
```

```python
import math
import numpy as np
from contextlib import ExitStack
import concourse.bass as bass
import concourse.mybir as mybir
from concourse.bass_utils import run_bass_kernel_spmd

F32 = mybir.dt.float32
BF16 = mybir.dt.bfloat16
U8 = mybir.dt.uint8
AF = mybir.ActivationFunctionType
ALU = mybir.AluOpType

N_DMA_SEMS = 24
D = 2048
NT_CTX = 25
NT_OWN = 8
NT = NT_CTX + NT_OWN
NSLOT = NT * 128
CTX = NT_CTX * 128
EPS = 1e-6
O_Q, O_K, O_V, O_QI, O_KI, O_WI, O_Z, O_XS, O_B, O_C, O_DT, O_G = 0, 2048, 2560, 3072, 4096, 4160, 4176, 8272, 12368, 13392, 14416, 14480
R_Q, R_K, R_QI, R_KI = 0, 2048, 2560, 3584
NEG = -1.0e30
NEC = 32


class KB:
    ENGS = ("pe", "act", "dve", "pool", "sp")

    def __init__(self, nc):
        self.nc = nc
        self.es = ExitStack()
        self.ops = {e: [] for e in self.ENGS}
        self.sem = {e: self.es.enter_context(nc.semaphore("s_" + e)) for e in self.ENGS}
        self.cnt = {e: 0 for e in self.ENGS}
        self.dsem = [self.es.enter_context(nc.semaphore("s_dma%d" % i)) for i in range(N_DMA_SEMS)]
        self.dcnt = [0] * N_DMA_SEMS
        self.dnext = 0
        self.seen = {e: {} for e in self.ENGS}
        self.res = {}
        self.semobj = {}
        for e in self.ENGS:
            self.semobj[("e", e)] = self.sem[e]
        for i in range(N_DMA_SEMS):
            self.semobj[("d", i)] = self.dsem[i]
        self.arena = self.es.enter_context(nc.sbuf_tensor("arena", [128, 204 * 1024], U8))
        self.aoff = 0
        self.psum = [self.es.enter_context(nc.psum_tensor("psb%d" % i, [128, 2048], F32)) for i in range(2)]

    def alloc(self, shape, dt):
        esz = 4 if dt == F32 else 2
        n = int(np.prod(shape[1:]))
        nb = (n * esz + 63) // 64 * 64
        o = self.aoff
        self.aoff += nb
        assert self.aoff <= 204 * 1024, "SBUF arena overflow %d" % self.aoff
        v = self.arena[:, o:o + n * esz].bitcast(dt)
        if len(shape) == 3:
            v = v.rearrange("p (a b) -> p a b", b=shape[2])
        elif len(shape) == 4:
            v = v.rearrange("p (a b c) -> p a b c", b=shape[2], c=shape[3])
        if shape[0] < 128:
            v = v[0:shape[0]]
        return v

    def view(self, off, shape, dt):
        save = self.aoff
        self.aoff = off
        v = self.alloc(shape, dt)
        self.aoff = save
        return v

    def barrier(self):
        toks = [(("e", e), self.cnt[e]) for e in self.ENGS if self.cnt[e] > 0]
        toks += [(("d", i), self.dcnt[i]) for i in range(N_DMA_SEMS) if self.dcnt[i] > 0]
        for e in self.ENGS:
            w = []
            for kk, v in toks:
                if kk == ("e", e) or self.seen[e].get(kk, 0) >= v:
                    continue
                self.seen[e][kk] = v
                w.append((kk, v))
            if w:
                self.ops[e].append((w, None, None))

    def bank(self, i, dt=F32):
        t = self.psum[i // 4]
        v = t[:, (i % 4) * 512:(i % 4 + 1) * 512]
        if dt == BF16:
            v = v.bitcast(BF16)
        return v

    def _deps(self, eng, reads, writes):
        need = {}

        def add(tok):
            if tok is None:
                return
            k, v = tok
            if need.get(k, 0) < v:
                need[k] = v

        for r in reads:
            st = self.res.get(r)
            if st is not None:
                add(st[0])
        for w in writes:
            st = self.res.get(w)
            if st is not None:
                add(st[0])
                for k, v in st[1].items():
                    add((k, v))
        waits = []
        for k, v in need.items():
            if k == ("e", "pe") and eng == "pe":
                continue
            if self.seen[eng].get(k, 0) >= v:
                continue
            self.seen[eng][k] = v
            waits.append((k, v))
        return waits

    def _commit(self, tok, reads, writes):
        k, v = tok
        for r in reads:
            st = self.res.setdefault(r, [None, {}])
            if st[1].get(k, 0) < v:
                st[1][k] = v
        for w in writes:
            self.res[w] = [tok, {}]

    @staticmethod
    def _excl(reads, writes):
        ps = [r for r in reads if len(r) == 2 and r[0] == "b" and r[1].isdigit()]
        return (reads, list(writes) + ps) if ps else (reads, writes)

    def op(self, eng, fn, reads=(), writes=()):
        reads, writes = self._excl(reads, writes)
        waits = self._deps(eng, reads, writes)
        self.cnt[eng] += 1
        tok = (("e", eng), self.cnt[eng])
        self.ops[eng].append((waits, fn, (("e", eng), 1)))
        self._commit(tok, reads, writes)
        return tok

    def dma(self, eng, out, in_, reads=(), writes=()):
        i = self.dnext
        self.dnext = (self.dnext + 1) % N_DMA_SEMS
        waits = self._deps(eng, reads, writes)
        prev = self.dcnt[i]
        k = ("d", i)
        if prev > 0 and self.seen[eng].get(k, 0) < prev:
            self.seen[eng][k] = prev
            waits.append((k, prev))
        self.dcnt[i] += 16
        tok = (k, self.dcnt[i])
        self.ops[eng].append((waits, lambda e: e.dma_start(out=out, in_=in_), (k, 16)))
        self._commit(tok, reads, writes)
        return tok

    def wait_all(self, eng, toks):
        self.ops[eng].append((list(toks), None, None))

    def mm(self, out, lhsT, rhs, start, stop, reads, writes):
        return self.op("pe", lambda e: e.matmul(out, lhsT, rhs, start=start, stop=stop), reads, writes)

    def tr(self, out, in_, ident, reads, writes):
        return self.op("pe", lambda e: e.transpose(out, in_, ident), reads, writes)

    def act(self, out, in_, func, reads, writes, bias=None, scale=None, accum_out=None):
        kw = {}
        if bias is not None:
            kw["bias"] = bias
        if scale is not None:
            kw["scale"] = scale
        if accum_out is not None:
            kw["accum_out"] = accum_out
        return self.op("act", lambda e: e.activation(out, in_, func, **kw), reads, writes)

    def tt(self, eng, out, in0, in1, op, reads, writes):
        return self.op(eng, lambda e: e.tensor_tensor(out=out, in0=in0, in1=in1, op=op), reads, writes)

    def ts(self, eng, out, in0, s1, op0, reads, writes, s2=None, op1=None, accum_out=None):
        kw = {}
        if op1 is not None:
            kw["op1"] = op1
        if accum_out is not None:
            kw["accum_out"] = accum_out
        return self.op(eng, lambda e: e.tensor_scalar(out=out, in0=in0, scalar1=s1, scalar2=s2, op0=op0, **kw), reads, writes)

    def stt(self, eng, out, in0, scalar, in1, op0, op1, reads, writes, accum_out=None):
        kw = {}
        if accum_out is not None:
            kw["accum_out"] = accum_out
        return self.op(eng, lambda e: e.scalar_tensor_tensor(out=out, in0=in0, scalar=scalar, in1=in1, op0=op0, op1=op1, **kw), reads, writes)

    def cp(self, eng, out, in_, reads, writes):
        if eng == "act":
            return self.op("act", lambda e: e.copy(out, in_), reads, writes)
        return self.op(eng, lambda e: e.tensor_copy(out, in_), reads, writes)

    def memset(self, eng, ap, val, writes):
        return self.op(eng, lambda e: e.memset(ap, val), (), writes)

    def emit(self):
        nc = self.nc
        engmap = {"pe": "tensor", "act": "scalar", "dve": "vector", "pool": "gpsimd", "sp": "sync"}
        with nc.Block() as block:
            for en in self.ENGS:
                ops = self.ops[en]

                def body(engobj, ops=ops):
                    for waits, fn, inc in ops:
                        for k, v in waits:
                            engobj.wait_ge(self.semobj[k], v)
                        if fn is not None:
                            ins = fn(engobj)
                            ins.then_inc(self.semobj[inc[0]], inc[1])

                getattr(block, engmap[en])(body)
        self.es.close()


DBG_STOP = [None]


class _Stop(Exception):
    pass


def ck(n):
    if DBG_STOP[0] == n:
        raise _Stop()


def bc(ap, axis, shape):
    return ap.unsqueeze(axis).to_broadcast(list(shape))


def build_program(phases=(1, 2, 3, 4, 5), dbg=False, gsel=None):
    nc = bass.Bass("TRN2", target_bir_lowering=False)
    st = {}
    try:
        _build_body(nc, phases, dbg, gsel, st)
    except _Stop:
        pass
    k = st["k"]
    k.wait_all("sp", st["out_toks"])
    k.emit()
    return nc, st["used"]


def _build_body(nc, phases, dbg, gsel, st):
    used_inputs = []
    NEEDS = {"xin": (1, 2, 3, 4), "tabs": (2, 3), "valid": (1,), "padb": (3,), "cst": (1, 2, 3, 4, 5), "w_in": (1, 2, 3, 4),
             "w_rot": (2, 3), "convw": (1,), "convb": (1,), "dtb": (1,), "alog": (1,), "dsk": (1,), "snw": (1,),
             "nmix": (1, 2, 3, 4), "nffn": (5,), "nfin": (5,), "w_pa": (4,), "w_ps": (4,), "w_o": (4,), "w_pq": (5,),
             "keysT": (5,), "uT": (5,), "pv": (5,)}

    def din(name, shape, dt=F32):
        if not any(p in phases for p in NEEDS[name]):
            return None
        used_inputs.append(name)
        return nc.dram_tensor(name, list(shape), dt, kind="ExternalInput").ap()

    xin = din("xin", [NSLOT, D])
    tabs = din("tabs", [4, 128, NSLOT])
    valid = din("valid", [128, NT])
    padb = din("padb", [CTX])
    cst = din("cst", [128, 5, 128])
    w_in = din("w_in", [D, 18576])
    w_rot = din("w_rot", [D, 3648])
    convw = din("convw", [128, 48, 4])
    convb = din("convb", [128, 48])
    dtb = din("dtb", [64])
    alog = din("alog", [64])
    dsk = din("dsk", [64])
    snw = din("snw", [128, 32])
    nmix = din("nmix", [128, 16])
    nffn = din("nffn", [128, 16])
    nfin = din("nfin", [D])
    w_pa = din("w_pa", [2048, 2048])
    w_ps = din("w_ps", [4096, 2048])
    w_o = din("w_o", [2048, 2048])
    w_pq = din("w_pq", [2048, 2048])
    keysT = din("keysT", [128, 2, 128])
    uT = din("uT", [2048, 16384])
    pv = din("pv", [16384, 2048])
    yout = nc.dram_tensor("y", [NT_OWN * 128, D], F32, kind="ExternalOutput").ap()
    st_attn = nc.dram_tensor("st_attn", [128, 16, NT_OWN * 128], BF16).ap()
    st_ssm = nc.dram_tensor("st_ssm", [128, 32, NT_OWN * 128], BF16).ap()
    dbg_out = {}
    if dbg:
        dbg_out["d_ssm"] = nc.dram_tensor("d_ssm", [128, 32, NT_OWN * 128], BF16, kind="ExternalOutput").ap()
        dbg_out["d_attn"] = nc.dram_tensor("d_attn", [128, 16, NT_OWN * 128], BF16, kind="ExternalOutput").ap()
        dbg_out["d_h2"] = nc.dram_tensor("d_h2", [128, NT_OWN, D], F32, kind="ExternalOutput").ap()
        dbg_out["d_score"] = nc.dram_tensor("d_score", [128, NSLOT], F32, kind="ExternalOutput").ap()
        dbg_out["d_mask"] = nc.dram_tensor("d_mask", [128, NSLOT], BF16, kind="ExternalOutput").ap()
        dbg_out["d_thr"] = nc.dram_tensor("d_thr", [128, 8], F32, kind="ExternalOutput").ap()
        dbg_out["d_po"] = nc.dram_tensor("d_po", [128, 1024], F32, kind="ExternalOutput").ap()
        dbg_out["d_q"] = nc.dram_tensor("d_q", [128, 16, 128], BF16, kind="ExternalOutput").ap()
        dbg_out["d_k"] = nc.dram_tensor("d_k", [128, 4, NSLOT], BF16, kind="ExternalOutput").ap()

    k = KB(nc)
    out_toks = []
    st["k"] = k
    st["out_toks"] = out_toks
    st["used"] = used_inputs

    ck(-1)
    cstf = k.alloc([128, 5, 128], F32)
    k.dma("sp", cstf, cst, writes=["cst"])
    identf, tri_le, negtri, causb, onesf = (cstf[:, i, :] for i in range(5))
    identb = k.alloc([128, 128], BF16)
    k.dma("pool", identb, cst[:, 0, :], writes=["identb"])
    ck(-2)
    nw_fm = k.alloc([128, 16], F32)
    xt = k.alloc([128, D], F32)
    hn = k.alloc([128, D], BF16)
    sq = k.alloc([128, 4], F32)
    hnT = k.alloc([128, 16, 512], BF16)
    wb = [k.alloc([128, 16 * 512], BF16) for _ in range(2)]
    wsel = [0]

    def load_w(dram, K, c0, ncols, buf=None):
        if buf is None:
            i = wsel[0]
            wsel[0] ^= 1
        else:
            i = buf
        kt = K // 128
        v = wb[i][:, 0:kt * ncols].rearrange("p (a b) -> p a b", b=ncols)
        k.dma("pool", v, dram[:, c0:c0 + ncols].rearrange("(a p) c -> p a c", p=128), writes=["wb%d" % i])
        return v, "wb%d" % i

    def make_hnT(src_tiles, keys=None):
        for j, src in enumerate(src_tiles):
            if keys is None:
                k.dma("sp", xt, src, writes=["xt"])
                xsrc, xkey = xt, "xt"
            else:
                xsrc, xkey = src, keys[j]
            k.memset("pool", sq[:, 0:1], 0.0, ["sq0"])
            k.act(hn, xsrc, AF.Square, [xkey, "sq0"], ["hn", "sq0"], accum_out=sq[:, 0:1])
            k.ts("dve", sq[:, 1:2], sq[:, 0:1], 1.0 / D, ALU.mult, ["sq0"], ["sq1"], s2=EPS, op1=ALU.add)
            k.act(sq[:, 2:3], sq[:, 1:2], AF.Sqrt, ["sq1"], ["sq2"])
            k.op("dve", lambda e: e.reciprocal(sq[:, 3:4], sq[:, 2:3]), ["sq2"], ["sq3"])
            k.ts("dve", hn, xsrc, sq[:, 3:4], ALU.mult, [xkey, "sq3", "hn"], ["hn"])
            ck(11)
            pT = k.psum[1][:, 0:1024].bitcast(BF16)
            for kt in range(16):
                k.tr(pT[:, kt * 128:(kt + 1) * 128], hn[:, kt * 128:(kt + 1) * 128], identb, ["hn", "identb"], ["b4", "b5"])
            ck(12)
            k.tt("dve", hnT[:, :, j * 128:(j + 1) * 128], pT.rearrange("p (a b) -> p a b", b=128),
                 bc(nw_fm, 2, [128, 16, 128]), ALU.mult, ["b4", "b5", "nw"], ["hnT"])

    def proj_fm(wv, wkey, cc, N, bank, nk=16, rhs=None, rkey="hnT", start=True, stop=True):
        ps = k.bank(bank)[:, 0:N]
        r = hnT if rhs is None else rhs
        for kt in range(nk):
            k.mm(ps, wv[:, kt, cc * 128:(cc + 1) * 128], r[:, kt, 0:N], start and kt == 0, stop and kt == nk - 1,
                 [wkey, rkey], ["b%d" % bank])
        return ps

    groups = [[0]] + [list(range(1 + 4 * i, 5 + 4 * i)) for i in range(8)]
    gsel = tuple(range(9)) if gsel is None else tuple(gsel)

    mark0 = k.aoff
    if 1 in phases:
        k.dma("sp", nw_fm, nmix, writes=["nw"])
        cw = k.alloc([128, 48, 4], F32)
        cb = k.alloc([128, 48], F32)
        k.dma("sp", cw, convw, writes=["cw"])
        k.dma("sp", cb, convb, writes=["cw"])
        snwf = k.alloc([128, 32], F32)
        k.dma("sp", snwf, snw, writes=["snw"])
        validf = k.alloc([128, NT], F32)
        k.dma("sp", validf, valid, writes=["valid"])
        ck(-3)
        dtb_r = k.alloc([128, 64], F32)
        A_r = k.alloc([128, 64], F32)
        dsk_r = k.alloc([128, 64], F32)
        k.dma("sp", dtb_r, dtb.partition_broadcast(128), writes=["dtb"])
        k.dma("sp", A_r, alog.partition_broadcast(128), writes=["A"])
        k.dma("sp", dsk_r, dsk.partition_broadcast(128), writes=["dsk"])
        k.act(A_r, A_r, AF.Exp, ["A"], ["A"])
        k.ts("dve", A_r, A_r, -1.0, ALU.mult, ["A"], ["A"])
        ck(-4)
        hist = k.alloc([128, 48, 3], F32)
        k.memset("pool", hist, 0.0, ["hist"])
        H = k.alloc([128, 4096], F32)
        k.memset("pool", H, 0.0, ["H"])
        Hb = k.alloc([128, 512], BF16)
        ext_ = [k.alloc([128, 515], F32) for _ in range(2)]
        acc_ = [k.alloc([128, 512], F32) for _ in range(2)]
        xc_ = [k.alloc([128, 512], BF16) for _ in range(2)]
        cvsel = [0]
        Xdt = k.alloc([128, 4, 512], BF16)
        Xdec = k.alloc([128, 4, 512], BF16)
        XD = k.alloc([128, 4, 512], F32)
        Btm = k.alloc([128, 4, 128], BF16)
        BT = k.alloc([128, 512], BF16)
        CT = k.alloc([128, 512], BF16)
        sz = k.alloc([128, 4, 512], BF16)
        dtv = k.alloc([128, 4, 64], F32)
        av = k.alloc([128, 4, 64], F32)
        acs = k.alloc([128, 4, 64], F32)
        nacs = k.alloc([128, 4, 64], F32)
        edec = k.alloc([128, 4, 64], F32)
        wst = k.alloc([128, 4, 64], F32)
        eav = k.alloc([128, 4, 64], F32)
        tmp64 = k.alloc([128, 64], F32)
        AT = k.alloc([128, 8, 128], F32)
        LT = k.alloc([128, 8, 128], BF16)
        MT = k.alloc([128, 8, 128], BF16)
        yv = k.alloc([128, 512], F32)
        ynb = k.alloc([128, 512], BF16)
        ysq = k.alloc([128, 4], F32)
        sst = k.alloc([128, 4, 128], BF16)
        ck(-5)
        wdt = k.alloc([128, 16, 64], BF16)
        k.dma("pool", wdt, w_in[:, O_DT:O_DT + 64].rearrange("(a p) c -> p a c", p=128), writes=["wdt"])
        ck(-6)
        negtri4 = k.alloc([128, 4, 128], F32)
        for i in range(4):
            k.cp("dve", negtri4[:, i, :], negtri, ["cst"], ["negtri4"])

        def conv_chunk(ps, N, ch, outbf, okey, first_tile_reads):
            cv = cvsel[0]
            cvsel[0] ^= 1
            ext, acc, ek, ak = ext_[cv], acc_[cv], "ext%d" % cv, "acc%d" % cv
            k.cp("dve", ext[:, 0:3], hist[:, ch, :], ["hist"], [ek])
            k.cp("act", ext[:, 3:3 + N], ps, first_tile_reads, [ek])
            k.cp("dve", hist[:, ch, :], ext[:, N:N + 3], [ek], ["hist"])
            k.ts("dve", acc[:, 0:N], ext[:, 3:3 + N], cw[:, ch, 3:4], ALU.mult, [ek, "cw"], [ak], s2=cb[:, ch:ch + 1], op1=ALU.add)
            for tap in range(3):
                k.stt("dve", acc[:, 0:N], ext[:, tap:tap + N], cw[:, ch, tap:tap + 1], acc[:, 0:N], ALU.mult, ALU.add, [ek, "cw", ak], [ak])
            k.act(outbf, acc[:, 0:N], AF.Silu, [ak], [okey])

        ck(1)
        for gi, tiles in enumerate(groups):
            if gi not in gsel:
                continue
            own = gi >= 7
            needC = gi >= 6
            nt = len(tiles)
            N = 128 * nt
            make_hnT([xin[t * 128:(t + 1) * 128, :] for t in tiles])
            ck(2)
            for j, t in enumerate(tiles):
                ps = k.bank(6)[:, 0:64]
                for kt in range(16):
                    k.mm(ps, hnT[:, kt, j * 128:(j + 1) * 128], wdt[:, kt, :], kt == 0, kt == 15, ["hnT", "wdt"], ["b6"])
                k.tt("dve", tmp64, ps, dtb_r, ALU.add, ["b6", "dtb"], ["tmp64"])
                ck(31)
                k.act(tmp64, tmp64, AF.Exp, ["tmp64"], ["tmp64"])
                k.act(tmp64, tmp64, AF.Ln, ["tmp64"], ["tmp64"], bias=1.0)
                ck(32)
                k.ts("dve", dtv[:, j, :], tmp64, validf[:, t:t + 1], ALU.mult, ["tmp64", "valid"], ["dtv"])
                k.tt("dve", av[:, j, :], dtv[:, j, :], A_r, ALU.mult, ["dtv", "A"], ["av"])
                ps2 = k.bank(6)[:, 64:128]
                k.mm(ps2, tri_le, av[:, j, :], True, True, ["cst", "av"], ["b6"])
                ps3 = k.bank(6)[:, 128:192]
                k.mm(ps3, onesf, av[:, j, :], True, True, ["cst", "av"], ["b6"])
                ck(33)
                k.cp("dve", acs[:, j, :], ps2, ["b6"], ["acs"])
                ck(331)
                k.ts("dve", nacs[:, j, :], ps2, -1.0, ALU.mult, ["b6"], ["nacs"])
                ck(332)
                k.act(edec[:, j, :], ps3, AF.Exp, ["b6"], ["edec"])
                ck(333)
                k.tt("dve", tmp64, ps3, acs[:, j, :], ALU.subtract, ["b6", "acs"], ["tmp64"])
                ck(334)
                k.act(tmp64, tmp64, AF.Exp, ["tmp64"], ["tmp64"])
                ck(335)
                k.tt("dve", wst[:, j, :], tmp64, dtv[:, j, :], ALU.mult, ["tmp64", "dtv"], ["wst"])
                ck(34)
                if own:
                    k.act(eav[:, j, :], acs[:, j, :], AF.Exp, ["acs"], ["eav"])
            ck(3)
            for g in range(8):
                hs = slice(g * 8, (g + 1) * 8)
                wv, wkey = load_w(w_in, D, O_XS + g * 512, 512, buf=0)
                wvb = wb[1][:, 0:16 * 256].rearrange("p (a b) -> p a b", b=256)
                k.dma("pool", wvb[:, :, 0:128], w_in[:, O_B + g * 128:O_B + (g + 1) * 128].rearrange("(a p) c -> p a c", p=128), writes=["wb1"])
                if needC:
                    k.dma("pool", wvb[:, :, 128:256], w_in[:, O_C + g * 128:O_C + (g + 1) * 128].rearrange("(a p) c -> p a c", p=128), writes=["wb1"])
                pTX = k.psum[0][:, 1024:2048].bitcast(BF16).rearrange("p (a b) -> p a b", b=512)
                pTB = k.bank(2, BF16)
                chunks = [("xs", cc) for cc in range(4)] + [("B", 0)] + ([("C", 1)] if needC else [])

                def do_proj(ch, wv=wv, wkey=wkey, wvb=wvb):
                    kind, i = ch
                    if kind == "xs":
                        return proj_fm(wv, wkey, i, N, i % 2)
                    return proj_fm(wvb, "wb1", i, N, i % 2)

                def do_post(ch, ps, g=g, hs=hs):
                    kind, i = ch
                    if kind == "xs":
                        xc, xck = xc_[i % 2], "xc%d" % (i % 2)
                        conv_chunk(ps, N, g * 4 + i, xc[:, 0:N], xck, ["b%d" % (i % 2)])
                        for j in range(nt):
                            k.tr(pTX[:, j, i * 128:(i + 1) * 128], xc[:, j * 128:(j + 1) * 128], identb, [xck, "identb"], ["b2", "b3"])
                        if i == 3:
                            for j in range(nt):
                                src = pTX[:, j, :].rearrange("p (h d) -> p h d", d=64)
                                k.tt("dve", Xdt[:, j, :].rearrange("p (h d) -> p h d", d=64), src, bc(dtv[:, j, hs], 2, [128, 8, 64]), ALU.mult, ["b2", "b3", "dtv"], ["Xdt"])
                                k.tt("dve", Xdec[:, j, :].rearrange("p (h d) -> p h d", d=64), src, bc(wst[:, j, hs], 2, [128, 8, 64]), ALU.mult, ["b2", "b3", "wst"], ["Xdec"])
                                if own:
                                    k.tt("dve", XD[:, j, :].rearrange("p (h d) -> p h d", d=64), src, bc(dsk_r[:, hs], 2, [128, 8, 64]), ALU.mult, ["b2", "b3", "dsk"], ["XD"])
                    elif kind == "B":
                        conv_chunk(ps, N, 32 + g, BT[:, 0:N], "BT", ["b0"])
                        for j in range(nt):
                            k.tr(pTB[:, j * 128:(j + 1) * 128], BT[:, j * 128:(j + 1) * 128], identb, ["BT", "identb"], ["b2", "b3"])
                        k.cp("act", Btm[:, 0:nt, :], pTB[:, 0:N].rearrange("p (a b) -> p a b", b=128), ["b2", "b3"], ["Btm"])
                    else:
                        conv_chunk(ps, N, 40 + g, CT[:, 0:N], "CT", ["b1"])

                pending = None
                for ch in chunks:
                    ps = do_proj(ch)
                    if pending is not None:
                        do_post(*pending)
                    pending = (ch, ps)
                do_post(*pending)
                if own:
                    wvz, wkeyz = load_w(w_in, D, O_Z + g * 512, 512, buf=0)
                    for j in range(nt):
                        ps = k.bank(j % 2)
                        for kt in range(16):
                            k.mm(ps, hnT[:, kt, j * 128:(j + 1) * 128], wvz[:, kt, :], kt == 0, kt == 15, ["hnT", wkeyz], ["b%d" % (j % 2)])
                        k.act(sz[:, j, :], ps, AF.Silu, ["b%d" % (j % 2)], ["sz"])
                ck(7)
                Hg = H[:, g * 512:(g + 1) * 512]
                for j, t in enumerate(tiles):
                    js = slice(j * 128, (j + 1) * 128)
                    if own:
                        k.cp("act", Hb, Hg, ["H"], ["Hb"])
                        ps_yo = k.bank(6)
                        k.mm(ps_yo, CT[:, js], Hb, True, True, ["CT", "Hb"], ["b6"])
                        ps_cb = k.bank(7)[:, 0:128]
                        k.mm(ps_cb, BT[:, js], CT[:, js], True, True, ["BT", "CT"], ["b7"])
                        k.tt("pool", AT, bc(tri_le, 1, [128, 8, 128]), bc(av[:, j, hs], 2, [128, 8, 128]), ALU.mult, ["cst", "av"], ["AT"])
                        psL = k.psum[1][:, 0:1024].rearrange("p (a b) -> p a b", b=128)
                        for hf in range(2):
                            k.mm(psL[:, hf * 4:(hf + 1) * 4, :], onesf, AT[:, hf * 4:(hf + 1) * 4, :], True, False, ["cst", "AT"], ["b4", "b5"])
                            k.mm(psL[:, hf * 4:(hf + 1) * 4, :], identf, negtri4, False, True, ["cst", "negtri4"], ["b4", "b5"])
                        for hh in range(8):
                            k.act(LT[:, hh, :], psL[:, hh, :], AF.Exp, ["b4", "b5", "nacs"], ["LT"], bias=nacs[:, j, g * 8 + hh:g * 8 + hh + 1])
                        k.tt("dve", MT, LT, bc(ps_cb, 1, [128, 8, 128]), ALU.mult, ["LT", "b7"], ["MT"])
                        ps_y = k.bank(7)[:, 0:512]
                        for hh in range(8):
                            k.mm(ps_y[:, hh * 64:(hh + 1) * 64], MT[:, hh, :], Xdt[:, j, hh * 64:(hh + 1) * 64], True, True, ["MT", "Xdt"], ["b7"])
                        y3 = yv.rearrange("p (h d) -> p h d", d=64)
                        k.tt("dve", y3, ps_yo.rearrange("p (h d) -> p h d", d=64), bc(eav[:, j, hs], 2, [128, 8, 64]), ALU.mult, ["b6", "eav"], ["yv"])
                        k.tt("dve", yv, yv, ps_y, ALU.add, ["yv", "b7"], ["yv"])
                        k.tt("dve", yv, yv, XD[:, j, :], ALU.add, ["yv", "XD"], ["yv"])
                        k.tt("dve", yv, yv, sz[:, j, :], ALU.mult, ["yv", "sz"], ["yv"])
                        k.memset("pool", ysq[:, 0:1], 0.0, ["ysq0"])
                        k.act(ynb, yv, AF.Square, ["yv", "ysq0"], ["ynb", "ysq0"], accum_out=ysq[:, 0:1])
                        k.ts("dve", ysq[:, 1:2], ysq[:, 0:1], 1.0 / 512, ALU.mult, ["ysq0"], ["ysq1"], s2=EPS, op1=ALU.add)
                        k.act(ysq[:, 2:3], ysq[:, 1:2], AF.Sqrt, ["ysq1"], ["ysq2"])
                        k.op("dve", lambda e: e.reciprocal(ysq[:, 3:4], ysq[:, 2:3]), ["ysq2"], ["ysq3"])
                        k.ts("dve", ynb, yv, ysq[:, 3:4], ALU.mult, ["yv", "ysq3", "ynb"], ["ynb"])
                        pTy = k.bank(3, BF16)
                        for cc in range(4):
                            k.tr(pTy[:, cc * 128:(cc + 1) * 128], ynb[:, cc * 128:(cc + 1) * 128], identb, ["ynb", "identb"], ["b2", "b3"])
                        for cc in range(4):
                            k.act(sst[:, cc, :], pTy[:, cc * 128:(cc + 1) * 128], AF.Copy, ["b2", "b3", "snw"], ["sst"], scale=snwf[:, g * 4 + cc:g * 4 + cc + 1])
                        ot = t - NT_CTX
                        k.dma("sp", st_ssm[:, g * 4:(g + 1) * 4, ot * 128:(ot + 1) * 128], sst, reads=["sst"], writes=["st_ssm"])
                    sb_ = 6 if own else 6 + j % 2
                    ps_S = k.bank(sb_)
                    k.mm(ps_S, Btm[:, j, :], Xdec[:, j, :], True, True, ["Btm", "Xdec"], ["b%d" % sb_])
                    Hg3 = Hg.rearrange("p (h d) -> p h d", d=64)
                    k.tt("dve", Hg3, Hg3, bc(edec[:, j, hs], 2, [128, 8, 64]), ALU.mult, ["H", "edec"], ["H"])
                    k.tt("dve", Hg, Hg, ps_S, ALU.add, ["H", "b%d" % sb_], ["H"])
                ck(8)
        if dbg:
            dtile = k.alloc([128, 32, 128], BF16)
            for ot in range(NT_OWN):
                k.dma("sp", dtile, st_ssm[:, :, ot * 128:(ot + 1) * 128], reads=["st_ssm"], writes=["dtile"])
                out_toks.append(k.dma("sp", dbg_out["d_ssm"][:, :, ot * 128:(ot + 1) * 128], dtile, reads=["dtile"]))
    k.aoff = mark0

    def rope_fm(ps_a, ps_b, tbuf, tsel, N, out, okey, akey, bkey, r1, r2):
        k.tt("dve", r1[:, 0:N], ps_a, tbuf[:, tsel, 0:N], ALU.mult, [akey, "tb"], ["r1"])
        k.tt("dve", r2[:, 0:N], ps_b, tbuf[:, tsel + 1, 0:N], ALU.mult, [bkey, "tb"], ["r2"])
        k.tt("dve", out, r1[:, 0:N], r2[:, 0:N], ALU.add, ["r1", "r2"], [okey])

    if 2 in phases:
        k.barrier()
        k.dma("sp", nw_fm, nmix, writes=["nw"])
        KT = k.alloc([128, 4, NSLOT], BF16)
        V = k.alloc([128, NT, 4, 130], BF16)
        kiT = k.alloc([128, NSLOT], BF16)
        mark2 = k.aoff
        k.memset("pool", V, 1.0, ["V"])
        tb = k.alloc([128, 4, 512], F32)
        r1 = k.alloc([128, 512], F32)
        r2 = k.alloc([128, 512], F32)
        for gi, tiles in enumerate(groups):
            if gi not in gsel:
                continue
            nt = len(tiles)
            N = 128 * nt
            s0 = tiles[0] * 128
            make_hnT([xin[t * 128:(t + 1) * 128, :] for t in tiles])
            k.dma("sp", tb[:, :, 0:N], tabs[:, :, s0:s0 + N].rearrange("a p s -> p a s"), writes=["tb"])
            wv, wkey = load_w(w_in, D, O_K, 512, buf=0)
            wr, rkey = load_w(w_rot, D, R_K, 512, buf=1)
            for cc in range(4):
                pa = proj_fm(wv, wkey, cc, N, 0)
                pb = proj_fm(wr, rkey, cc, N, 1)
                rope_fm(pa, pb, tb, 0, N, KT[:, cc, s0:s0 + N], "KT", "b0", "b1", r1, r2)
            wki = wb[0][:, 0:16 * 256].rearrange("p (a b) -> p a b", b=256)
            for half in range(2):
                k.dma("pool", wki[:, :, half * 64:(half + 1) * 64], w_in[:, O_KI:O_KI + 64].rearrange("(a p) c -> p a c", p=128), writes=["wb0"])
                k.dma("pool", wki[:, :, 128 + half * 64:128 + (half + 1) * 64], w_rot[:, R_KI:R_KI + 64].rearrange("(a p) c -> p a c", p=128), writes=["wb0"])
            pa = proj_fm(wki, "wb0", 0, N, 0)
            pb = proj_fm(wki, "wb0", 1, N, 1)
            rope_fm(pa, pb, tb, 2, N, kiT[:, s0:s0 + N], "kiT", "b0", "b1", r1, r2)
            wvv, wkeyv = load_w(w_in, D, O_V, 512, buf=1)
            for j, t in enumerate(tiles):
                ps = k.bank(2 + j % 2)
                for kt in range(16):
                    k.mm(ps, hnT[:, kt, j * 128:(j + 1) * 128], wvv[:, kt, :], kt == 0, kt == 15, ["hnT", wkeyv], ["b%d" % (2 + j % 2)])
                k.cp("act", V[:, t, :, 0:128], ps.rearrange("p (g d) -> p g d", d=128), ["b%d" % (2 + j % 2)], ["V"])
        k.aoff = mark2

    if 3 in phases:
        k.barrier()
        padb_r = k.alloc([128, CTX], BF16)
        k.dma("pool", padb_r, padb.partition_broadcast(128), writes=["padb"])
        tb3 = k.alloc([128, 4, 128], F32)
        r1 = k.alloc([128, 128], F32)
        r2 = k.alloc([128, 128], F32)
        qT = k.alloc([128, 16, 128], BF16)
        qiT = k.alloc([128, 8, 128], BF16)
        wis = k.alloc([128, 16], F32)
        score = k.alloc([128, NSLOT], F32)
        wm_off = k.aoff
        work = k.alloc([128, NSLOT], F32)
        maskb = k.view(wm_off, [128, NSLOT], BF16)
        maskT = k.view(wm_off + NSLOT * 2, [128, NT, 128], BF16)
        rl_ = [k.alloc([128, 512], F32) for _ in range(2)]
        m8 = k.alloc([128, 8], F32)
        thr = k.alloc([128, 1], F32)
        Et_ = [k.alloc([128, 4, 128], BF16) for _ in range(2)]
        Em_ = [k.alloc([128, 4, 128], BF16) for _ in range(2)]
        ao = k.alloc([128, 16, 128], BF16)
        rs = k.alloc([128, 4], F32)
        aT = k.alloc([128, 16, 128], BF16)
        wwi = k.alloc([128, 16, 16], BF16)
        k.dma("pool", wwi, w_in[:, O_WI:O_WI + 16].rearrange("(a p) c -> p a c", p=128), writes=["wwi"])
        for t in range(NT_CTX, NT):
            if (7 if t < NT_CTX + 4 else 8) not in gsel:
                continue
            ot = t - NT_CTX
            N = 128
            s0 = t * 128
            js = slice(0, 128)
            make_hnT([xin[t * 128:(t + 1) * 128, :]])
            k.dma("sp", tb3, tabs[:, :, s0:s0 + N].rearrange("a p s -> p a s"), writes=["tb"])
            for c4 in range(4):
                wv, wkey = load_w(w_in, D, O_Q + c4 * 512, 512, buf=0)
                wr, rkey = load_w(w_rot, D, R_Q + c4 * 512, 512, buf=1)
                for cc in range(4):
                    pa = proj_fm(wv, wkey, cc, N, 0)
                    pb = proj_fm(wr, rkey, cc, N, 1)
                    rope_fm(pa, pb, tb3, 0, N, qT[:, c4 * 4 + cc, :], "qT", "b0", "b1", r1, r2)
            for c4 in range(2):
                wv, wkey = load_w(w_in, D, O_QI + c4 * 512, 512, buf=0)
                wr, rkey = load_w(w_rot, D, R_QI + c4 * 512, 512, buf=1)
                for cc in range(4):
                    pa = proj_fm(wv, wkey, cc, N, 0)
                    pb = proj_fm(wr, rkey, cc, N, 1)
                    rope_fm(pa, pb, tb3, 2, N, qiT[:, c4 * 4 + cc, :], "qiT", "b0", "b1", r1, r2)
            ps = k.bank(2)[:, 0:16]
            for kt in range(16):
                k.mm(ps, hnT[:, kt, 0:128], wwi[:, kt, :], kt == 0, kt == 15, ["hnT", "wwi"], ["b2"])
            k.ts("dve", wis, ps, 1.0 / 32.0, ALU.mult, ["b2"], ["wis"])
            nk = t + 1
            S = nk * 128
            chunks = [(c0, min(512, CTX - c0)) for c0 in range(0, CTX, 512)] + [(c0, min(512, S - c0)) for c0 in range(CTX, S, 512)]
            for ci, (c0, n) in enumerate(chunks):
                for h in range(16):
                    hp = slice((h % 2) * 64, (h % 2) * 64 + 64)
                    bk = 2 + (h % 2)
                    rl, rlk = rl_[h % 2], "rl%d" % (h % 2)
                    ps = k.bank(bk)[:, 0:n]
                    k.mm(ps, qiT[hp, h // 2, :], kiT[hp, c0:c0 + n], True, True, ["qiT", "kiT"], ["b%d" % bk])
                    k.act(rl[:, 0:n], ps, AF.Relu, ["b%d" % bk], [rlk])
                    if h == 0:
                        if c0 < CTX:
                            k.stt("dve", score[:, c0:c0 + n], rl[:, 0:n], wis[:, 0:1], padb_r[:, c0:c0 + n], ALU.mult, ALU.add, [rlk, "wis", "padb"], ["score"])
                        else:
                            k.ts("dve", score[:, c0:c0 + n], rl[:, 0:n], wis[:, 0:1], ALU.mult, [rlk, "wis"], ["score"])
                    else:
                        k.stt("dve", score[:, c0:c0 + n], rl[:, 0:n], wis[:, h:h + 1], score[:, c0:c0 + n], ALU.mult, ALU.add, [rlk, "wis", "score"], ["score"])
            k.tt("dve", score[:, S - 128:S], score[:, S - 128:S], causb, ALU.add, ["score", "cst"], ["score"])
            if dbg and t == NT_CTX:
                out_toks.append(k.dma("sp", dbg_out["d_score"], score, reads=["score"]))
                out_toks.append(k.dma("sp", dbg_out["d_q"], qT, reads=["qT"]))
                out_toks.append(k.dma("sp", dbg_out["d_k"], KT, reads=["KT"]))
            cur, ckey = score, "score"
            for r in range(32):
                k.op("dve", lambda e, cur=cur, S=S: e.max(out=m8, in_=cur[:, 0:S]), [ckey], ["m8"])
                if r < 31:
                    k.op("dve", lambda e, cur=cur, S=S: e.match_replace(out=work[:, 0:S], in_to_replace=m8, in_values=cur[:, 0:S], imm_value=NEG),
                         [ckey, "m8", "wm"], ["wm"])
                    cur, ckey = work, "wm"
            k.ts("dve", thr, m8[:, 7:8], -1.0e29, ALU.max, ["m8"], ["thr"])
            k.ts("dve", maskb[:, 0:S], score[:, 0:S], thr, ALU.is_ge, ["score", "thr", "wm"], ["wm"])
            if dbg and t == NT_CTX:
                out_toks.append(k.dma("sp", dbg_out["d_mask"], maskb, reads=["wm"]))
                out_toks.append(k.dma("sp", dbg_out["d_thr"], m8, reads=["m8"]))
            for kt0 in range(0, nk, 8):
                nb = min(8, nk - kt0)
                pTm = k.bank(4, BF16)
                for i2 in range(nb):
                    k.tr(pTm[:, i2 * 128:(i2 + 1) * 128], maskb[:, (kt0 + i2) * 128:(kt0 + i2 + 1) * 128], identb, ["wm", "identb"], ["b4"])
                k.cp("act", maskT[:, kt0:kt0 + nb, :], pTm[:, 0:nb * 128].rearrange("p (a b) -> p a b", b=128), ["b4", "wm"], ["wm"])
            for kvg in range(4):
                po = k.psum[1][:, 1024:2048].rearrange("p (a b) -> p a b", b=256)

                def sc_stage(kt, kvg=kvg):
                    bk = kt % 2
                    Et, Em, etk, emk = Et_[bk], Em_[bk], "Et%d" % bk, "Em%d" % bk
                    ps = k.bank(bk)
                    k.mm(ps, KT[:, kvg, kt * 128:(kt + 1) * 128], qT[:, kvg * 4:(kvg + 1) * 4, :], True, True, ["KT", "qT"], ["b%d" % bk])
                    k.act(Et, ps.rearrange("p (a b) -> p a b", b=128), AF.Exp, ["b%d" % bk], [etk], scale=1.0 / math.sqrt(128.0))
                    k.tt("dve", Em, Et, bc(maskT[:, kt, :], 1, [128, 4, 128]), ALU.mult, [etk, "wm"], [emk])

                def pv_stage(kt, kvg=kvg, po=po):
                    bk = kt % 2
                    Em, emk = Em_[bk], "Em%d" % bk
                    for hh in range(4):
                        k.mm(po[:, hh, 0:129], Em[:, hh, :], V[:, kt, kvg, 0:129], kt == 0 and hh % 2 == 0, kt == nk - 1, [emk, "V"], ["b%d" % (6 + hh // 2)])

                sc_stage(0)
                for kt in range(nk):
                    if kt + 1 < nk:
                        sc_stage(kt + 1)
                    pv_stage(kt)
                k.op("dve", lambda e, po=po: e.reciprocal(rs, po[:, :, 128]), ["b6", "b7"], ["rs"])
                for hh in range(4):
                    k.act(ao[:, kvg * 4 + hh, :], po[:, hh, 0:128], AF.Copy, ["b%d" % (6 + hh // 2), "rs"], ["ao"], scale=rs[:, hh:hh + 1])
            pTa = k.psum[1][:, 0:1024].bitcast(BF16)
            for h in range(16):
                k.tr(pTa[:, h * 128:(h + 1) * 128], ao[:, h, :], identb, ["ao", "identb"], ["b4", "b5"])
            k.cp("act", aT, pTa.rearrange("p (a b) -> p a b", b=128), ["b4", "b5"], ["aT"])
            k.dma("sp", st_attn[:, :, ot * 128:(ot + 1) * 128], aT, reads=["aT"], writes=["st_attn"])
        if dbg:
            for ot in range(NT_OWN):
                k.dma("sp", aT, st_attn[:, :, ot * 128:(ot + 1) * 128], reads=["st_attn"], writes=["aT"])
                out_toks.append(k.dma("sp", dbg_out["d_attn"][:, :, ot * 128:(ot + 1) * 128], aT, reads=["aT"]))
    k.aoff = mark0

    h2 = k.alloc([128, 4, D], F32)
    mark4 = k.aoff
    for gi in (7, 8):
        tiles = groups[gi]
        otiles = [t - NT_CTX for t in tiles]
        o0 = otiles[0] * 128
        if 4 in phases:
            k.barrier()
            k.aoff = mark4
            k.dma("sp", nw_fm, nmix, writes=["nw"])
            wb2 = k.alloc([128, 16 * 512], BF16)
            actT = k.alloc([128, 32, 512], BF16)
            mg = k.alloc([128, 16, 512], BF16)
            sga = k.alloc([128, 512], F32)
            t1 = k.alloc([128, 512], F32)
            make_hnT([xin[t * 128:(t + 1) * 128, :] for t in tiles])
            k.dma("sp", actT[:, 0:16, :], st_attn[:, :, o0:o0 + 512], reads=["st_attn"], writes=["actT"])
            for c4 in range(4):
                wg, gkey = load_w(w_in, D, O_G + c4 * 512, 512, buf=0)
                wv, wkey = load_w(w_pa, 2048, c4 * 512, 512, buf=1)
                for cc in range(4):
                    pg = proj_fm(wg, gkey, cc, 512, 0)
                    pa = proj_fm(wv, wkey, cc, 512, 1, rhs=actT, rkey="actT")
                    k.act(sga, pg, AF.Sigmoid, ["b0"], ["sga"])
                    k.tt("dve", mg[:, c4 * 4 + cc, :], sga, pa, ALU.mult, ["sga", "b1"], ["mg"])
            k.dma("sp", actT, st_ssm[:, :, o0:o0 + 512], reads=["st_ssm", "actT"], writes=["actT"])
            for c4 in range(4):
                wg = wb2[:, :].rearrange("p (a b) -> p a b", b=512)
                k.dma("pool", wg, w_in[:, O_G + 2048 + c4 * 512:O_G + 2048 + (c4 + 1) * 512].rearrange("(a p) c -> p a c", p=128), writes=["wb2"])
                for c2 in range(2):
                    wv, wkey = load_w(w_ps, 4096, c4 * 512 + c2 * 256, 256, buf=c2)
                    for cc in range(2):
                        col = c4 * 4 + c2 * 2 + cc
                        pg = proj_fm(wg, "wb2", c2 * 2 + cc, 512, 0)
                        pa = proj_fm(wv, wkey, cc, 512, 1, nk=32, rhs=actT, rkey="actT")
                        k.act(sga, pg, AF.Sigmoid, ["b0"], ["sga"])
                        k.tt("dve", t1, sga, pa, ALU.mult, ["sga", "b1"], ["t1"])
                        k.tt("dve", mg[:, col, :], mg[:, col, :], t1, ALU.add, ["mg", "t1"], ["mg"])
            for c4 in range(4):
                wv, wkey = load_w(w_o, 2048, c4 * 512, 512, buf=c4 % 2)
                for j, t in enumerate(tiles):
                    bk = 2 + j % 2
                    ps = k.bank(bk)
                    for kt in range(16):
                        k.mm(ps, mg[:, kt, j * 128:(j + 1) * 128], wv[:, kt, :], kt == 0, kt == 15, ["mg", wkey], ["b%d" % bk])
                    if c4 == 0:
                        k.dma("sp", h2[:, j, :], xin[t * 128:(t + 1) * 128, :], writes=["h2_%d" % j])
                    k.tt("dve", h2[:, j, c4 * 512:(c4 + 1) * 512], h2[:, j, c4 * 512:(c4 + 1) * 512], ps, ALU.add, ["h2_%d" % j, "b%d" % bk], ["h2_%d" % j])
            if dbg:
                for j, ot in enumerate(otiles):
                    out_toks.append(k.dma("sp", dbg_out["d_h2"][:, ot, :], h2[:, j, :], reads=["h2_%d" % j]))
        if 5 in phases:
            k.barrier()
            k.aoff = mark4
            k.dma("sp", nw_fm, nffn, writes=["nw"])
            nfin_r = xt
            k.dma("sp", nfin_r, nfin.partition_broadcast(128), writes=["xt"])
            keysb = k.alloc([128, 2, 128], BF16)
            k.dma("pool", keysb, keysT, writes=["keysb"])
            q_off = k.aoff
            qpT = k.alloc([128, 16, 512], BF16)
            osb = k.view(q_off, [128, D], F32)
            ssc = k.alloc([128, 4, 16, 128], F32)
            top = k.alloc([128, 16, 16], F32)
            wk2 = k.alloc([128, 128], F32)
            cand = k.alloc([128, 256], F32)
            cand2 = k.alloc([128, 256], F32)
            c8 = k.alloc([128, 8], F32)
            thr5 = k.alloc([128, 4, 8], F32)
            nb5 = k.alloc([128, 4, 8], F32)
            zz = k.alloc([128, 4], F32)
            Gt_ = [k.alloc([128, 512], BF16) for _ in range(2)]
            GT_ = [k.alloc([128, 4, 128], BF16) for _ in range(2)]
            cand3 = k.alloc([128, 256], F32)
            c8b = k.alloc([128, 8], F32)
            tau5 = k.alloc([128, 4, 8], F32)
            tsum = k.alloc([128, 8], F32)
            Ex_ = [k.alloc([128, 512], F32) for _ in range(3)]
            Wh_ = [k.alloc([128, 512], BF16) for _ in range(3)]
            GWT_ = [k.alloc([128, 4, 128], BF16) for _ in range(2)]
            wb5 = [wb[0], wb[1], k.alloc([128, 16 * 512], BF16), k.alloc([128, 16 * 512], BF16)]
            make_hnT([h2[:, j, :] for j in range(4)], keys=["h2_%d" % j for j in range(4)])
            for c4 in range(4):
                wv, wkey = load_w(w_pq, 2048, c4 * 512, 512, buf=c4 % 2)
                for cc in range(4):
                    pa = proj_fm(wv, wkey, cc, 512, 0)
                    k.cp("act", qpT[:, c4 * 4 + cc, :], pa, ["b0"], ["qpT"])
            for j in range(4):
                js = slice(j * 128, (j + 1) * 128)
                for q4 in range(4):
                    bk = 2 + q4 % 2
                    ps = k.bank(bk)
                    for i2 in range(4):
                        hc = q4 * 4 + i2
                        k.mm(ps[:, i2 * 128:(i2 + 1) * 128], qpT[:, hc, js], keysb[:, hc % 2, :], True, True, ["qpT", "keysb"], ["b%d" % bk])
                    k.cp("act", ssc[:, j, q4 * 4:(q4 + 1) * 4, :], ps.rearrange("p (a b) -> p a b", b=128), ["b%d" % bk], ["ssc"])
                for hc in range(16):
                    k.op("dve", lambda e, hc=hc, j=j: e.max(out=top[:, hc, 0:8], in_=ssc[:, j, hc, :]), ["ssc"], ["top"])
                    k.op("dve", lambda e, hc=hc, j=j: e.match_replace(out=wk2, in_to_replace=top[:, hc, 0:8], in_values=ssc[:, j, hc, :], imm_value=NEG), ["ssc", "top", "wk2"], ["wk2"])
                    k.op("dve", lambda e, hc=hc: e.max(out=top[:, hc, 8:16], in_=wk2), ["wk2", "top"], ["top"])
                for h in range(8):
                    c3 = cand.rearrange("p (a b) -> p a b", b=16)
                    k.tt("dve", c3, bc(top[:, 2 * h, :], 2, [128, 16, 16]), bc(top[:, 2 * h + 1, :], 1, [128, 16, 16]), ALU.add, ["top"], ["cand"])
                    k.op("dve", lambda e: e.max(out=c8, in_=cand), ["cand"], ["c8"])
                    k.ts("dve", zz[:, 0:1], c8[:, 0:1], -1.0, ALU.mult, ["c8"], ["zz0"])
                    k.op("dve", lambda e: e.match_replace(out=cand2, in_to_replace=c8, in_values=cand, imm_value=NEG), ["cand", "c8", "cand2"], ["cand2"])
                    k.op("dve", lambda e: e.max(out=c8, in_=cand2), ["cand2", "c8"], ["c8"])
                    k.cp("dve", thr5[:, j, h:h + 1], c8[:, 7:8], ["c8"], ["thr5"])
                    k.op("dve", lambda e: e.match_replace(out=cand3, in_to_replace=c8, in_values=cand2, imm_value=NEG), ["cand2", "c8", "cand3"], ["cand3"])
                    k.op("dve", lambda e: e.max(out=c8b, in_=cand3), ["cand3", "c8b"], ["c8b"])
                    k.tt("dve", tsum[:, h:h + 1], c8[:, 7:8], c8b[:, 0:1], ALU.add, ["c8", "c8b"], ["tsum"])
                    k.act(cand3, cand, AF.Exp, ["cand", "zz0", "cand3"], ["cand3"], bias=zz[:, 0:1])
                    k.stt("dve", cand3, cand, thr5[:, j, h:h + 1], cand3, ALU.is_ge, ALU.mult, ["cand", "thr5", "cand3"], ["cand3"])
                    k.op("dve", lambda e: e.reduce_sum(out=zz[:, 1:2], in_=cand3, axis=mybir.AxisListType.X), ["cand3"], ["zz1"])
                    k.act(zz[:, 2:3], zz[:, 1:2], AF.Ln, ["zz1"], ["zz2"])
                    k.tt("dve", nb5[:, j, h:h + 1], zz[:, 0:1], zz[:, 2:3], ALU.subtract, ["zz0", "zz2"], ["nb5"])
                k.stt("dve", tau5[:, j, :], tsum, 0.5, nb5[:, j, :], ALU.mult, ALU.add, ["tsum", "nb5"], ["tau5"])
                k.act(tau5[:, j, :], tau5[:, j, :], AF.Exp, ["tau5"], ["tau5"])
                for h in range(8):
                    k.act(ssc[:, j, 2 * h, :], ssc[:, j, 2 * h, :], AF.Exp, ["ssc", "nb5"], ["ssc"], bias=nb5[:, j, h:h + 1])
                    k.act(ssc[:, j, 2 * h + 1, :], ssc[:, j, 2 * h + 1, :], AF.Exp, ["ssc"], ["ssc"])
            iters = [(ec, j) for ec in range(NEC) for j in range(4)]

            def bufs(ec):
                uv = wb5[(ec % 2) * 2][:, :].rearrange("p (a b) -> p a b", b=512)
                vv = wb5[(ec % 2) * 2 + 1][:, :].rearrange("p (a b) -> p a b", b=2048)
                ukey = "wb0" if ec % 2 == 0 else "wu1"
                vkey = "wb1" if ec % 2 == 0 else "wv1"
                return uv, vv, ukey, vkey

            def load5(ec):
                uv, vv, ukey, vkey = bufs(ec)
                k.dma("pool", uv, uT[:, ec * 512:(ec + 1) * 512].rearrange("(a p) c -> p a c", p=128), writes=[ukey])
                k.dma("pool", vv, pv[ec * 512:(ec + 1) * 512, :].rearrange("(a p) c -> p a c", p=128), writes=[vkey])

            def stageA(idx):
                ec, j = iters[idx]
                uv, vv, ukey, vkey = bufs(ec)
                js = slice(j * 128, (j + 1) * 128)
                jb = idx % 2
                Gt, GT, GWT = Gt_[jb], GT_[jb], GWT_[jb]
                gk, gtk, gwtk = "Gt%d" % jb, "GT%d" % jb, "GWT%d" % jb
                ps = k.bank(jb)
                for kt in range(16):
                    k.mm(ps, hnT[:, kt, js], uv[:, kt, :], kt == 0, kt == 15, ["hnT", ukey], ["b%d" % jb])
                k.act(Gt, ps, AF.Gelu, ["b%d" % jb], [gk])
                if idx >= 1:
                    stageB_pe(idx - 1)
                pTg = k.bank(jb, BF16)
                for b4 in range(4):
                    k.tr(pTg[:, b4 * 128:(b4 + 1) * 128], Gt[:, b4 * 128:(b4 + 1) * 128], identb, [gk, "identb"], ["b%d" % jb])
                k.cp("act", GT, pTg[:, 0:512].rearrange("p (a b) -> p a b", b=128), ["b%d" % jb], [gtk])
                pW = k.psum[0][:, 1024:2048].rearrange("p (a b) -> p a b", b=256)[:, :, 0:128]
                for h in range(8):
                    hb = (idx * 8 + h) % 3
                    Ex, Wh = Ex_[hb], Wh_[hb]
                    ek, whk = "Ex%d" % hb, "Wh%d" % hb
                    eng = "dve" if h % 4 == 3 else "pool"
                    k.tt(eng, Ex.rearrange("p (a b) -> p a b", b=128), bc(ssc[:, j, 2 * h, ec * 4:(ec + 1) * 4], 2, [128, 4, 128]),
                         bc(ssc[:, j, 2 * h + 1, :], 1, [128, 4, 128]), ALU.mult, ["ssc"], [ek])
                    k.stt("dve", Wh, Ex, tau5[:, j, h:h + 1], Ex, ALU.is_ge, ALU.mult, ["tau5", ek], [whk])
                    for b4 in range(4):
                        k.mm(pW[:, b4, :], Wh[:, b4 * 128:(b4 + 1) * 128], identb, h == 0 and b4 % 2 == 0, h == 7, [whk, "identb"], ["b%d" % (2 + b4 // 2)])
                k.tt("dve", GWT, pW, GT, ALU.mult, ["b2", "b3", gtk], [gwtk])
                if idx >= 1:
                    stageB_dve(idx - 1)

            def stageB_pe(idx):
                ec, j = iters[idx]
                uv, vv, ukey, vkey = bufs(ec)
                jb = idx % 2
                GWT, gwtk = GWT_[jb], "GWT%d" % jb
                po = k.psum[1][:, 0:2048]
                for b4 in range(4):
                    for dc in range(4):
                        k.mm(po[:, dc * 512:(dc + 1) * 512], GWT[:, b4, :], vv[:, b4, dc * 512:(dc + 1) * 512], b4 == 0, b4 == 3, [gwtk, vkey], ["b%d" % (4 + dc)])

            def stageB_dve(idx):
                ec, j = iters[idx]
                po = k.psum[1][:, 0:2048]
                k.tt("dve", h2[:, j, :], h2[:, j, :], po, ALU.add, ["h2_%d" % j, "b4", "b5", "b6", "b7"], ["h2_%d" % j])

            load5(0)
            for idx in range(len(iters)):
                ec, j = iters[idx]
                stageA(idx)
                if j == 1 and ec + 1 < NEC:
                    load5(ec + 1)
            stageB_pe(len(iters) - 1)
            stageB_dve(len(iters) - 1)
            k.barrier()
            for j, ot in enumerate(otiles):
                k.memset("pool", sq[:, 0:1], 0.0, ["sq0"])
                k.act(osb, h2[:, j, :], AF.Square, ["h2_%d" % j, "sq0"], ["osb", "sq0"], accum_out=sq[:, 0:1])
                k.ts("dve", sq[:, 1:2], sq[:, 0:1], 1.0 / D, ALU.mult, ["sq0"], ["sq1"], s2=EPS, op1=ALU.add)
                k.act(sq[:, 2:3], sq[:, 1:2], AF.Sqrt, ["sq1"], ["sq2"])
                k.op("dve", lambda e: e.reciprocal(sq[:, 3:4], sq[:, 2:3]), ["sq2"], ["sq3"])
                k.stt("dve", osb, h2[:, j, :], sq[:, 3:4], nfin_r, ALU.mult, ALU.mult, ["h2_%d" % j, "sq3", "xt", "osb"], ["osb"])
                out_toks.append(k.dma("sp", yout[ot * 128:(ot + 1) * 128, :], osb, reads=["osb"]))


def _rot_cols(w, head_dim):
    half = head_dim // 2
    n = w.shape[1]
    idx = np.arange(n)
    r = (idx // head_dim) * head_dim + (idx % head_dim + half) % head_dim
    return w[:, r]


def _tables(pos):
    res = []
    for hd in (128, 64):
        half = hd // 2
        p = np.arange(128) % hd
        inv = (10000.0 ** (-(np.arange(half, dtype=np.float32)) / np.float32(half))).astype(np.float32)
        ang = pos.astype(np.float32)[None, :] * inv[p % half][:, None]
        c = np.cos(ang).astype(np.float32)
        s = np.sin(ang).astype(np.float32)
        sgn = np.where(p < half, -1.0, 1.0).astype(np.float32)[:, None]
        res += [c, s * sgn]
    return np.stack(res, 0).astype(np.float32)


_NC_CACHE = {}


def kernel(x, meta_tokens, norm_mix_w, w_in, conv_w, conv_b, dt_bias, a_log, d_skip, ssm_norm_w,
           w_branch_attn, w_branch_ssm, w_out, norm_ffn_w, peer_w_query, peer_sub_keys, peer_u, peer_v,
           norm_final_w, _phases=(1, 2, 3, 4, 5), _dbg=False, _gsel=None, _ncores=8, _trace=False):
    f = lambda a: np.ascontiguousarray(np.asarray(a, dtype=np.float32))
    x = f(x)
    meta = f(meta_tokens)
    w_in0 = f(w_in)[0]
    w_rot = np.concatenate([
        _rot_cols(w_in0[:, O_Q:O_Q + 2048], 128), _rot_cols(w_in0[:, O_K:O_K + 512], 128),
        _rot_cols(w_in0[:, O_QI:O_QI + 1024], 64), _rot_cols(w_in0[:, O_KI:O_KI + 64], 64)], axis=1)
    w_rot = np.ascontiguousarray(w_rot)
    cst = np.zeros((128, 5, 128), np.float32)
    ii = np.arange(128)
    cst[:, 0, :] = np.eye(128)
    cst[:, 1, :] = (ii[:, None] <= ii[None, :])
    cst[:, 2, :] = np.where(ii[:, None] > ii[None, :], -30000.0, 0.0)
    cst[:, 3, :] = np.where(ii[None, :] <= ii[:, None], 0.0, NEG)
    cst[:, 4, :] = 1.0
    common = {
        "cst": cst, "w_in": w_in0, "w_rot": w_rot,
        "convw": np.ascontiguousarray(f(conv_w)[0].T.reshape(48, 128, 4).transpose(1, 0, 2)),
        "convb": np.ascontiguousarray(f(conv_b)[0].reshape(48, 128).T),
        "dtb": f(dt_bias)[0], "alog": f(a_log)[0], "dsk": f(d_skip)[0],
        "snw": np.ascontiguousarray(f(ssm_norm_w)[0].reshape(32, 128).T),
        "nmix": np.ascontiguousarray(f(norm_mix_w)[0].reshape(16, 128).T),
        "nffn": np.ascontiguousarray(f(norm_ffn_w)[0].reshape(16, 128).T),
        "nfin": f(norm_final_w),
        "w_pa": f(w_branch_attn)[0], "w_ps": f(w_branch_ssm)[0], "w_o": f(w_out)[0], "w_pq": f(peer_w_query)[0],
        "keysT": np.ascontiguousarray(f(peer_sub_keys)[0].transpose(2, 0, 1)),
        "uT": np.ascontiguousarray(f(peer_u)[0].T), "pv": f(peer_v)[0],
    }
    in_maps = []
    for core in range(8):
        b, c = core // 4, core % 4
        own_start = 16 + 1024 * c
        pos = own_start - CTX + np.arange(NSLOT)
        xin = np.zeros((NSLOT, D), np.float32)
        seq = np.concatenate([meta, x[b]], axis=0)
        ok = (pos >= 0)
        xin[ok] = seq[pos[ok]]
        vmask = ok.astype(np.float32)
        m = dict(common)
        m["xin"] = xin
        m["tabs"] = _tables(pos)
        m["valid"] = np.ascontiguousarray(vmask.reshape(NT, 128).T)
        m["padb"] = np.where(ok[:CTX], 0.0, NEG).astype(np.float32)
        in_maps.append(m)
    key = (tuple(_phases), _dbg, _gsel)
    if key not in _NC_CACHE:
        _NC_CACHE[key] = build_program(_phases, _dbg, _gsel)
    nc, used = _NC_CACHE[key]
    in_maps = [{n: m[n] for n in used} for m in in_maps][:_ncores]
    res = run_bass_kernel_spmd(nc, in_maps, core_ids=list(range(_ncores)), **({'trace': True} if _trace else {}))
    out = np.zeros((2, 4096, D), np.float32)
    for core in range(_ncores):
        b, c = core // 4, core % 4
        out[b, c * 1024:(c + 1) * 1024] = res.results[core]["y"]
    if _dbg:
        return out, res
    return out
```

```python
import math
import numpy as np
from contextlib import ExitStack
import concourse.bass as bass
import concourse.mybir as mybir
from concourse.bass_utils import run_bass_kernel_spmd

F32 = mybir.dt.float32
BF16 = mybir.dt.bfloat16
U8 = mybir.dt.uint8
AF = mybir.ActivationFunctionType
ALU = mybir.AluOpType

N_DMA_SEMS = 24
D = 2048
NT_CTX = 25
NT_OWN = 8
NT = NT_CTX + NT_OWN
NSLOT = NT * 128
CTX = NT_CTX * 128
EPS = 1e-6
O_Q, O_K, O_V, O_QI, O_KI, O_WI, O_Z, O_XS, O_B, O_C, O_DT, O_G = 0, 2048, 2560, 3072, 4096, 4160, 4176, 8272, 12368, 13392, 14416, 14480
R_Q, R_K, R_QI, R_KI = 0, 2048, 2560, 3584
NEG = -1.0e30
NEC = 32


class KB:
    ENGS = ("pe", "act", "dve", "pool", "sp")

    def __init__(self, nc):
        self.nc = nc
        self.es = ExitStack()
        self.ops = {e: [] for e in self.ENGS}
        self.sem = {e: self.es.enter_context(nc.semaphore("s_" + e)) for e in self.ENGS}
        self.cnt = {e: 0 for e in self.ENGS}
        self.dsem = [self.es.enter_context(nc.semaphore("s_dma%d" % i)) for i in range(N_DMA_SEMS)]
        self.dcnt = [0] * N_DMA_SEMS
        self.dnext = 0
        self.seen = {e: {} for e in self.ENGS}
        self.res = {}
        self.semobj = {}
        for e in self.ENGS:
            self.semobj[("e", e)] = self.sem[e]
        for i in range(N_DMA_SEMS):
            self.semobj[("d", i)] = self.dsem[i]
        self.arena = self.es.enter_context(nc.sbuf_tensor("arena", [128, 204 * 1024], U8))
        self.aoff = 0
        self.psum = [self.es.enter_context(nc.psum_tensor("psb%d" % i, [128, 2048], F32)) for i in range(2)]

    def alloc(self, shape, dt):
        esz = 4 if dt == F32 else 2
        n = int(np.prod(shape[1:]))
        nb = (n * esz + 63) // 64 * 64
        o = self.aoff
        self.aoff += nb
        assert self.aoff <= 204 * 1024, "SBUF arena overflow %d" % self.aoff
        v = self.arena[:, o:o + n * esz].bitcast(dt)
        if len(shape) == 3:
            v = v.rearrange("p (a b) -> p a b", b=shape[2])
        elif len(shape) == 4:
            v = v.rearrange("p (a b c) -> p a b c", b=shape[2], c=shape[3])
        if shape[0] < 128:
            v = v[0:shape[0]]
        return v

    def view(self, off, shape, dt):
        save = self.aoff
        self.aoff = off
        v = self.alloc(shape, dt)
        self.aoff = save
        return v

    def barrier(self):
        toks = [(("e", e), self.cnt[e]) for e in self.ENGS if self.cnt[e] > 0]
        toks += [(("d", i), self.dcnt[i]) for i in range(N_DMA_SEMS) if self.dcnt[i] > 0]
        for e in self.ENGS:
            w = []
            for kk, v in toks:
                if kk == ("e", e) or self.seen[e].get(kk, 0) >= v:
                    continue
                self.seen[e][kk] = v
                w.append((kk, v))
            if w:
                self.ops[e].append((w, None, None))

    def bank(self, i, dt=F32):
        t = self.psum[i // 4]
        v = t[:, (i % 4) * 512:(i % 4 + 1) * 512]
        if dt == BF16:
            v = v.bitcast(BF16)
        return v

    def _deps(self, eng, reads, writes):
        need = {}

        def add(tok):
            if tok is None:
                return
            k, v = tok
            if need.get(k, 0) < v:
                need[k] = v

        for r in reads:
            st = self.res.get(r)
            if st is not None:
                add(st[0])
        for w in writes:
            st = self.res.get(w)
            if st is not None:
                add(st[0])
                for k, v in st[1].items():
                    add((k, v))
        waits = []
        for k, v in need.items():
            if k == ("e", "pe") and eng == "pe":
                continue
            if self.seen[eng].get(k, 0) >= v:
                continue
            self.seen[eng][k] = v
            waits.append((k, v))
        return waits

    def _commit(self, tok, reads, writes):
        k, v = tok
        for r in reads:
            st = self.res.setdefault(r, [None, {}])
            if st[1].get(k, 0) < v:
                st[1][k] = v
        for w in writes:
            self.res[w] = [tok, {}]

    @staticmethod
    def _excl(reads, writes):
        ps = [r for r in reads if len(r) == 2 and r[0] == "b" and r[1].isdigit()]
        return (reads, list(writes) + ps) if ps else (reads, writes)

    def op(self, eng, fn, reads=(), writes=()):
        reads, writes = self._excl(reads, writes)
        waits = self._deps(eng, reads, writes)
        self.cnt[eng] += 1
        tok = (("e", eng), self.cnt[eng])
        self.ops[eng].append((waits, fn, (("e", eng), 1)))
        self._commit(tok, reads, writes)
        return tok

    def dma(self, eng, out, in_, reads=(), writes=()):
        i = self.dnext
        self.dnext = (self.dnext + 1) % N_DMA_SEMS
        waits = self._deps(eng, reads, writes)
        prev = self.dcnt[i]
        k = ("d", i)
        if prev > 0 and self.seen[eng].get(k, 0) < prev:
            self.seen[eng][k] = prev
            waits.append((k, prev))
        self.dcnt[i] += 16
        tok = (k, self.dcnt[i])
        self.ops[eng].append((waits, lambda e: e.dma_start(out=out, in_=in_), (k, 16)))
        self._commit(tok, reads, writes)
        return tok

    def wait_all(self, eng, toks):
        self.ops[eng].append((list(toks), None, None))

    def mm(self, out, lhsT, rhs, start, stop, reads, writes):
        return self.op("pe", lambda e: e.matmul(out, lhsT, rhs, start=start, stop=stop), reads, writes)

    def tr(self, out, in_, ident, reads, writes):
        return self.op("pe", lambda e: e.transpose(out, in_, ident), reads, writes)

    def act(self, out, in_, func, reads, writes, bias=None, scale=None, accum_out=None):
        kw = {}
        if bias is not None:
            kw["bias"] = bias
        if scale is not None:
            kw["scale"] = scale
        if accum_out is not None:
            kw["accum_out"] = accum_out
        return self.op("act", lambda e: e.activation(out, in_, func, **kw), reads, writes)

    def tt(self, eng, out, in0, in1, op, reads, writes):
        return self.op(eng, lambda e: e.tensor_tensor(out=out, in0=in0, in1=in1, op=op), reads, writes)

    def ts(self, eng, out, in0, s1, op0, reads, writes, s2=None, op1=None, accum_out=None):
        kw = {}
        if op1 is not None:
            kw["op1"] = op1
        if accum_out is not None:
            kw["accum_out"] = accum_out
        return self.op(eng, lambda e: e.tensor_scalar(out=out, in0=in0, scalar1=s1, scalar2=s2, op0=op0, **kw), reads, writes)

    def stt(self, eng, out, in0, scalar, in1, op0, op1, reads, writes, accum_out=None):
        kw = {}
        if accum_out is not None:
            kw["accum_out"] = accum_out
        return self.op(eng, lambda e: e.scalar_tensor_tensor(out=out, in0=in0, scalar=scalar, in1=in1, op0=op0, op1=op1, **kw), reads, writes)

    def cp(self, eng, out, in_, reads, writes):
        if eng == "act":
            return self.op("act", lambda e: e.copy(out, in_), reads, writes)
        return self.op(eng, lambda e: e.tensor_copy(out, in_), reads, writes)

    def memset(self, eng, ap, val, writes):
        return self.op(eng, lambda e: e.memset(ap, val), (), writes)

    def emit(self):
        nc = self.nc
        engmap = {"pe": "tensor", "act": "scalar", "dve": "vector", "pool": "gpsimd", "sp": "sync"}
        with nc.Block() as block:
            for en in self.ENGS:
                ops = self.ops[en]

                def body(engobj, ops=ops):
                    for waits, fn, inc in ops:
                        for k, v in waits:
                            engobj.wait_ge(self.semobj[k], v)
                        if fn is not None:
                            ins = fn(engobj)
                            ins.then_inc(self.semobj[inc[0]], inc[1])

                getattr(block, engmap[en])(body)
        self.es.close()


DBG_STOP = [None]


class _Stop(Exception):
    pass


def ck(n):
    if DBG_STOP[0] == n:
        raise _Stop()


def bc(ap, axis, shape):
    return ap.unsqueeze(axis).to_broadcast(list(shape))


def build_program(phases=(1, 2, 3, 4, 5), dbg=False, gsel=None):
    nc = bass.Bass("TRN2", target_bir_lowering=False)
    st = {}
    try:
        _build_body(nc, phases, dbg, gsel, st)
    except _Stop:
        pass
    k = st["k"]
    k.wait_all("sp", st["out_toks"])
    k.emit()
    return nc, st["used"]


def _build_body(nc, phases, dbg, gsel, st):
    used_inputs = []
    NEEDS = {"xin": (1, 2, 3, 4), "tabs": (2, 3), "valid": (1,), "padb": (3,), "cst": (1, 2, 3, 4, 5), "w_in": (1, 2, 3, 4),
             "w_rot": (2, 3), "convw": (1,), "convb": (1,), "dtb": (1,), "alog": (1,), "dsk": (1,), "snw": (1,),
             "nmix": (1, 2, 3, 4), "nffn": (5,), "nfin": (5,), "w_pa": (4,), "w_ps": (4,), "w_o": (4,), "w_pq": (5,),
             "keysT": (5,), "uT": (5,), "pv": (5,)}

    def din(name, shape, dt=F32):
        if not any(p in phases for p in NEEDS[name]):
            return None
        used_inputs.append(name)
        return nc.dram_tensor(name, list(shape), dt, kind="ExternalInput").ap()

    xin = din("xin", [NSLOT, D])
    tabs = din("tabs", [4, 128, NSLOT])
    valid = din("valid", [128, NT])
    padb = din("padb", [CTX])
    cst = din("cst", [128, 5, 128])
    w_in = din("w_in", [D, 18576])
    w_rot = din("w_rot", [D, 3648])
    convw = din("convw", [128, 48, 4])
    convb = din("convb", [128, 48])
    dtb = din("dtb", [64])
    alog = din("alog", [64])
    dsk = din("dsk", [64])
    snw = din("snw", [128, 32])
    nmix = din("nmix", [128, 16])
    nffn = din("nffn", [128, 16])
    nfin = din("nfin", [D])
    w_pa = din("w_pa", [2048, 2048])
    w_ps = din("w_ps", [4096, 2048])
    w_o = din("w_o", [2048, 2048])
    w_pq = din("w_pq", [2048, 2048])
    keysT = din("keysT", [128, 2, 128])
    uT = din("uT", [2048, 16384])
    pv = din("pv", [16384, 2048])
    yout = nc.dram_tensor("y", [NT_OWN * 128, D], F32, kind="ExternalOutput").ap()
    st_attn = nc.dram_tensor("st_attn", [128, 16, NT_OWN * 128], BF16).ap()
    st_ssm = nc.dram_tensor("st_ssm", [128, 32, NT_OWN * 128], BF16).ap()
    dbg_out = {}
    if dbg:
        dbg_out["d_ssm"] = nc.dram_tensor("d_ssm", [128, 32, NT_OWN * 128], BF16, kind="ExternalOutput").ap()
        dbg_out["d_attn"] = nc.dram_tensor("d_attn", [128, 16, NT_OWN * 128], BF16, kind="ExternalOutput").ap()
        dbg_out["d_h2"] = nc.dram_tensor("d_h2", [128, NT_OWN, D], F32, kind="ExternalOutput").ap()
        dbg_out["d_score"] = nc.dram_tensor("d_score", [128, NSLOT], F32, kind="ExternalOutput").ap()
        dbg_out["d_mask"] = nc.dram_tensor("d_mask", [128, NSLOT], BF16, kind="ExternalOutput").ap()
        dbg_out["d_thr"] = nc.dram_tensor("d_thr", [128, 8], F32, kind="ExternalOutput").ap()
        dbg_out["d_po"] = nc.dram_tensor("d_po", [128, 1024], F32, kind="ExternalOutput").ap()
        dbg_out["d_q"] = nc.dram_tensor("d_q", [128, 16, 128], BF16, kind="ExternalOutput").ap()
        dbg_out["d_k"] = nc.dram_tensor("d_k", [128, 4, NSLOT], BF16, kind="ExternalOutput").ap()

    k = KB(nc)
    out_toks = []
    st["k"] = k
    st["out_toks"] = out_toks
    st["used"] = used_inputs

    ck(-1)
    cstf = k.alloc([128, 5, 128], F32)
    k.dma("sp", cstf, cst, writes=["cst"])
    identf, tri_le, negtri, causb, onesf = (cstf[:, i, :] for i in range(5))
    identb = k.alloc([128, 128], BF16)
    k.dma("pool", identb, cst[:, 0, :], writes=["identb"])
    ck(-2)
    nw_fm = k.alloc([128, 16], F32)
    xt = k.alloc([128, D], F32)
    hn = k.alloc([128, D], BF16)
    sq = k.alloc([128, 4], F32)
    hnT_off = k.aoff
    hnT = k.alloc([128, 16, 512], BF16)
    wb = [k.alloc([128, 16 * 512], BF16) for _ in range(2)]
    wsel = [0]

    def load_w(dram, K, c0, ncols, buf=None):
        if buf is None:
            i = wsel[0]
            wsel[0] ^= 1
        else:
            i = buf
        kt = K // 128
        v = wb[i][:, 0:kt * ncols].rearrange("p (a b) -> p a b", b=ncols)
        k.dma("pool", v, dram[:, c0:c0 + ncols].rearrange("(a p) c -> p a c", p=128), writes=["wb%d" % i])
        return v, "wb%d" % i

    def make_hnT(src_tiles, keys=None, dst=None, dkey="hnT"):
        dst = hnT if dst is None else dst
        for j, src in enumerate(src_tiles):
            if keys is None:
                k.dma("sp", xt, src, writes=["xt"])
                xsrc, xkey = xt, "xt"
            else:
                xsrc, xkey = src, keys[j]
            k.memset("pool", sq[:, 0:1], 0.0, ["sq0"])
            k.act(hn, xsrc, AF.Square, [xkey, "sq0"], ["hn", "sq0"], accum_out=sq[:, 0:1])
            k.ts("dve", sq[:, 1:2], sq[:, 0:1], 1.0 / D, ALU.mult, ["sq0"], ["sq1"], s2=EPS, op1=ALU.add)
            k.act(sq[:, 2:3], sq[:, 1:2], AF.Sqrt, ["sq1"], ["sq2"])
            k.op("dve", lambda e: e.reciprocal(sq[:, 3:4], sq[:, 2:3]), ["sq2"], ["sq3"])
            k.ts("dve", hn, xsrc, sq[:, 3:4], ALU.mult, [xkey, "sq3", "hn"], ["hn"])
            ck(11)
            pT = k.psum[1][:, 0:1024].bitcast(BF16)
            for kt in range(16):
                k.tr(pT[:, kt * 128:(kt + 1) * 128], hn[:, kt * 128:(kt + 1) * 128], identb, ["hn", "identb"], ["b4", "b5"])
            ck(12)
            k.tt("dve", dst[:, :, j * 128:(j + 1) * 128], pT.rearrange("p (a b) -> p a b", b=128),
                 bc(nw_fm, 2, [128, 16, 128]), ALU.mult, ["b4", "b5", "nw"], [dkey])

    def proj_fm(wv, wkey, cc, N, bank, nk=16, rhs=None, rkey="hnT", start=True, stop=True):
        ps = k.bank(bank)[:, 0:N]
        r = hnT if rhs is None else rhs
        for kt in range(nk):
            k.mm(ps, wv[:, kt, cc * 128:(cc + 1) * 128], r[:, kt, 0:N], start and kt == 0, stop and kt == nk - 1,
                 [wkey, rkey], ["b%d" % bank])
        return ps

    groups = [[0]] + [list(range(1 + 4 * i, 5 + 4 * i)) for i in range(8)]
    gsel = tuple(range(9)) if gsel is None else tuple(gsel)

    mark0 = k.aoff
    if 1 in phases:
        k.dma("sp", nw_fm, nmix, writes=["nw"])
        cw = k.alloc([128, 48, 4], F32)
        cb = k.alloc([128, 48], F32)
        k.dma("sp", cw, convw, writes=["cw"])
        k.dma("sp", cb, convb, writes=["cw"])
        snwf = k.alloc([128, 32], F32)
        k.dma("sp", snwf, snw, writes=["snw"])
        validf = k.alloc([128, NT], F32)
        k.dma("sp", validf, valid, writes=["valid"])
        ck(-3)
        dtb_r = k.alloc([128, 64], F32)
        A_r = k.alloc([128, 64], F32)
        dsk_r = k.alloc([128, 64], F32)
        k.dma("sp", dtb_r, dtb.partition_broadcast(128), writes=["dtb"])
        k.dma("sp", A_r, alog.partition_broadcast(128), writes=["A"])
        k.dma("sp", dsk_r, dsk.partition_broadcast(128), writes=["dsk"])
        k.act(A_r, A_r, AF.Exp, ["A"], ["A"])
        k.ts("dve", A_r, A_r, -1.0, ALU.mult, ["A"], ["A"])
        ck(-4)
        hist = k.alloc([128, 48, 3], F32)
        k.memset("pool", hist, 0.0, ["hist"])
        H = k.alloc([128, 4096], F32)
        k.memset("pool", H, 0.0, ["H"])
        Hb = k.alloc([128, 512], BF16)
        ext_ = [k.alloc([128, 515], F32) for _ in range(2)]
        acc_ = [k.alloc([128, 512], F32) for _ in range(2)]
        xc_ = [k.alloc([128, 512], BF16) for _ in range(2)]
        cvsel = [0]
        Xdt = k.alloc([128, 4, 512], BF16)
        Xdec = k.alloc([128, 4, 512], BF16)
        XD = k.alloc([128, 4, 512], F32)
        Btm = k.alloc([128, 4, 128], BF16)
        BT = k.alloc([128, 512], BF16)
        CT = k.alloc([128, 512], BF16)
        sz = k.alloc([128, 4, 512], BF16)
        dtv = k.alloc([128, 4, 64], F32)
        av = k.alloc([128, 4, 64], F32)
        acs = k.alloc([128, 4, 64], F32)
        nacs = k.alloc([128, 4, 64], F32)
        edec = k.alloc([128, 4, 64], F32)
        wst = k.alloc([128, 4, 64], F32)
        eav = k.alloc([128, 4, 64], F32)
        tmp64 = k.alloc([128, 64], F32)
        AT = k.alloc([128, 8, 128], F32)
        LT = k.alloc([128, 8, 128], BF16)
        MT = k.alloc([128, 8, 128], BF16)
        yv = k.alloc([128, 512], F32)
        ynb = k.alloc([128, 512], BF16)
        ysq = k.alloc([128, 4], F32)
        sst = k.alloc([128, 4, 128], BF16)
        ck(-5)
        wdt = k.alloc([128, 16, 64], BF16)
        k.dma("pool", wdt, w_in[:, O_DT:O_DT + 64].rearrange("(a p) c -> p a c", p=128), writes=["wdt"])
        ck(-6)
        negtri4 = k.alloc([128, 4, 128], F32)
        for i in range(4):
            k.cp("dve", negtri4[:, i, :], negtri, ["cst"], ["negtri4"])

        def conv_chunk(ps, N, ch, outbf, okey, first_tile_reads):
            cv = cvsel[0]
            cvsel[0] ^= 1
            ext, acc, ek, ak = ext_[cv], acc_[cv], "ext%d" % cv, "acc%d" % cv
            k.cp("dve", ext[:, 0:3], hist[:, ch, :], ["hist"], [ek])
            k.cp("act", ext[:, 3:3 + N], ps, first_tile_reads, [ek])
            k.cp("dve", hist[:, ch, :], ext[:, N:N + 3], [ek], ["hist"])
            k.ts("dve", acc[:, 0:N], ext[:, 3:3 + N], cw[:, ch, 3:4], ALU.mult, [ek, "cw"], [ak], s2=cb[:, ch:ch + 1], op1=ALU.add)
            for tap in range(3):
                k.stt("dve", acc[:, 0:N], ext[:, tap:tap + N], cw[:, ch, tap:tap + 1], acc[:, 0:N], ALU.mult, ALU.add, [ek, "cw", ak], [ak])
            k.act(outbf, acc[:, 0:N], AF.Silu, [ak], [okey])

        ck(1)
        for gi, tiles in enumerate(groups):
            if gi not in gsel:
                continue
            own = gi >= 7
            needC = gi >= 6
            nt = len(tiles)
            N = 128 * nt
            make_hnT([xin[t * 128:(t + 1) * 128, :] for t in tiles])
            ck(2)
            for j, t in enumerate(tiles):
                ps = k.bank(6)[:, 0:64]
                for kt in range(16):
                    k.mm(ps, hnT[:, kt, j * 128:(j + 1) * 128], wdt[:, kt, :], kt == 0, kt == 15, ["hnT", "wdt"], ["b6"])
                k.tt("dve", tmp64, ps, dtb_r, ALU.add, ["b6", "dtb"], ["tmp64"])
                ck(31)
                k.act(tmp64, tmp64, AF.Exp, ["tmp64"], ["tmp64"])
                k.act(tmp64, tmp64, AF.Ln, ["tmp64"], ["tmp64"], bias=1.0)
                ck(32)
                k.ts("dve", dtv[:, j, :], tmp64, validf[:, t:t + 1], ALU.mult, ["tmp64", "valid"], ["dtv"])
                k.tt("dve", av[:, j, :], dtv[:, j, :], A_r, ALU.mult, ["dtv", "A"], ["av"])
                ps2 = k.bank(6)[:, 64:128]
                k.mm(ps2, tri_le, av[:, j, :], True, True, ["cst", "av"], ["b6"])
                ps3 = k.bank(6)[:, 128:192]
                k.mm(ps3, onesf, av[:, j, :], True, True, ["cst", "av"], ["b6"])
                ck(33)
                k.cp("dve", acs[:, j, :], ps2, ["b6"], ["acs"])
                ck(331)
                k.ts("dve", nacs[:, j, :], ps2, -1.0, ALU.mult, ["b6"], ["nacs"])
                ck(332)
                k.act(edec[:, j, :], ps3, AF.Exp, ["b6"], ["edec"])
                ck(333)
                k.tt("dve", tmp64, ps3, acs[:, j, :], ALU.subtract, ["b6", "acs"], ["tmp64"])
                ck(334)
                k.act(tmp64, tmp64, AF.Exp, ["tmp64"], ["tmp64"])
                ck(335)
                k.tt("dve", wst[:, j, :], tmp64, dtv[:, j, :], ALU.mult, ["tmp64", "dtv"], ["wst"])
                ck(34)
                if own:
                    k.act(eav[:, j, :], acs[:, j, :], AF.Exp, ["acs"], ["eav"])
            ck(3)
            for g in range(8):
                hs = slice(g * 8, (g + 1) * 8)
                wv, wkey = load_w(w_in, D, O_XS + g * 512, 512, buf=0)
                wvb = wb[1][:, 0:16 * 256].rearrange("p (a b) -> p a b", b=256)
                k.dma("pool", wvb[:, :, 0:128], w_in[:, O_B + g * 128:O_B + (g + 1) * 128].rearrange("(a p) c -> p a c", p=128), writes=["wb1"])
                if needC:
                    k.dma("pool", wvb[:, :, 128:256], w_in[:, O_C + g * 128:O_C + (g + 1) * 128].rearrange("(a p) c -> p a c", p=128), writes=["wb1"])
                pTX = k.psum[0][:, 1024:2048].bitcast(BF16).rearrange("p (a b) -> p a b", b=512)
                pTB = k.bank(2, BF16)
                chunks = [("xs", cc) for cc in range(4)] + [("B", 0)] + ([("C", 1)] if needC else [])

                def do_proj(ch, wv=wv, wkey=wkey, wvb=wvb):
                    kind, i = ch
                    if kind == "xs":
                        return proj_fm(wv, wkey, i, N, i % 2)
                    return proj_fm(wvb, "wb1", i, N, i % 2)

                def do_post(ch, ps, g=g, hs=hs):
                    kind, i = ch
                    if kind == "xs":
                        xc, xck = xc_[i % 2], "xc%d" % (i % 2)
                        conv_chunk(ps, N, g * 4 + i, xc[:, 0:N], xck, ["b%d" % (i % 2)])
                        for j in range(nt):
                            k.tr(pTX[:, j, i * 128:(i + 1) * 128], xc[:, j * 128:(j + 1) * 128], identb, [xck, "identb"], ["b2", "b3"])
                        if i == 3:
                            for j in range(nt):
                                src = pTX[:, j, :].rearrange("p (h d) -> p h d", d=64)
                                k.tt("dve", Xdt[:, j, :].rearrange("p (h d) -> p h d", d=64), src, bc(dtv[:, j, hs], 2, [128, 8, 64]), ALU.mult, ["b2", "b3", "dtv"], ["Xdt"])
                                k.tt("dve", Xdec[:, j, :].rearrange("p (h d) -> p h d", d=64), src, bc(wst[:, j, hs], 2, [128, 8, 64]), ALU.mult, ["b2", "b3", "wst"], ["Xdec"])
                                if own:
                                    k.tt("dve", XD[:, j, :].rearrange("p (h d) -> p h d", d=64), src, bc(dsk_r[:, hs], 2, [128, 8, 64]), ALU.mult, ["b2", "b3", "dsk"], ["XD"])
                    elif kind == "B":
                        conv_chunk(ps, N, 32 + g, BT[:, 0:N], "BT", ["b0"])
                        for j in range(nt):
                            k.tr(pTB[:, j * 128:(j + 1) * 128], BT[:, j * 128:(j + 1) * 128], identb, ["BT", "identb"], ["b2", "b3"])
                        k.cp("act", Btm[:, 0:nt, :], pTB[:, 0:N].rearrange("p (a b) -> p a b", b=128), ["b2", "b3"], ["Btm"])
                    else:
                        conv_chunk(ps, N, 40 + g, CT[:, 0:N], "CT", ["b1"])

                pending = None
                for ch in chunks:
                    ps = do_proj(ch)
                    if pending is not None:
                        do_post(*pending)
                    pending = (ch, ps)
                do_post(*pending)
                if own:
                    wvz, wkeyz = load_w(w_in, D, O_Z + g * 512, 512, buf=0)
                    for j in range(nt):
                        ps = k.bank(j % 2)
                        for kt in range(16):
                            k.mm(ps, hnT[:, kt, j * 128:(j + 1) * 128], wvz[:, kt, :], kt == 0, kt == 15, ["hnT", wkeyz], ["b%d" % (j % 2)])
                        k.act(sz[:, j, :], ps, AF.Silu, ["b%d" % (j % 2)], ["sz"])
                ck(7)
                Hg = H[:, g * 512:(g + 1) * 512]
                for j, t in enumerate(tiles):
                    js = slice(j * 128, (j + 1) * 128)
                    if own:
                        k.cp("act", Hb, Hg, ["H"], ["Hb"])
                        ps_yo = k.bank(6)
                        k.mm(ps_yo, CT[:, js], Hb, True, True, ["CT", "Hb"], ["b6"])
                        ps_cb = k.bank(7)[:, 0:128]
                        k.mm(ps_cb, BT[:, js], CT[:, js], True, True, ["BT", "CT"], ["b7"])
                        k.tt("pool", AT, bc(tri_le, 1, [128, 8, 128]), bc(av[:, j, hs], 2, [128, 8, 128]), ALU.mult, ["cst", "av"], ["AT"])
                        psL = k.psum[1][:, 0:1024].rearrange("p (a b) -> p a b", b=128)
                        for hf in range(2):
                            k.mm(psL[:, hf * 4:(hf + 1) * 4, :], onesf, AT[:, hf * 4:(hf + 1) * 4, :], True, False, ["cst", "AT"], ["b4", "b5"])
                            k.mm(psL[:, hf * 4:(hf + 1) * 4, :], identf, negtri4, False, True, ["cst", "negtri4"], ["b4", "b5"])
                        for hh in range(8):
                            k.act(LT[:, hh, :], psL[:, hh, :], AF.Exp, ["b4", "b5", "nacs"], ["LT"], bias=nacs[:, j, g * 8 + hh:g * 8 + hh + 1])
                        k.tt("dve", MT, LT, bc(ps_cb, 1, [128, 8, 128]), ALU.mult, ["LT", "b7"], ["MT"])
                        ps_y = k.bank(7)[:, 0:512]
                        for hh in range(8):
                            k.mm(ps_y[:, hh * 64:(hh + 1) * 64], MT[:, hh, :], Xdt[:, j, hh * 64:(hh + 1) * 64], True, True, ["MT", "Xdt"], ["b7"])
                        y3 = yv.rearrange("p (h d) -> p h d", d=64)
                        k.tt("dve", y3, ps_yo.rearrange("p (h d) -> p h d", d=64), bc(eav[:, j, hs], 2, [128, 8, 64]), ALU.mult, ["b6", "eav"], ["yv"])
                        k.tt("dve", yv, yv, ps_y, ALU.add, ["yv", "b7"], ["yv"])
                        k.tt("dve", yv, yv, XD[:, j, :], ALU.add, ["yv", "XD"], ["yv"])
                        k.tt("dve", yv, yv, sz[:, j, :], ALU.mult, ["yv", "sz"], ["yv"])
                        k.memset("pool", ysq[:, 0:1], 0.0, ["ysq0"])
                        k.act(ynb, yv, AF.Square, ["yv", "ysq0"], ["ynb", "ysq0"], accum_out=ysq[:, 0:1])
                        k.ts("dve", ysq[:, 1:2], ysq[:, 0:1], 1.0 / 512, ALU.mult, ["ysq0"], ["ysq1"], s2=EPS, op1=ALU.add)
                        k.act(ysq[:, 2:3], ysq[:, 1:2], AF.Sqrt, ["ysq1"], ["ysq2"])
                        k.op("dve", lambda e: e.reciprocal(ysq[:, 3:4], ysq[:, 2:3]), ["ysq2"], ["ysq3"])
                        k.ts("dve", ynb, yv, ysq[:, 3:4], ALU.mult, ["yv", "ysq3", "ynb"], ["ynb"])
                        pTy = k.bank(3, BF16)
                        for cc in range(4):
                            k.tr(pTy[:, cc * 128:(cc + 1) * 128], ynb[:, cc * 128:(cc + 1) * 128], identb, ["ynb", "identb"], ["b2", "b3"])
                        for cc in range(4):
                            k.act(sst[:, cc, :], pTy[:, cc * 128:(cc + 1) * 128], AF.Copy, ["b2", "b3", "snw"], ["sst"], scale=snwf[:, g * 4 + cc:g * 4 + cc + 1])
                        ot = t - NT_CTX
                        k.dma("sp", st_ssm[:, g * 4:(g + 1) * 4, ot * 128:(ot + 1) * 128], sst, reads=["sst"], writes=["st_ssm"])
                    sb_ = 6 if own else 6 + j % 2
                    ps_S = k.bank(sb_)
                    k.mm(ps_S, Btm[:, j, :], Xdec[:, j, :], True, True, ["Btm", "Xdec"], ["b%d" % sb_])
                    Hg3 = Hg.rearrange("p (h d) -> p h d", d=64)
                    k.tt("dve", Hg3, Hg3, bc(edec[:, j, hs], 2, [128, 8, 64]), ALU.mult, ["H", "edec"], ["H"])
                    k.tt("dve", Hg, Hg, ps_S, ALU.add, ["H", "b%d" % sb_], ["H"])
                ck(8)
        if dbg:
            dtile = k.alloc([128, 32, 128], BF16)
            for ot in range(NT_OWN):
                k.dma("sp", dtile, st_ssm[:, :, ot * 128:(ot + 1) * 128], reads=["st_ssm"], writes=["dtile"])
                out_toks.append(k.dma("sp", dbg_out["d_ssm"][:, :, ot * 128:(ot + 1) * 128], dtile, reads=["dtile"]))
    k.aoff = mark0

    def rope_fm(ps_a, ps_b, tbuf, tsel, N, out, okey, akey, bkey, r1, r2):
        k.tt("dve", r1[:, 0:N], ps_a, tbuf[:, tsel, 0:N], ALU.mult, [akey, "tb"], ["r1"])
        k.tt("dve", r2[:, 0:N], ps_b, tbuf[:, tsel + 1, 0:N], ALU.mult, [bkey, "tb"], ["r2"])
        k.tt("dve", out, r1[:, 0:N], r2[:, 0:N], ALU.add, ["r1", "r2"], [okey])

    if 2 in phases:
        k.barrier()
        k.dma("sp", nw_fm, nmix, writes=["nw"])
        KT = k.alloc([128, 4, NSLOT], BF16)
        V = k.alloc([128, NT, 4, 130], BF16)
        kiT = k.alloc([128, NSLOT], BF16)
        mark2 = k.aoff
        k.memset("pool", V, 1.0, ["V"])
        tb = k.alloc([128, 4, 512], F32)
        r1 = k.alloc([128, 512], F32)
        r2 = k.alloc([128, 512], F32)
        for gi, tiles in enumerate(groups):
            if gi not in gsel:
                continue
            nt = len(tiles)
            N = 128 * nt
            s0 = tiles[0] * 128
            make_hnT([xin[t * 128:(t + 1) * 128, :] for t in tiles])
            k.dma("sp", tb[:, :, 0:N], tabs[:, :, s0:s0 + N].rearrange("a p s -> p a s"), writes=["tb"])
            wv, wkey = load_w(w_in, D, O_K, 512, buf=0)
            wr, rkey = load_w(w_rot, D, R_K, 512, buf=1)
            for cc in range(4):
                pa = proj_fm(wv, wkey, cc, N, 0)
                pb = proj_fm(wr, rkey, cc, N, 1)
                rope_fm(pa, pb, tb, 0, N, KT[:, cc, s0:s0 + N], "KT", "b0", "b1", r1, r2)
            wki = wb[0][:, 0:16 * 256].rearrange("p (a b) -> p a b", b=256)
            for half in range(2):
                k.dma("pool", wki[:, :, half * 64:(half + 1) * 64], w_in[:, O_KI:O_KI + 64].rearrange("(a p) c -> p a c", p=128), writes=["wb0"])
                k.dma("pool", wki[:, :, 128 + half * 64:128 + (half + 1) * 64], w_rot[:, R_KI:R_KI + 64].rearrange("(a p) c -> p a c", p=128), writes=["wb0"])
            pa = proj_fm(wki, "wb0", 0, N, 0)
            pb = proj_fm(wki, "wb0", 1, N, 1)
            rope_fm(pa, pb, tb, 2, N, kiT[:, s0:s0 + N], "kiT", "b0", "b1", r1, r2)
            wvv, wkeyv = load_w(w_in, D, O_V, 512, buf=1)
            for j, t in enumerate(tiles):
                ps = k.bank(2 + j % 2)
                for kt in range(16):
                    k.mm(ps, hnT[:, kt, j * 128:(j + 1) * 128], wvv[:, kt, :], kt == 0, kt == 15, ["hnT", wkeyv], ["b%d" % (2 + j % 2)])
                k.cp("act", V[:, t, :, 0:128], ps.rearrange("p (g d) -> p g d", d=128), ["b%d" % (2 + j % 2)], ["V"])
        k.aoff = mark2

    if 3 in phases:
        k.barrier()
        MB = k.view(hnT_off, [128, NT, 128], BF16)
        tb3 = k.view(hnT_off + 8448 + 4096, [128, 4, 128], F32)
        hnT3 = k.alloc([128, 16, 128], BF16)
        padb_r = k.alloc([128, CTX], BF16)
        k.dma("pool", padb_r, padb.partition_broadcast(128), writes=["padb"])
        r1 = k.alloc([128, 128], F32)
        r2 = k.alloc([128, 128], F32)
        qT_ = [k.alloc([128, 16, 128], BF16), k.view(hnT_off + 8448, [128, 16, 128], BF16)]
        qiT = k.alloc([128, 8, 128], BF16)
        wis = k.alloc([128, 16], F32)
        score = k.alloc([128, NSLOT], F32)
        wm_off = k.aoff
        work = k.alloc([128, NSLOT], F32)
        maskb = k.view(wm_off, [128, NSLOT], BF16)
        maskT = k.view(wm_off + NSLOT * 2, [128, NT, 128], BF16)
        rl_ = [k.alloc([128, 512], F32) for _ in range(2)]
        m8 = k.alloc([128, 8], F32)
        thr = k.alloc([128, 1], F32)
        Et_ = [k.alloc([128, 4, 128], BF16) for _ in range(2)]
        ao = k.alloc([128, 16, 128], BF16)
        rs = k.alloc([128, 4], F32)
        aT = k.alloc([128, 16, 128], BF16)
        wwi = k.alloc([128, 16, 16], BF16)
        k.dma("pool", wwi, w_in[:, O_WI:O_WI + 16].rearrange("(a p) c -> p a c", p=128), writes=["wwi"])

        def A1(t):
            ot = t - NT_CTX
            qT, qk = qT_[ot % 2], "qT%d" % (ot % 2)
            N = 128
            s0 = t * 128
            make_hnT([xin[t * 128:(t + 1) * 128, :]], dst=hnT3, dkey="hnT3")
            k.dma("sp", tb3, tabs[:, :, s0:s0 + N].rearrange("a p s -> p a s"), writes=["tb"])
            for c4 in range(4):
                wv, wkey = load_w(w_in, D, O_Q + c4 * 512, 512, buf=0)
                wr, rkey = load_w(w_rot, D, R_Q + c4 * 512, 512, buf=1)
                for cc in range(4):
                    pa = proj_fm(wv, wkey, cc, N, 0, rhs=hnT3, rkey="hnT3")
                    pb = proj_fm(wr, rkey, cc, N, 1, rhs=hnT3, rkey="hnT3")
                    rope_fm(pa, pb, tb3, 0, N, qT[:, c4 * 4 + cc, :], qk, "b0", "b1", r1, r2)
            for c4 in range(2):
                wv, wkey = load_w(w_in, D, O_QI + c4 * 512, 512, buf=0)
                wr, rkey = load_w(w_rot, D, R_QI + c4 * 512, 512, buf=1)
                for cc in range(4):
                    pa = proj_fm(wv, wkey, cc, N, 0, rhs=hnT3, rkey="hnT3")
                    pb = proj_fm(wr, rkey, cc, N, 1, rhs=hnT3, rkey="hnT3")
                    rope_fm(pa, pb, tb3, 2, N, qiT[:, c4 * 4 + cc, :], "qiT", "b0", "b1", r1, r2)
            ps = k.bank(2)[:, 0:16]
            for kt in range(16):
                k.mm(ps, hnT3[:, kt, 0:128], wwi[:, kt, :], kt == 0, kt == 15, ["hnT3", "wwi"], ["b2"])
            k.ts("dve", wis, ps, 1.0 / 32.0, ALU.mult, ["b2"], ["wis"])
            nk = t + 1
            S = nk * 128
            chunks = [(c0, min(512, CTX - c0)) for c0 in range(0, CTX, 512)] + [(c0, min(512, S - c0)) for c0 in range(CTX, S, 512)]
            for ci, (c0, n) in enumerate(chunks):
                for h in range(16):
                    hp = slice((h % 2) * 64, (h % 2) * 64 + 64)
                    bk = 2 + (h % 2)
                    rl, rlk = rl_[h % 2], "rl%d" % (h % 2)
                    ps = k.bank(bk)[:, 0:n]
                    k.mm(ps, qiT[hp, h // 2, :], kiT[hp, c0:c0 + n], True, True, ["qiT", "kiT"], ["b%d" % bk])
                    k.act(rl[:, 0:n], ps, AF.Relu, ["b%d" % bk], [rlk])
                    if h == 0:
                        if c0 < CTX:
                            k.stt("dve", score[:, c0:c0 + n], rl[:, 0:n], wis[:, 0:1], padb_r[:, c0:c0 + n], ALU.mult, ALU.add, [rlk, "wis", "padb"], ["score"])
                        else:
                            k.ts("dve", score[:, c0:c0 + n], rl[:, 0:n], wis[:, 0:1], ALU.mult, [rlk, "wis"], ["score"])
                    else:
                        k.stt("dve", score[:, c0:c0 + n], rl[:, 0:n], wis[:, h:h + 1], score[:, c0:c0 + n], ALU.mult, ALU.add, [rlk, "wis", "score"], ["score"])
            k.tt("dve", score[:, S - 128:S], score[:, S - 128:S], causb, ALU.add, ["score", "cst"], ["score"])
            if dbg and t == NT_CTX:
                out_toks.append(k.dma("sp", dbg_out["d_score"], score, reads=["score"]))

        def TK(t):
            S = (t + 1) * 128
            cur, ckey = score, "score"
            for r in range(32):
                k.op("dve", lambda e, cur=cur, S=S: e.max(out=m8, in_=cur[:, 0:S]), [ckey], ["m8"])
                if r < 31:
                    k.op("dve", lambda e, cur=cur, S=S: e.match_replace(out=work[:, 0:S], in_to_replace=m8, in_values=cur[:, 0:S], imm_value=NEG),
                         [ckey, "m8", "wm"], ["wm"])
                    cur, ckey = work, "wm"
            k.ts("dve", thr, m8[:, 7:8], -1.0e29, ALU.max, ["m8"], ["thr"])
            k.ts("dve", maskb[:, 0:S], score[:, 0:S], thr, ALU.is_ge, ["score", "thr", "wm"], ["wm"])
            if dbg and t == NT_CTX:
                out_toks.append(k.dma("sp", dbg_out["d_mask"], maskb, reads=["wm"]))
                out_toks.append(k.dma("sp", dbg_out["d_thr"], m8, reads=["m8"]))

        def A2(t):
            nk = t + 1
            for kt0 in range(0, nk, 8):
                nb = min(8, nk - kt0)
                pTm = k.bank(4, BF16)
                for i2 in range(nb):
                    k.tr(pTm[:, i2 * 128:(i2 + 1) * 128], maskb[:, (kt0 + i2) * 128:(kt0 + i2 + 1) * 128], identb, ["wm", "identb"], ["b4"])
                k.cp("act", maskT[:, kt0:kt0 + nb, :], pTm[:, 0:nb * 128].rearrange("p (a b) -> p a b", b=128), ["b4", "wm"], ["wm"])
            k.ts("dve", MB[:, 0:nk, :], maskT[:, 0:nk, :], -1.0, ALU.add, ["wm"], ["MB"], s2=30000.0, op1=ALU.mult)

        def B(t):
            ot = t - NT_CTX
            qT, qk = qT_[ot % 2], "qT%d" % (ot % 2)
            nk = t + 1
            for kvg in range(4):
                po = k.psum[1][:, 1024:2048].rearrange("p (a b) -> p a b", b=256)

                def sc_stage(kt, kvg=kvg):
                    bk = kt % 2
                    Et, etk = Et_[bk], "Et%d" % bk
                    ps = k.bank(bk)
                    k.mm(ps, KT[:, kvg, kt * 128:(kt + 1) * 128], qT[:, kvg * 4:(kvg + 1) * 4, :], True, False, ["KT", qk], ["b%d" % bk])
                    for hh in range(4):
                        k.mm(ps[:, hh * 128:(hh + 1) * 128], identb, MB[:, kt, :], False, hh == 3, ["identb", "MB"], ["b%d" % bk])
                    k.act(Et, ps.rearrange("p (a b) -> p a b", b=128), AF.Exp, ["b%d" % bk], [etk], scale=1.0 / math.sqrt(128.0))

                def pv_stage(kt, kvg=kvg, po=po):
                    bk = kt % 2
                    Et, etk = Et_[bk], "Et%d" % bk
                    for hh in range(4):
                        k.mm(po[:, hh, 0:129], Et[:, hh, :], V[:, kt, kvg, 0:129], kt == 0 and hh % 2 == 0, kt == nk - 1, [etk, "V"], ["b%d" % (6 + hh // 2)])

                sc_stage(0)
                for kt in range(nk):
                    if kt + 1 < nk:
                        sc_stage(kt + 1)
                    pv_stage(kt)
                k.act(rs, po[:, :, 128], AF.Ln, ["b6", "b7"], ["rs"])
                k.act(rs, rs, AF.Exp, ["rs"], ["rs"], scale=-1.0)
                for hh in range(4):
                    k.act(ao[:, kvg * 4 + hh, :], po[:, hh, 0:128], AF.Copy, ["b%d" % (6 + hh // 2), "rs"], ["ao"], scale=rs[:, hh:hh + 1])
            pTa = k.psum[1][:, 0:1024].bitcast(BF16)
            for h in range(16):
                k.tr(pTa[:, h * 128:(h + 1) * 128], ao[:, h, :], identb, ["ao", "identb"], ["b4", "b5"])
            k.cp("act", aT, pTa.rearrange("p (a b) -> p a b", b=128), ["b4", "b5"], ["aT"])
            k.dma("sp", st_attn[:, :, ot * 128:(ot + 1) * 128], aT, reads=["aT"], writes=["st_attn"])

        sel = [t for t in range(NT_CTX, NT) if (7 if t < NT_CTX + 4 else 8) in gsel]
        prev = None
        for t in sel:
            A1(t)
            TK(t)
            if prev is not None:
                B(prev)
            A2(t)
            prev = t
        if prev is not None:
            B(prev)
        if dbg:
            for ot in range(NT_OWN):
                k.dma("sp", aT, st_attn[:, :, ot * 128:(ot + 1) * 128], reads=["st_attn"], writes=["aT"])
                out_toks.append(k.dma("sp", dbg_out["d_attn"][:, :, ot * 128:(ot + 1) * 128], aT, reads=["aT"]))
    k.aoff = mark0

    h2 = k.alloc([128, 4, D], F32)
    mark4 = k.aoff
    for gi in (7, 8):
        tiles = groups[gi]
        otiles = [t - NT_CTX for t in tiles]
        o0 = otiles[0] * 128
        if 4 in phases:
            k.barrier()
            k.aoff = mark4
            k.dma("sp", nw_fm, nmix, writes=["nw"])
            wb2 = k.alloc([128, 16 * 512], BF16)
            actT = k.alloc([128, 32, 512], BF16)
            mg = k.alloc([128, 16, 512], BF16)
            sga = k.alloc([128, 512], F32)
            t1 = k.alloc([128, 512], F32)
            make_hnT([xin[t * 128:(t + 1) * 128, :] for t in tiles])
            k.dma("sp", actT[:, 0:16, :], st_attn[:, :, o0:o0 + 512], reads=["st_attn"], writes=["actT"])
            for c4 in range(4):
                wg, gkey = load_w(w_in, D, O_G + c4 * 512, 512, buf=0)
                wv, wkey = load_w(w_pa, 2048, c4 * 512, 512, buf=1)
                for cc in range(4):
                    pg = proj_fm(wg, gkey, cc, 512, 0)
                    pa = proj_fm(wv, wkey, cc, 512, 1, rhs=actT, rkey="actT")
                    k.act(sga, pg, AF.Sigmoid, ["b0"], ["sga"])
                    k.tt("dve", mg[:, c4 * 4 + cc, :], sga, pa, ALU.mult, ["sga", "b1"], ["mg"])
            k.dma("sp", actT, st_ssm[:, :, o0:o0 + 512], reads=["st_ssm", "actT"], writes=["actT"])
            for c4 in range(4):
                wg = wb2[:, :].rearrange("p (a b) -> p a b", b=512)
                k.dma("pool", wg, w_in[:, O_G + 2048 + c4 * 512:O_G + 2048 + (c4 + 1) * 512].rearrange("(a p) c -> p a c", p=128), writes=["wb2"])
                for c2 in range(2):
                    wv, wkey = load_w(w_ps, 4096, c4 * 512 + c2 * 256, 256, buf=c2)
                    for cc in range(2):
                        col = c4 * 4 + c2 * 2 + cc
                        pg = proj_fm(wg, "wb2", c2 * 2 + cc, 512, 0)
                        pa = proj_fm(wv, wkey, cc, 512, 1, nk=32, rhs=actT, rkey="actT")
                        k.act(sga, pg, AF.Sigmoid, ["b0"], ["sga"])
                        k.tt("dve", t1, sga, pa, ALU.mult, ["sga", "b1"], ["t1"])
                        k.tt("dve", mg[:, col, :], mg[:, col, :], t1, ALU.add, ["mg", "t1"], ["mg"])
            for c4 in range(4):
                wv, wkey = load_w(w_o, 2048, c4 * 512, 512, buf=c4 % 2)
                for j, t in enumerate(tiles):
                    bk = 2 + j % 2
                    ps = k.bank(bk)
                    for kt in range(16):
                        k.mm(ps, mg[:, kt, j * 128:(j + 1) * 128], wv[:, kt, :], kt == 0, kt == 15, ["mg", wkey], ["b%d" % bk])
                    if c4 == 0:
                        k.dma("sp", h2[:, j, :], xin[t * 128:(t + 1) * 128, :], writes=["h2_%d" % j])
                    k.tt("dve", h2[:, j, c4 * 512:(c4 + 1) * 512], h2[:, j, c4 * 512:(c4 + 1) * 512], ps, ALU.add, ["h2_%d" % j, "b%d" % bk], ["h2_%d" % j])
            if dbg:
                for j, ot in enumerate(otiles):
                    out_toks.append(k.dma("sp", dbg_out["d_h2"][:, ot, :], h2[:, j, :], reads=["h2_%d" % j]))
        if 5 in phases:
            k.barrier()
            k.aoff = mark4
            k.dma("sp", nw_fm, nffn, writes=["nw"])
            nfin_r = xt
            k.dma("sp", nfin_r, nfin.partition_broadcast(128), writes=["xt"])
            keysb = k.alloc([128, 2, 128], BF16)
            k.dma("pool", keysb, keysT, writes=["keysb"])
            q_off = k.aoff
            qpT = k.alloc([128, 16, 512], BF16)
            osb = k.view(q_off, [128, D], F32)
            ssc = k.alloc([128, 4, 16, 128], F32)
            top = k.alloc([128, 16, 16], F32)
            wk2 = k.alloc([128, 128], F32)
            cand = k.alloc([128, 256], F32)
            cand2 = k.alloc([128, 256], F32)
            c8 = k.alloc([128, 8], F32)
            thr5 = k.alloc([128, 4, 8], F32)
            nb5 = k.alloc([128, 4, 8], F32)
            zz = k.alloc([128, 4], F32)
            Gt_ = [k.alloc([128, 512], BF16) for _ in range(2)]
            GT_ = [k.alloc([128, 4, 128], BF16) for _ in range(2)]
            cand3 = k.alloc([128, 256], F32)
            c8b = k.alloc([128, 8], F32)
            tau5 = k.alloc([128, 4, 8], F32)
            tsum = k.alloc([128, 8], F32)
            Ex_ = [k.alloc([128, 512], F32) for _ in range(3)]
            Wh_ = [k.alloc([128, 512], BF16) for _ in range(3)]
            GWT_ = [k.alloc([128, 4, 128], BF16) for _ in range(2)]
            wb5 = [wb[0], wb[1], k.alloc([128, 16 * 512], BF16), k.alloc([128, 16 * 512], BF16)]
            make_hnT([h2[:, j, :] for j in range(4)], keys=["h2_%d" % j for j in range(4)])
            for c4 in range(4):
                wv, wkey = load_w(w_pq, 2048, c4 * 512, 512, buf=c4 % 2)
                for cc in range(4):
                    pa = proj_fm(wv, wkey, cc, 512, 0)
                    k.cp("act", qpT[:, c4 * 4 + cc, :], pa, ["b0"], ["qpT"])
            for j in range(4):
                js = slice(j * 128, (j + 1) * 128)
                for q4 in range(4):
                    bk = 2 + q4 % 2
                    ps = k.bank(bk)
                    for i2 in range(4):
                        hc = q4 * 4 + i2
                        k.mm(ps[:, i2 * 128:(i2 + 1) * 128], qpT[:, hc, js], keysb[:, hc % 2, :], True, True, ["qpT", "keysb"], ["b%d" % bk])
                    k.cp("act", ssc[:, j, q4 * 4:(q4 + 1) * 4, :], ps.rearrange("p (a b) -> p a b", b=128), ["b%d" % bk], ["ssc"])
                for hc in range(16):
                    k.op("dve", lambda e, hc=hc, j=j: e.max(out=top[:, hc, 0:8], in_=ssc[:, j, hc, :]), ["ssc"], ["top"])
                    k.op("dve", lambda e, hc=hc, j=j: e.match_replace(out=wk2, in_to_replace=top[:, hc, 0:8], in_values=ssc[:, j, hc, :], imm_value=NEG), ["ssc", "top", "wk2"], ["wk2"])
                    k.op("dve", lambda e, hc=hc: e.max(out=top[:, hc, 8:16], in_=wk2), ["wk2", "top"], ["top"])
                for h in range(8):
                    c3 = cand.rearrange("p (a b) -> p a b", b=16)
                    k.tt("dve", c3, bc(top[:, 2 * h, :], 2, [128, 16, 16]), bc(top[:, 2 * h + 1, :], 1, [128, 16, 16]), ALU.add, ["top"], ["cand"])
                    k.op("dve", lambda e: e.max(out=c8, in_=cand), ["cand"], ["c8"])
                    k.ts("dve", zz[:, 0:1], c8[:, 0:1], -1.0, ALU.mult, ["c8"], ["zz0"])
                    k.op("dve", lambda e: e.match_replace(out=cand2, in_to_replace=c8, in_values=cand, imm_value=NEG), ["cand", "c8", "cand2"], ["cand2"])
                    k.op("dve", lambda e: e.max(out=c8, in_=cand2), ["cand2", "c8"], ["c8"])
                    k.cp("dve", thr5[:, j, h:h + 1], c8[:, 7:8], ["c8"], ["thr5"])
                    k.op("dve", lambda e: e.match_replace(out=cand3, in_to_replace=c8, in_values=cand2, imm_value=NEG), ["cand2", "c8", "cand3"], ["cand3"])
                    k.op("dve", lambda e: e.max(out=c8b, in_=cand3), ["cand3", "c8b"], ["c8b"])
                    k.tt("dve", tsum[:, h:h + 1], c8[:, 7:8], c8b[:, 0:1], ALU.add, ["c8", "c8b"], ["tsum"])
                    k.act(cand3, cand, AF.Exp, ["cand", "zz0", "cand3"], ["cand3"], bias=zz[:, 0:1])
                    k.stt("dve", cand3, cand, thr5[:, j, h:h + 1], cand3, ALU.is_ge, ALU.mult, ["cand", "thr5", "cand3"], ["cand3"])
                    k.op("dve", lambda e: e.reduce_sum(out=zz[:, 1:2], in_=cand3, axis=mybir.AxisListType.X), ["cand3"], ["zz1"])
                    k.act(zz[:, 2:3], zz[:, 1:2], AF.Ln, ["zz1"], ["zz2"])
                    k.tt("dve", nb5[:, j, h:h + 1], zz[:, 0:1], zz[:, 2:3], ALU.subtract, ["zz0", "zz2"], ["nb5"])
                k.stt("dve", tau5[:, j, :], tsum, 0.5, nb5[:, j, :], ALU.mult, ALU.add, ["tsum", "nb5"], ["tau5"])
                k.act(tau5[:, j, :], tau5[:, j, :], AF.Exp, ["tau5"], ["tau5"])
                for h in range(8):
                    k.act(ssc[:, j, 2 * h, :], ssc[:, j, 2 * h, :], AF.Exp, ["ssc", "nb5"], ["ssc"], bias=nb5[:, j, h:h + 1])
                    k.act(ssc[:, j, 2 * h + 1, :], ssc[:, j, 2 * h + 1, :], AF.Exp, ["ssc"], ["ssc"])
            iters = [(ec, j) for ec in range(NEC) for j in range(4)]

            def bufs(ec):
                uv = wb5[(ec % 2) * 2][:, :].rearrange("p (a b) -> p a b", b=512)
                vv = wb5[(ec % 2) * 2 + 1][:, :].rearrange("p (a b) -> p a b", b=2048)
                ukey = "wb0" if ec % 2 == 0 else "wu1"
                vkey = "wb1" if ec % 2 == 0 else "wv1"
                return uv, vv, ukey, vkey

            def load5(ec):
                uv, vv, ukey, vkey = bufs(ec)
                k.dma("pool", uv, uT[:, ec * 512:(ec + 1) * 512].rearrange("(a p) c -> p a c", p=128), writes=[ukey])
                k.dma("pool", vv, pv[ec * 512:(ec + 1) * 512, :].rearrange("(a p) c -> p a c", p=128), writes=[vkey])

            def stageA(idx):
                ec, j = iters[idx]
                uv, vv, ukey, vkey = bufs(ec)
                js = slice(j * 128, (j + 1) * 128)
                jb = idx % 2
                Gt, GT, GWT = Gt_[jb], GT_[jb], GWT_[jb]
                gk, gtk, gwtk = "Gt%d" % jb, "GT%d" % jb, "GWT%d" % jb
                ps = k.bank(jb)
                for kt in range(16):
                    k.mm(ps, hnT[:, kt, js], uv[:, kt, :], kt == 0, kt == 15, ["hnT", ukey], ["b%d" % jb])
                k.act(Gt, ps, AF.Gelu, ["b%d" % jb], [gk])
                if idx >= 1:
                    stageB_pe(idx - 1)
                pTg = k.bank(jb, BF16)
                for b4 in range(4):
                    k.tr(pTg[:, b4 * 128:(b4 + 1) * 128], Gt[:, b4 * 128:(b4 + 1) * 128], identb, [gk, "identb"], ["b%d" % jb])
                k.cp("act", GT, pTg[:, 0:512].rearrange("p (a b) -> p a b", b=128), ["b%d" % jb], [gtk])
                pW = k.psum[0][:, 1024:2048].rearrange("p (a b) -> p a b", b=256)[:, :, 0:128]
                for h in range(8):
                    hb = (idx * 8 + h) % 3
                    Ex, Wh = Ex_[hb], Wh_[hb]
                    ek, whk = "Ex%d" % hb, "Wh%d" % hb
                    eng = "dve" if h % 4 == 3 else "pool"
                    k.tt(eng, Ex.rearrange("p (a b) -> p a b", b=128), bc(ssc[:, j, 2 * h, ec * 4:(ec + 1) * 4], 2, [128, 4, 128]),
                         bc(ssc[:, j, 2 * h + 1, :], 1, [128, 4, 128]), ALU.mult, ["ssc"], [ek])
                    k.stt("dve", Wh, Ex, tau5[:, j, h:h + 1], Ex, ALU.is_ge, ALU.mult, ["tau5", ek], [whk])
                    for b4 in range(4):
                        k.mm(pW[:, b4, :], Wh[:, b4 * 128:(b4 + 1) * 128], identb, h == 0 and b4 % 2 == 0, h == 7, [whk, "identb"], ["b%d" % (2 + b4 // 2)])
                k.tt("dve", GWT, pW, GT, ALU.mult, ["b2", "b3", gtk], [gwtk])
                if idx >= 1:
                    stageB_dve(idx - 1)

            def stageB_pe(idx):
                ec, j = iters[idx]
                uv, vv, ukey, vkey = bufs(ec)
                jb = idx % 2
                GWT, gwtk = GWT_[jb], "GWT%d" % jb
                po = k.psum[1][:, 0:2048]
                for b4 in range(4):
                    for dc in range(4):
                        k.mm(po[:, dc * 512:(dc + 1) * 512], GWT[:, b4, :], vv[:, b4, dc * 512:(dc + 1) * 512], b4 == 0, b4 == 3, [gwtk, vkey], ["b%d" % (4 + dc)])

            def stageB_dve(idx):
                ec, j = iters[idx]
                po = k.psum[1][:, 0:2048]
                k.tt("dve", h2[:, j, :], h2[:, j, :], po, ALU.add, ["h2_%d" % j, "b4", "b5", "b6", "b7"], ["h2_%d" % j])

            load5(0)
            for idx in range(len(iters)):
                ec, j = iters[idx]
                stageA(idx)
                if j == 1 and ec + 1 < NEC:
                    load5(ec + 1)
            stageB_pe(len(iters) - 1)
            stageB_dve(len(iters) - 1)
            k.barrier()
            for j, ot in enumerate(otiles):
                k.memset("pool", sq[:, 0:1], 0.0, ["sq0"])
                k.act(osb, h2[:, j, :], AF.Square, ["h2_%d" % j, "sq0"], ["osb", "sq0"], accum_out=sq[:, 0:1])
                k.ts("dve", sq[:, 1:2], sq[:, 0:1], 1.0 / D, ALU.mult, ["sq0"], ["sq1"], s2=EPS, op1=ALU.add)
                k.act(sq[:, 2:3], sq[:, 1:2], AF.Sqrt, ["sq1"], ["sq2"])
                k.op("dve", lambda e: e.reciprocal(sq[:, 3:4], sq[:, 2:3]), ["sq2"], ["sq3"])
                k.stt("dve", osb, h2[:, j, :], sq[:, 3:4], nfin_r, ALU.mult, ALU.mult, ["h2_%d" % j, "sq3", "xt", "osb"], ["osb"])
                out_toks.append(k.dma("sp", yout[ot * 128:(ot + 1) * 128, :], osb, reads=["osb"]))


def _rot_cols(w, head_dim):
    half = head_dim // 2
    n = w.shape[1]
    idx = np.arange(n)
    r = (idx // head_dim) * head_dim + (idx % head_dim + half) % head_dim
    return w[:, r]


def _tables(pos):
    res = []
    for hd in (128, 64):
        half = hd // 2
        p = np.arange(128) % hd
        inv = (10000.0 ** (-(np.arange(half, dtype=np.float32)) / np.float32(half))).astype(np.float32)
        ang = pos.astype(np.float32)[None, :] * inv[p % half][:, None]
        c = np.cos(ang).astype(np.float32)
        s = np.sin(ang).astype(np.float32)
        sgn = np.where(p < half, -1.0, 1.0).astype(np.float32)[:, None]
        res += [c, s * sgn]
    return np.stack(res, 0).astype(np.float32)


_NC_CACHE = {}


def kernel(x, meta_tokens, norm_mix_w, w_in, conv_w, conv_b, dt_bias, a_log, d_skip, ssm_norm_w,
           w_branch_attn, w_branch_ssm, w_out, norm_ffn_w, peer_w_query, peer_sub_keys, peer_u, peer_v,
           norm_final_w, _phases=(1, 2, 3, 4, 5), _dbg=False, _gsel=None, _ncores=8, _trace=False):
    f = lambda a: np.ascontiguousarray(np.asarray(a, dtype=np.float32))
    x = f(x)
    meta = f(meta_tokens)
    w_in0 = f(w_in)[0]
    w_rot = np.concatenate([
        _rot_cols(w_in0[:, O_Q:O_Q + 2048], 128), _rot_cols(w_in0[:, O_K:O_K + 512], 128),
        _rot_cols(w_in0[:, O_QI:O_QI + 1024], 64), _rot_cols(w_in0[:, O_KI:O_KI + 64], 64)], axis=1)
    w_rot = np.ascontiguousarray(w_rot)
    cst = np.zeros((128, 5, 128), np.float32)
    ii = np.arange(128)
    cst[:, 0, :] = np.eye(128)
    cst[:, 1, :] = (ii[:, None] <= ii[None, :])
    cst[:, 2, :] = np.where(ii[:, None] > ii[None, :], -30000.0, 0.0)
    cst[:, 3, :] = np.where(ii[None, :] <= ii[:, None], 0.0, NEG)
    cst[:, 4, :] = 1.0
    common = {
        "cst": cst, "w_in": w_in0, "w_rot": w_rot,
        "convw": np.ascontiguousarray(f(conv_w)[0].T.reshape(48, 128, 4).transpose(1, 0, 2)),
        "convb": np.ascontiguousarray(f(conv_b)[0].reshape(48, 128).T),
        "dtb": f(dt_bias)[0], "alog": f(a_log)[0], "dsk": f(d_skip)[0],
        "snw": np.ascontiguousarray(f(ssm_norm_w)[0].reshape(32, 128).T),
        "nmix": np.ascontiguousarray(f(norm_mix_w)[0].reshape(16, 128).T),
        "nffn": np.ascontiguousarray(f(norm_ffn_w)[0].reshape(16, 128).T),
        "nfin": f(norm_final_w),
        "w_pa": f(w_branch_attn)[0], "w_ps": f(w_branch_ssm)[0], "w_o": f(w_out)[0], "w_pq": f(peer_w_query)[0],
        "keysT": np.ascontiguousarray(f(peer_sub_keys)[0].transpose(2, 0, 1)),
        "uT": np.ascontiguousarray(f(peer_u)[0].T), "pv": f(peer_v)[0],
    }
    in_maps = []
    for core in range(8):
        b, c = core // 4, core % 4
        own_start = 16 + 1024 * c
        pos = own_start - CTX + np.arange(NSLOT)
        xin = np.zeros((NSLOT, D), np.float32)
        seq = np.concatenate([meta, x[b]], axis=0)
        ok = (pos >= 0)
        xin[ok] = seq[pos[ok]]
        vmask = ok.astype(np.float32)
        m = dict(common)
        m["xin"] = xin
        m["tabs"] = _tables(pos)
        m["valid"] = np.ascontiguousarray(vmask.reshape(NT, 128).T)
        m["padb"] = np.where(ok[:CTX], 0.0, NEG).astype(np.float32)
        in_maps.append(m)
    key = (tuple(_phases), _dbg, _gsel)
    if key not in _NC_CACHE:
        _NC_CACHE[key] = build_program(_phases, _dbg, _gsel)
    nc, used = _NC_CACHE[key]
    in_maps = [{n: m[n] for n in used} for m in in_maps][:_ncores]
    res = run_bass_kernel_spmd(nc, in_maps, core_ids=list(range(_ncores)), **({'trace': True} if _trace else {}))
    out = np.zeros((2, 4096, D), np.float32)
    for core in range(_ncores):
        b, c = core // 4, core % 4
        out[b, c * 1024:(c + 1) * 1024] = res.results[core]["y"]
    if _dbg:
        return out, res
    return out
```

```python
import math
import numpy as np
from contextlib import ExitStack
import concourse.bass as bass
import concourse.mybir as mybir
from concourse.bass_utils import run_bass_kernel_spmd

F32 = mybir.dt.float32
BF16 = mybir.dt.bfloat16
U8 = mybir.dt.uint8
AF = mybir.ActivationFunctionType
ALU = mybir.AluOpType

N_DMA_SEMS = 24
D = 2048
NT_CTX = 25
NT_OWN = 8
NT = NT_CTX + NT_OWN
NSLOT = NT * 128
CTX = NT_CTX * 128
EPS = 1e-6
O_Q, O_K, O_V, O_QI, O_KI, O_WI, O_Z, O_XS, O_B, O_C, O_DT, O_G = 0, 2048, 2560, 3072, 4096, 4160, 4176, 8272, 12368, 13392, 14416, 14480
R_Q, R_K, R_QI, R_KI = 0, 2048, 2560, 3584
NEG = -1.0e30
NEC = 32


class KB:
    ENGS = ("pe", "act", "dve", "pool", "sp")

    def __init__(self, nc):
        self.nc = nc
        self.es = ExitStack()
        self.ops = {e: [] for e in self.ENGS}
        self.sem = {e: self.es.enter_context(nc.semaphore("s_" + e)) for e in self.ENGS}
        self.cnt = {e: 0 for e in self.ENGS}
        self.dsem = [self.es.enter_context(nc.semaphore("s_dma%d" % i)) for i in range(N_DMA_SEMS)]
        self.dcnt = [0] * N_DMA_SEMS
        self.dnext = 0
        self.seen = {e: {} for e in self.ENGS}
        self.res = {}
        self.semobj = {}
        for e in self.ENGS:
            self.semobj[("e", e)] = self.sem[e]
        for i in range(N_DMA_SEMS):
            self.semobj[("d", i)] = self.dsem[i]
        self.arena = self.es.enter_context(nc.sbuf_tensor("arena", [128, 204 * 1024], U8))
        self.aoff = 0
        self.psum = [self.es.enter_context(nc.psum_tensor("psb%d" % i, [128, 2048], F32)) for i in range(2)]

    def alloc(self, shape, dt):
        esz = 4 if dt == F32 else 2
        n = int(np.prod(shape[1:]))
        nb = (n * esz + 63) // 64 * 64
        o = self.aoff
        self.aoff += nb
        assert self.aoff <= 204 * 1024, "SBUF arena overflow %d" % self.aoff
        v = self.arena[:, o:o + n * esz].bitcast(dt)
        if len(shape) == 3:
            v = v.rearrange("p (a b) -> p a b", b=shape[2])
        elif len(shape) == 4:
            v = v.rearrange("p (a b c) -> p a b c", b=shape[2], c=shape[3])
        if shape[0] < 128:
            v = v[0:shape[0]]
        return v

    def view(self, off, shape, dt):
        save = self.aoff
        self.aoff = off
        v = self.alloc(shape, dt)
        self.aoff = save
        return v

    def barrier(self):
        toks = [(("e", e), self.cnt[e]) for e in self.ENGS if self.cnt[e] > 0]
        toks += [(("d", i), self.dcnt[i]) for i in range(N_DMA_SEMS) if self.dcnt[i] > 0]
        for e in self.ENGS:
            w = []
            for kk, v in toks:
                if kk == ("e", e) or self.seen[e].get(kk, 0) >= v:
                    continue
                self.seen[e][kk] = v
                w.append((kk, v))
            if w:
                self.ops[e].append((w, None, None))

    def bank(self, i, dt=F32):
        t = self.psum[i // 4]
        v = t[:, (i % 4) * 512:(i % 4 + 1) * 512]
        if dt == BF16:
            v = v.bitcast(BF16)
        return v

    def _deps(self, eng, reads, writes):
        need = {}

        def add(tok):
            if tok is None:
                return
            k, v = tok
            if need.get(k, 0) < v:
                need[k] = v

        for r in reads:
            st = self.res.get(r)
            if st is not None:
                add(st[0])
        for w in writes:
            st = self.res.get(w)
            if st is not None:
                add(st[0])
                for k, v in st[1].items():
                    add((k, v))
        waits = []
        for k, v in need.items():
            if k == ("e", "pe") and eng == "pe":
                continue
            if self.seen[eng].get(k, 0) >= v:
                continue
            self.seen[eng][k] = v
            waits.append((k, v))
        return waits

    def _commit(self, tok, reads, writes):
        k, v = tok
        for r in reads:
            st = self.res.setdefault(r, [None, {}])
            if st[1].get(k, 0) < v:
                st[1][k] = v
        for w in writes:
            self.res[w] = [tok, {}]

    @staticmethod
    def _excl(reads, writes):
        ps = [r for r in reads if len(r) == 2 and r[0] == "b" and r[1].isdigit()]
        return (reads, list(writes) + ps) if ps else (reads, writes)

    def op(self, eng, fn, reads=(), writes=()):
        reads, writes = self._excl(reads, writes)
        waits = self._deps(eng, reads, writes)
        self.cnt[eng] += 1
        tok = (("e", eng), self.cnt[eng])
        self.ops[eng].append((waits, fn, (("e", eng), 1)))
        self._commit(tok, reads, writes)
        return tok

    def dma(self, eng, out, in_, reads=(), writes=()):
        i = self.dnext
        self.dnext = (self.dnext + 1) % N_DMA_SEMS
        waits = self._deps(eng, reads, writes)
        prev = self.dcnt[i]
        k = ("d", i)
        if prev > 0 and self.seen[eng].get(k, 0) < prev:
            self.seen[eng][k] = prev
            waits.append((k, prev))
        self.dcnt[i] += 16
        tok = (k, self.dcnt[i])
        self.ops[eng].append((waits, lambda e: e.dma_start(out=out, in_=in_), (k, 16)))
        self._commit(tok, reads, writes)
        return tok

    def wait_all(self, eng, toks):
        self.ops[eng].append((list(toks), None, None))

    def mm(self, out, lhsT, rhs, start, stop, reads, writes):
        return self.op("pe", lambda e: e.matmul(out, lhsT, rhs, start=start, stop=stop), reads, writes)

    def tr(self, out, in_, ident, reads, writes):
        return self.op("pe", lambda e: e.transpose(out, in_, ident), reads, writes)

    def act(self, out, in_, func, reads, writes, bias=None, scale=None, accum_out=None):
        kw = {}
        if bias is not None:
            kw["bias"] = bias
        if scale is not None:
            kw["scale"] = scale
        if accum_out is not None:
            kw["accum_out"] = accum_out
        return self.op("act", lambda e: e.activation(out, in_, func, **kw), reads, writes)

    def tt(self, eng, out, in0, in1, op, reads, writes):
        return self.op(eng, lambda e: e.tensor_tensor(out=out, in0=in0, in1=in1, op=op), reads, writes)

    def ts(self, eng, out, in0, s1, op0, reads, writes, s2=None, op1=None, accum_out=None):
        kw = {}
        if op1 is not None:
            kw["op1"] = op1
        if accum_out is not None:
            kw["accum_out"] = accum_out
        return self.op(eng, lambda e: e.tensor_scalar(out=out, in0=in0, scalar1=s1, scalar2=s2, op0=op0, **kw), reads, writes)

    def stt(self, eng, out, in0, scalar, in1, op0, op1, reads, writes, accum_out=None):
        kw = {}
        if accum_out is not None:
            kw["accum_out"] = accum_out
        return self.op(eng, lambda e: e.scalar_tensor_tensor(out=out, in0=in0, scalar=scalar, in1=in1, op0=op0, op1=op1, **kw), reads, writes)

    def cp(self, eng, out, in_, reads, writes):
        if eng == "act":
            return self.op("act", lambda e: e.copy(out, in_), reads, writes)
        return self.op(eng, lambda e: e.tensor_copy(out, in_), reads, writes)

    def memset(self, eng, ap, val, writes):
        return self.op(eng, lambda e: e.memset(ap, val), (), writes)

    def emit(self):
        nc = self.nc
        engmap = {"pe": "tensor", "act": "scalar", "dve": "vector", "pool": "gpsimd", "sp": "sync"}
        with nc.Block() as block:
            for en in self.ENGS:
                ops = self.ops[en]

                def body(engobj, ops=ops):
                    for waits, fn, inc in ops:
                        for k, v in waits:
                            engobj.wait_ge(self.semobj[k], v)
                        if fn is not None:
                            ins = fn(engobj)
                            ins.then_inc(self.semobj[inc[0]], inc[1])

                getattr(block, engmap[en])(body)
        self.es.close()


DBG_STOP = [None]


class _Stop(Exception):
    pass


def ck(n):
    if DBG_STOP[0] == n:
        raise _Stop()


def bc(ap, axis, shape):
    return ap.unsqueeze(axis).to_broadcast(list(shape))


def build_program(phases=(1, 2, 3, 4, 5), dbg=False, gsel=None):
    nc = bass.Bass("TRN2", target_bir_lowering=False)
    st = {}
    try:
        _build_body(nc, phases, dbg, gsel, st)
    except _Stop:
        pass
    k = st["k"]
    k.wait_all("sp", st["out_toks"])
    k.emit()
    return nc, st["used"]


def _build_body(nc, phases, dbg, gsel, st):
    used_inputs = []
    NEEDS = {"xin": (1, 2, 3, 4), "tabs": (2, 3), "valid": (1,), "padb": (3,), "cst": (1, 2, 3, 4, 5), "w_in": (1, 2, 3, 4),
             "w_rot": (2, 3), "convw": (1,), "convb": (1,), "dtb": (1,), "alog": (1,), "dsk": (1,), "snw": (1,),
             "nmix": (1, 2, 3, 4), "nffn": (5,), "nfin": (5,), "w_pa": (4,), "w_ps": (4,), "w_o": (4,), "w_pq": (5,),
             "keysT": (5,), "uT": (5,), "pv": (5,)}

    def din(name, shape, dt=F32):
        if not any(p in phases for p in NEEDS[name]):
            return None
        used_inputs.append(name)
        return nc.dram_tensor(name, list(shape), dt, kind="ExternalInput").ap()

    xin = din("xin", [NSLOT, D])
    tabs = din("tabs", [4, 128, NSLOT])
    valid = din("valid", [128, NT])
    padb = din("padb", [CTX])
    cst = din("cst", [128, 5, 128])
    w_in = din("w_in", [D, 18576])
    w_rot = din("w_rot", [D, 3648])
    convw = din("convw", [128, 48, 4])
    convb = din("convb", [128, 48])
    dtb = din("dtb", [64])
    alog = din("alog", [64])
    dsk = din("dsk", [64])
    snw = din("snw", [128, 32])
    nmix = din("nmix", [128, 16])
    nffn = din("nffn", [128, 16])
    nfin = din("nfin", [D])
    w_pa = din("w_pa", [2048, 2048])
    w_ps = din("w_ps", [4096, 2048])
    w_o = din("w_o", [2048, 2048])
    w_pq = din("w_pq", [2048, 2048])
    keysT = din("keysT", [128, 2, 128])
    uT = din("uT", [2048, 16384])
    pv = din("pv", [16384, 2048])
    yout = nc.dram_tensor("y", [NT_OWN * 128, D], F32, kind="ExternalOutput").ap()
    st_attn = nc.dram_tensor("st_attn", [128, 16, NT_OWN * 128], BF16).ap()
    st_ssm = nc.dram_tensor("st_ssm", [128, 32, NT_OWN * 128], BF16).ap()
    dbg_out = {}
    if dbg:
        dbg_out["d_ssm"] = nc.dram_tensor("d_ssm", [128, 32, NT_OWN * 128], BF16, kind="ExternalOutput").ap()
        dbg_out["d_attn"] = nc.dram_tensor("d_attn", [128, 16, NT_OWN * 128], BF16, kind="ExternalOutput").ap()
        dbg_out["d_h2"] = nc.dram_tensor("d_h2", [128, NT_OWN, D], F32, kind="ExternalOutput").ap()
        dbg_out["d_score"] = nc.dram_tensor("d_score", [128, NSLOT], F32, kind="ExternalOutput").ap()
        dbg_out["d_mask"] = nc.dram_tensor("d_mask", [128, NSLOT], BF16, kind="ExternalOutput").ap()
        dbg_out["d_thr"] = nc.dram_tensor("d_thr", [128, 8], F32, kind="ExternalOutput").ap()
        dbg_out["d_po"] = nc.dram_tensor("d_po", [128, 1024], F32, kind="ExternalOutput").ap()
        dbg_out["d_q"] = nc.dram_tensor("d_q", [128, 16, 128], BF16, kind="ExternalOutput").ap()
        dbg_out["d_k"] = nc.dram_tensor("d_k", [128, 4, NSLOT], BF16, kind="ExternalOutput").ap()

    k = KB(nc)
    out_toks = []
    st["k"] = k
    st["out_toks"] = out_toks
    st["used"] = used_inputs

    ck(-1)
    cstf = k.alloc([128, 5, 128], F32)
    k.dma("sp", cstf, cst, writes=["cst"])
    identf, tri_le, negtri, causb, onesf = (cstf[:, i, :] for i in range(5))
    identb = k.alloc([128, 128], BF16)
    k.dma("pool", identb, cst[:, 0, :], writes=["identb"])
    ck(-2)
    nw_fm = k.alloc([128, 16], F32)
    xt = k.alloc([128, D], F32)
    hn = k.alloc([128, D], BF16)
    sq = k.alloc([128, 4], F32)
    hnT_off = k.aoff
    hnT = k.alloc([128, 16, 512], BF16)
    wb = [k.alloc([128, 16 * 512], BF16) for _ in range(2)]
    wsel = [0]

    def load_w(dram, K, c0, ncols, buf=None):
        if buf is None:
            i = wsel[0]
            wsel[0] ^= 1
        else:
            i = buf
        kt = K // 128
        v = wb[i][:, 0:kt * ncols].rearrange("p (a b) -> p a b", b=ncols)
        k.dma("pool", v, dram[:, c0:c0 + ncols].rearrange("(a p) c -> p a c", p=128), writes=["wb%d" % i])
        return v, "wb%d" % i

    def make_hnT(src_tiles, keys=None, dst=None, dkey="hnT"):
        dst = hnT if dst is None else dst
        for j, src in enumerate(src_tiles):
            if keys is None:
                k.dma("sp", xt, src, writes=["xt"])
                xsrc, xkey = xt, "xt"
            else:
                xsrc, xkey = src, keys[j]
            k.memset("pool", sq[:, 0:1], 0.0, ["sq0"])
            k.act(hn, xsrc, AF.Square, [xkey, "sq0"], ["hn", "sq0"], accum_out=sq[:, 0:1])
            k.ts("dve", sq[:, 1:2], sq[:, 0:1], 1.0 / D, ALU.mult, ["sq0"], ["sq1"], s2=EPS, op1=ALU.add)
            k.act(sq[:, 2:3], sq[:, 1:2], AF.Sqrt, ["sq1"], ["sq2"])
            k.op("dve", lambda e: e.reciprocal(sq[:, 3:4], sq[:, 2:3]), ["sq2"], ["sq3"])
            k.ts("dve", hn, xsrc, sq[:, 3:4], ALU.mult, [xkey, "sq3", "hn"], ["hn"])
            ck(11)
            pT = k.psum[1][:, 0:1024].bitcast(BF16)
            for kt in range(16):
                k.tr(pT[:, kt * 128:(kt + 1) * 128], hn[:, kt * 128:(kt + 1) * 128], identb, ["hn", "identb"], ["b4", "b5"])
            ck(12)
            k.tt("dve", dst[:, :, j * 128:(j + 1) * 128], pT.rearrange("p (a b) -> p a b", b=128),
                 bc(nw_fm, 2, [128, 16, 128]), ALU.mult, ["b4", "b5", "nw"], [dkey])

    def proj_fm(wv, wkey, cc, N, bank, nk=16, rhs=None, rkey="hnT", start=True, stop=True):
        ps = k.bank(bank)[:, 0:N]
        r = hnT if rhs is None else rhs
        for kt in range(nk):
            k.mm(ps, wv[:, kt, cc * 128:(cc + 1) * 128], r[:, kt, 0:N], start and kt == 0, stop and kt == nk - 1,
                 [wkey, rkey], ["b%d" % bank])
        return ps

    groups = [[0]] + [list(range(1 + 4 * i, 5 + 4 * i)) for i in range(8)]
    gsel = tuple(range(9)) if gsel is None else tuple(gsel)

    mark0 = k.aoff
    if 1 in phases:
        k.dma("sp", nw_fm, nmix, writes=["nw"])
        cw = k.alloc([128, 48, 4], F32)
        cb = k.alloc([128, 48], F32)
        k.dma("sp", cw, convw, writes=["cw"])
        k.dma("sp", cb, convb, writes=["cw"])
        snwf = k.alloc([128, 32], F32)
        k.dma("sp", snwf, snw, writes=["snw"])
        validf = k.alloc([128, NT], F32)
        k.dma("sp", validf, valid, writes=["valid"])
        ck(-3)
        dtb_r = k.alloc([128, 64], F32)
        A_r = k.alloc([128, 64], F32)
        dsk_r = k.alloc([128, 64], F32)
        k.dma("sp", dtb_r, dtb.partition_broadcast(128), writes=["dtb"])
        k.dma("sp", A_r, alog.partition_broadcast(128), writes=["A"])
        k.dma("sp", dsk_r, dsk.partition_broadcast(128), writes=["dsk"])
        k.act(A_r, A_r, AF.Exp, ["A"], ["A"])
        k.ts("dve", A_r, A_r, -1.0, ALU.mult, ["A"], ["A"])
        ck(-4)
        hist = k.alloc([128, 48, 3], F32)
        k.memset("pool", hist, 0.0, ["hist"])
        H = k.alloc([128, 4096], F32)
        k.memset("pool", H, 0.0, ["H"])
        Hb = k.alloc([128, 512], BF16)
        ext_ = [k.alloc([128, 515], F32) for _ in range(2)]
        acc_ = [k.alloc([128, 512], F32) for _ in range(2)]
        xc_ = [k.alloc([128, 512], BF16) for _ in range(2)]
        cvsel = [0]
        Xdt = k.alloc([128, 4, 512], BF16)
        Xdec = k.alloc([128, 4, 512], BF16)
        XD = k.alloc([128, 4, 512], F32)
        Btm = k.alloc([128, 4, 128], BF16)
        BT = k.alloc([128, 512], BF16)
        CT = k.alloc([128, 512], BF16)
        sz = k.alloc([128, 4, 512], BF16)
        dtv = k.alloc([128, 4, 64], F32)
        av = k.alloc([128, 4, 64], F32)
        acs = k.alloc([128, 4, 64], F32)
        nacs = k.alloc([128, 4, 64], F32)
        edec = k.alloc([128, 4, 64], F32)
        wst = k.alloc([128, 4, 64], F32)
        eav = k.alloc([128, 4, 64], F32)
        tmp64 = k.alloc([128, 64], F32)
        AT_ = [k.alloc([128, 8, 128], F32) for _ in range(2)]
        LT_ = [k.alloc([128, 8, 128], BF16) for _ in range(2)]
        MT_ = [k.alloc([128, 8, 128], BF16) for _ in range(2)]
        Hb_ = [k.alloc([128, 512], BF16) for _ in range(4)]
        yv = k.alloc([128, 512], F32)
        ynb = k.alloc([128, 512], BF16)
        ysq = k.alloc([128, 4], F32)
        sst = k.alloc([128, 4, 128], BF16)
        ck(-5)
        wdt = k.alloc([128, 16, 64], BF16)
        k.dma("pool", wdt, w_in[:, O_DT:O_DT + 64].rearrange("(a p) c -> p a c", p=128), writes=["wdt"])
        ck(-6)
        negtri4 = k.alloc([128, 4, 128], F32)
        for i in range(4):
            k.cp("dve", negtri4[:, i, :], negtri, ["cst"], ["negtri4"])

        def conv_chunk(ps, N, ch, outbf, okey, first_tile_reads):
            cv = cvsel[0]
            cvsel[0] ^= 1
            ext, acc, ek, ak = ext_[cv], acc_[cv], "ext%d" % cv, "acc%d" % cv
            k.cp("dve", ext[:, 0:3], hist[:, ch, :], ["hist"], [ek])
            k.cp("act", ext[:, 3:3 + N], ps, first_tile_reads, [ek])
            k.cp("dve", hist[:, ch, :], ext[:, N:N + 3], [ek], ["hist"])
            k.ts("dve", acc[:, 0:N], ext[:, 3:3 + N], cw[:, ch, 3:4], ALU.mult, [ek, "cw"], [ak], s2=cb[:, ch:ch + 1], op1=ALU.add)
            for tap in range(3):
                k.stt("dve", acc[:, 0:N], ext[:, tap:tap + N], cw[:, ch, tap:tap + 1], acc[:, 0:N], ALU.mult, ALU.add, [ek, "cw", ak], [ak])
            k.act(outbf, acc[:, 0:N], AF.Silu, [ak], [okey])

        ck(1)
        for gi, tiles in enumerate(groups):
            if gi not in gsel:
                continue
            own = gi >= 7
            needC = gi >= 6
            nt = len(tiles)
            N = 128 * nt
            make_hnT([xin[t * 128:(t + 1) * 128, :] for t in tiles])
            ck(2)
            for j, t in enumerate(tiles):
                ps = k.bank(6)[:, 0:64]
                for kt in range(16):
                    k.mm(ps, hnT[:, kt, j * 128:(j + 1) * 128], wdt[:, kt, :], kt == 0, kt == 15, ["hnT", "wdt"], ["b6"])
                k.tt("dve", tmp64, ps, dtb_r, ALU.add, ["b6", "dtb"], ["tmp64"])
                ck(31)
                k.act(tmp64, tmp64, AF.Exp, ["tmp64"], ["tmp64"])
                k.act(tmp64, tmp64, AF.Ln, ["tmp64"], ["tmp64"], bias=1.0)
                ck(32)
                k.ts("dve", dtv[:, j, :], tmp64, validf[:, t:t + 1], ALU.mult, ["tmp64", "valid"], ["dtv"])
                k.tt("dve", av[:, j, :], dtv[:, j, :], A_r, ALU.mult, ["dtv", "A"], ["av"])
                ps2 = k.bank(6)[:, 64:128]
                k.mm(ps2, tri_le, av[:, j, :], True, True, ["cst", "av"], ["b6"])
                ps3 = k.bank(6)[:, 128:192]
                k.mm(ps3, onesf, av[:, j, :], True, True, ["cst", "av"], ["b6"])
                ck(33)
                k.cp("dve", acs[:, j, :], ps2, ["b6"], ["acs"])
                ck(331)
                k.ts("dve", nacs[:, j, :], ps2, -1.0, ALU.mult, ["b6"], ["nacs"])
                ck(332)
                k.act(edec[:, j, :], ps3, AF.Exp, ["b6"], ["edec"])
                ck(333)
                k.tt("dve", tmp64, ps3, acs[:, j, :], ALU.subtract, ["b6", "acs"], ["tmp64"])
                ck(334)
                k.act(tmp64, tmp64, AF.Exp, ["tmp64"], ["tmp64"])
                ck(335)
                k.tt("dve", wst[:, j, :], tmp64, dtv[:, j, :], ALU.mult, ["tmp64", "dtv"], ["wst"])
                ck(34)
                if own:
                    k.act(eav[:, j, :], acs[:, j, :], AF.Exp, ["acs"], ["eav"])
            ck(3)
            for g in range(8):
                hs = slice(g * 8, (g + 1) * 8)
                wv, wkey = load_w(w_in, D, O_XS + g * 512, 512, buf=0)
                wvb = wb[1][:, 0:16 * 256].rearrange("p (a b) -> p a b", b=256)
                k.dma("pool", wvb[:, :, 0:128], w_in[:, O_B + g * 128:O_B + (g + 1) * 128].rearrange("(a p) c -> p a c", p=128), writes=["wb1"])
                if needC:
                    k.dma("pool", wvb[:, :, 128:256], w_in[:, O_C + g * 128:O_C + (g + 1) * 128].rearrange("(a p) c -> p a c", p=128), writes=["wb1"])
                pTX = k.psum[0][:, 1024:2048].bitcast(BF16).rearrange("p (a b) -> p a b", b=512)
                pTB = k.bank(2, BF16)
                chunks = [("xs", cc) for cc in range(4)] + [("B", 0)] + ([("C", 1)] if needC else [])

                def do_proj(ch, wv=wv, wkey=wkey, wvb=wvb):
                    kind, i = ch
                    if kind == "xs":
                        return proj_fm(wv, wkey, i, N, i % 2)
                    return proj_fm(wvb, "wb1", i, N, i % 2)

                def do_post(ch, ps, g=g, hs=hs):
                    kind, i = ch
                    if kind == "xs":
                        xc, xck = xc_[i % 2], "xc%d" % (i % 2)
                        conv_chunk(ps, N, g * 4 + i, xc[:, 0:N], xck, ["b%d" % (i % 2)])
                        for j in range(nt):
                            k.tr(pTX[:, j, i * 128:(i + 1) * 128], xc[:, j * 128:(j + 1) * 128], identb, [xck, "identb"], ["b2", "b3"])
                        if i == 3:
                            for j in range(nt):
                                src = pTX[:, j, :].rearrange("p (h d) -> p h d", d=64)
                                k.tt("dve", Xdt[:, j, :].rearrange("p (h d) -> p h d", d=64), src, bc(dtv[:, j, hs], 2, [128, 8, 64]), ALU.mult, ["b2", "b3", "dtv"], ["Xdt"])
                                k.tt("dve", Xdec[:, j, :].rearrange("p (h d) -> p h d", d=64), src, bc(wst[:, j, hs], 2, [128, 8, 64]), ALU.mult, ["b2", "b3", "wst"], ["Xdec"])
                                if own:
                                    k.tt("dve", XD[:, j, :].rearrange("p (h d) -> p h d", d=64), src, bc(dsk_r[:, hs], 2, [128, 8, 64]), ALU.mult, ["b2", "b3", "dsk"], ["XD"])
                    elif kind == "B":
                        conv_chunk(ps, N, 32 + g, BT[:, 0:N], "BT", ["b0"])
                        for j in range(nt):
                            k.tr(pTB[:, j * 128:(j + 1) * 128], BT[:, j * 128:(j + 1) * 128], identb, ["BT", "identb"], ["b2", "b3"])
                        k.cp("act", Btm[:, 0:nt, :], pTB[:, 0:N].rearrange("p (a b) -> p a b", b=128), ["b2", "b3"], ["Btm"])
                    else:
                        conv_chunk(ps, N, 40 + g, CT[:, 0:N], "CT", ["b1"])

                pending = None
                for ch in chunks:
                    ps = do_proj(ch)
                    if pending is not None:
                        do_post(*pending)
                    pending = (ch, ps)
                do_post(*pending)
                if own:
                    wvz, wkeyz = load_w(w_in, D, O_Z + g * 512, 512, buf=0)
                    for j in range(nt):
                        ps = k.bank(j % 2)
                        for kt in range(16):
                            k.mm(ps, hnT[:, kt, j * 128:(j + 1) * 128], wvz[:, kt, :], kt == 0, kt == 15, ["hnT", wkeyz], ["b%d" % (j % 2)])
                        k.act(sz[:, j, :], ps, AF.Silu, ["b%d" % (j % 2)], ["sz"])
                ck(7)
                Hg = H[:, g * 512:(g + 1) * 512]
                Hg3 = Hg.rearrange("p (h d) -> p h d", d=64)

                def upd(j, Hg=Hg, Hg3=Hg3, hs=hs):
                    sb_ = 6 + j % 2
                    ps_S = k.bank(sb_)
                    k.mm(ps_S, Btm[:, j, :], Xdec[:, j, :], True, True, ["Btm", "Xdec"], ["b%d" % sb_])
                    k.tt("dve", Hg3, Hg3, bc(edec[:, j, hs], 2, [128, 8, 64]), ALU.mult, ["H", "edec"], ["H"])
                    k.tt("dve", Hg, Hg, ps_S, ALU.add, ["H", "b%d" % sb_], ["H"])

                if not own:
                    for j in range(nt):
                        upd(j)
                else:
                    for j in range(nt):
                        k.cp("act", Hb_[j], Hg, ["H"], ["Hb%d" % j])
                        upd(j)

                    def front(j, g=g, hs=hs):
                        js = slice(j * 128, (j + 1) * 128)
                        jb = j % 2
                        AT, LT, MT = AT_[jb], LT_[jb], MT_[jb]
                        atk, ltk, mtk = "AT%d" % jb, "LT%d" % jb, "MT%d" % jb
                        ps_yo = k.bank(jb)
                        k.mm(ps_yo, CT[:, js], Hb_[j], True, True, ["CT", "Hb%d" % j], ["b%d" % jb])
                        ps_cb = k.bank(7)[:, 0:128]
                        k.mm(ps_cb, BT[:, js], CT[:, js], True, True, ["BT", "CT"], ["b7"])
                        k.tt("pool", AT, bc(tri_le, 1, [128, 8, 128]), bc(av[:, j, hs], 2, [128, 8, 128]), ALU.mult, ["cst", "av"], [atk])
                        psL = k.psum[1][:, 0:1024].rearrange("p (a b) -> p a b", b=128)
                        for hf in range(2):
                            k.mm(psL[:, hf * 4:(hf + 1) * 4, :], onesf, AT[:, hf * 4:(hf + 1) * 4, :], True, False, ["cst", atk], ["b%d" % (4 + hf)])
                            k.mm(psL[:, hf * 4:(hf + 1) * 4, :], identf, negtri4, False, True, ["cst", "negtri4"], ["b%d" % (4 + hf)])
                        for hh in range(8):
                            k.act(LT[:, hh, :], psL[:, hh, :], AF.Exp, ["b%d" % (4 + hh // 4), "nacs"], [ltk], bias=nacs[:, j, g * 8 + hh:g * 8 + hh + 1])
                        k.tt("dve", MT, LT, bc(ps_cb, 1, [128, 8, 128]), ALU.mult, [ltk, "b7"], [mtk])

                    def back(j, g=g, hs=hs):
                        t = tiles[j]
                        jb = j % 2
                        MT, mtk = MT_[jb], "MT%d" % jb
                        ps_yo = k.bank(jb)
                        ps_y = k.bank(6)[:, 0:512]
                        for hh in range(8):
                            k.mm(ps_y[:, hh * 64:(hh + 1) * 64], MT[:, hh, :], Xdt[:, j, hh * 64:(hh + 1) * 64], True, True, [mtk, "Xdt"], ["b6"])
                        y3 = yv.rearrange("p (h d) -> p h d", d=64)
                        k.tt("dve", y3, ps_yo.rearrange("p (h d) -> p h d", d=64), bc(eav[:, j, hs], 2, [128, 8, 64]), ALU.mult, ["b%d" % jb, "eav"], ["yv"])
                        k.tt("dve", yv, yv, ps_y, ALU.add, ["yv", "b6"], ["yv"])
                        k.tt("dve", yv, yv, XD[:, j, :], ALU.add, ["yv", "XD"], ["yv"])
                        k.tt("dve", yv, yv, sz[:, j, :], ALU.mult, ["yv", "sz"], ["yv"])
                        k.memset("pool", ysq[:, 0:1], 0.0, ["ysq0"])
                        k.act(ynb, yv, AF.Square, ["yv", "ysq0"], ["ynb", "ysq0"], accum_out=ysq[:, 0:1])
                        k.ts("dve", ysq[:, 1:2], ysq[:, 0:1], 1.0 / 512, ALU.mult, ["ysq0"], ["ysq1"], s2=EPS, op1=ALU.add)
                        k.act(ysq[:, 2:3], ysq[:, 1:2], AF.Sqrt, ["ysq1"], ["ysq2"])
                        k.op("dve", lambda e: e.reciprocal(ysq[:, 3:4], ysq[:, 2:3]), ["ysq2"], ["ysq3"])
                        k.ts("dve", ynb, yv, ysq[:, 3:4], ALU.mult, ["yv", "ysq3", "ynb"], ["ynb"])
                        pTy = k.bank(3, BF16)
                        for cc in range(4):
                            k.tr(pTy[:, cc * 128:(cc + 1) * 128], ynb[:, cc * 128:(cc + 1) * 128], identb, ["ynb", "identb"], ["b2", "b3"])
                        for cc in range(4):
                            k.act(sst[:, cc, :], pTy[:, cc * 128:(cc + 1) * 128], AF.Copy, ["b2", "b3", "snw"], ["sst"], scale=snwf[:, g * 4 + cc:g * 4 + cc + 1])
                        ot = t - NT_CTX
                        k.dma("sp", st_ssm[:, g * 4:(g + 1) * 4, ot * 128:(ot + 1) * 128], sst, reads=["sst"], writes=["st_ssm"])

                    front(0)
                    for j in range(nt):
                        if j + 1 < nt:
                            front(j + 1)
                        back(j)
                ck(8)
        if dbg:
            dtile = k.alloc([128, 32, 128], BF16)
            for ot in range(NT_OWN):
                k.dma("sp", dtile, st_ssm[:, :, ot * 128:(ot + 1) * 128], reads=["st_ssm"], writes=["dtile"])
                out_toks.append(k.dma("sp", dbg_out["d_ssm"][:, :, ot * 128:(ot + 1) * 128], dtile, reads=["dtile"]))
    k.aoff = mark0

    def rope_fm(ps_a, ps_b, tbuf, tsel, N, out, okey, akey, bkey, r1, r2):
        k.tt("dve", r1[:, 0:N], ps_a, tbuf[:, tsel, 0:N], ALU.mult, [akey, "tb"], ["r1"])
        k.tt("dve", r2[:, 0:N], ps_b, tbuf[:, tsel + 1, 0:N], ALU.mult, [bkey, "tb"], ["r2"])
        k.tt("dve", out, r1[:, 0:N], r2[:, 0:N], ALU.add, ["r1", "r2"], [okey])

    if 2 in phases:
        k.barrier()
        k.dma("sp", nw_fm, nmix, writes=["nw"])
        KT = k.alloc([128, 4, NSLOT], BF16)
        V = k.alloc([128, NT, 4, 130], BF16)
        kiT = k.alloc([128, NSLOT], BF16)
        mark2 = k.aoff
        k.memset("pool", V, 1.0, ["V"])
        tb = k.alloc([128, 4, 512], F32)
        r1 = k.alloc([128, 512], F32)
        r2 = k.alloc([128, 512], F32)
        for gi, tiles in enumerate(groups):
            if gi not in gsel:
                continue
            nt = len(tiles)
            N = 128 * nt
            s0 = tiles[0] * 128
            make_hnT([xin[t * 128:(t + 1) * 128, :] for t in tiles])
            k.dma("sp", tb[:, :, 0:N], tabs[:, :, s0:s0 + N].rearrange("a p s -> p a s"), writes=["tb"])
            wv, wkey = load_w(w_in, D, O_K, 512, buf=0)
            wr, rkey = load_w(w_rot, D, R_K, 512, buf=1)
            for cc in range(4):
                pa = proj_fm(wv, wkey, cc, N, 0)
                pb = proj_fm(wr, rkey, cc, N, 1)
                rope_fm(pa, pb, tb, 0, N, KT[:, cc, s0:s0 + N], "KT", "b0", "b1", r1, r2)
            wki = wb[0][:, 0:16 * 256].rearrange("p (a b) -> p a b", b=256)
            for half in range(2):
                k.dma("pool", wki[:, :, half * 64:(half + 1) * 64], w_in[:, O_KI:O_KI + 64].rearrange("(a p) c -> p a c", p=128), writes=["wb0"])
                k.dma("pool", wki[:, :, 128 + half * 64:128 + (half + 1) * 64], w_rot[:, R_KI:R_KI + 64].rearrange("(a p) c -> p a c", p=128), writes=["wb0"])
            pa = proj_fm(wki, "wb0", 0, N, 0)
            pb = proj_fm(wki, "wb0", 1, N, 1)
            rope_fm(pa, pb, tb, 2, N, kiT[:, s0:s0 + N], "kiT", "b0", "b1", r1, r2)
            wvv, wkeyv = load_w(w_in, D, O_V, 512, buf=1)
            for j, t in enumerate(tiles):
                ps = k.bank(2 + j % 2)
                for kt in range(16):
                    k.mm(ps, hnT[:, kt, j * 128:(j + 1) * 128], wvv[:, kt, :], kt == 0, kt == 15, ["hnT", wkeyv], ["b%d" % (2 + j % 2)])
                k.cp("act", V[:, t, :, 0:128], ps.rearrange("p (g d) -> p g d", d=128), ["b%d" % (2 + j % 2)], ["V"])
        k.aoff = mark2

    if 3 in phases:
        k.barrier()
        MB = k.view(hnT_off, [128, NT, 128], BF16)
        tb3 = k.view(hnT_off + 8448 + 4096, [128, 4, 128], F32)
        hnT3 = k.alloc([128, 16, 128], BF16)
        padb_r = k.alloc([128, CTX], BF16)
        k.dma("pool", padb_r, padb.partition_broadcast(128), writes=["padb"])
        r1 = k.alloc([128, 128], F32)
        r2 = k.alloc([128, 128], F32)
        qT_ = [k.alloc([128, 16, 128], BF16), k.view(hnT_off + 8448, [128, 16, 128], BF16)]
        qiT = k.alloc([128, 8, 128], BF16)
        wis = k.alloc([128, 16], F32)
        score = k.alloc([128, NSLOT], F32)
        wm_off = k.aoff
        work = k.alloc([128, NSLOT], F32)
        maskb = k.view(wm_off, [128, NSLOT], BF16)
        maskT = k.view(wm_off + NSLOT * 2, [128, NT, 128], BF16)
        rl_ = [k.alloc([128, 512], F32) for _ in range(2)]
        m8 = k.alloc([128, 8], F32)
        thr = k.alloc([128, 1], F32)
        Et_ = [k.alloc([128, 4, 128], BF16) for _ in range(2)]
        ao = k.alloc([128, 16, 128], BF16)
        rs = k.alloc([128, 4], F32)
        aT = k.alloc([128, 16, 128], BF16)
        wwi = k.alloc([128, 16, 16], BF16)
        k.dma("pool", wwi, w_in[:, O_WI:O_WI + 16].rearrange("(a p) c -> p a c", p=128), writes=["wwi"])

        def A1(t):
            ot = t - NT_CTX
            qT, qk = qT_[ot % 2], "qT%d" % (ot % 2)
            N = 128
            s0 = t * 128
            make_hnT([xin[t * 128:(t + 1) * 128, :]], dst=hnT3, dkey="hnT3")
            k.dma("sp", tb3, tabs[:, :, s0:s0 + N].rearrange("a p s -> p a s"), writes=["tb"])
            for c4 in range(4):
                wv, wkey = load_w(w_in, D, O_Q + c4 * 512, 512, buf=0)
                wr, rkey = load_w(w_rot, D, R_Q + c4 * 512, 512, buf=1)
                for cc in range(4):
                    pa = proj_fm(wv, wkey, cc, N, 0, rhs=hnT3, rkey="hnT3")
                    pb = proj_fm(wr, rkey, cc, N, 1, rhs=hnT3, rkey="hnT3")
                    rope_fm(pa, pb, tb3, 0, N, qT[:, c4 * 4 + cc, :], qk, "b0", "b1", r1, r2)
            for c4 in range(2):
                wv, wkey = load_w(w_in, D, O_QI + c4 * 512, 512, buf=0)
                wr, rkey = load_w(w_rot, D, R_QI + c4 * 512, 512, buf=1)
                for cc in range(4):
                    pa = proj_fm(wv, wkey, cc, N, 0, rhs=hnT3, rkey="hnT3")
                    pb = proj_fm(wr, rkey, cc, N, 1, rhs=hnT3, rkey="hnT3")
                    rope_fm(pa, pb, tb3, 2, N, qiT[:, c4 * 4 + cc, :], "qiT", "b0", "b1", r1, r2)
            ps = k.bank(2)[:, 0:16]
            for kt in range(16):
                k.mm(ps, hnT3[:, kt, 0:128], wwi[:, kt, :], kt == 0, kt == 15, ["hnT3", "wwi"], ["b2"])
            k.ts("dve", wis, ps, 1.0 / 32.0, ALU.mult, ["b2"], ["wis"])
            nk = t + 1
            S = nk * 128
            chunks = [(c0, min(512, CTX - c0)) for c0 in range(0, CTX, 512)] + [(c0, min(512, S - c0)) for c0 in range(CTX, S, 512)]
            for ci, (c0, n) in enumerate(chunks):
                for h in range(16):
                    hp = slice((h % 2) * 64, (h % 2) * 64 + 64)
                    bk = 2 + (h % 2)
                    rl, rlk = rl_[h % 2], "rl%d" % (h % 2)
                    ps = k.bank(bk)[:, 0:n]
                    k.mm(ps, qiT[hp, h // 2, :], kiT[hp, c0:c0 + n], True, True, ["qiT", "kiT"], ["b%d" % bk])
                    k.act(rl[:, 0:n], ps, AF.Relu, ["b%d" % bk], [rlk])
                    if h == 0:
                        if c0 < CTX:
                            k.stt("dve", score[:, c0:c0 + n], rl[:, 0:n], wis[:, 0:1], padb_r[:, c0:c0 + n], ALU.mult, ALU.add, [rlk, "wis", "padb"], ["score"])
                        else:
                            k.ts("dve", score[:, c0:c0 + n], rl[:, 0:n], wis[:, 0:1], ALU.mult, [rlk, "wis"], ["score"])
                    else:
                        k.stt("dve", score[:, c0:c0 + n], rl[:, 0:n], wis[:, h:h + 1], score[:, c0:c0 + n], ALU.mult, ALU.add, [rlk, "wis", "score"], ["score"])
            k.tt("dve", score[:, S - 128:S], score[:, S - 128:S], causb, ALU.add, ["score", "cst"], ["score"])
            if dbg and t == NT_CTX:
                out_toks.append(k.dma("sp", dbg_out["d_score"], score, reads=["score"]))

        def TK(t):
            S = (t + 1) * 128
            cur, ckey = score, "score"
            for r in range(32):
                k.op("dve", lambda e, cur=cur, S=S: e.max(out=m8, in_=cur[:, 0:S]), [ckey], ["m8"])
                if r < 31:
                    k.op("dve", lambda e, cur=cur, S=S: e.match_replace(out=work[:, 0:S], in_to_replace=m8, in_values=cur[:, 0:S], imm_value=NEG),
                         [ckey, "m8", "wm"], ["wm"])
                    cur, ckey = work, "wm"
            k.ts("dve", thr, m8[:, 7:8], -1.0e29, ALU.max, ["m8"], ["thr"])
            k.ts("dve", maskb[:, 0:S], score[:, 0:S], thr, ALU.is_ge, ["score", "thr", "wm"], ["wm"])
            if dbg and t == NT_CTX:
                out_toks.append(k.dma("sp", dbg_out["d_mask"], maskb, reads=["wm"]))
                out_toks.append(k.dma("sp", dbg_out["d_thr"], m8, reads=["m8"]))

        def A2(t):
            nk = t + 1
            for kt0 in range(0, nk, 8):
                nb = min(8, nk - kt0)
                pTm = k.bank(4, BF16)
                for i2 in range(nb):
                    k.tr(pTm[:, i2 * 128:(i2 + 1) * 128], maskb[:, (kt0 + i2) * 128:(kt0 + i2 + 1) * 128], identb, ["wm", "identb"], ["b4"])
                k.cp("act", maskT[:, kt0:kt0 + nb, :], pTm[:, 0:nb * 128].rearrange("p (a b) -> p a b", b=128), ["b4", "wm"], ["wm"])
            k.ts("dve", MB[:, 0:nk, :], maskT[:, 0:nk, :], -1.0, ALU.add, ["wm"], ["MB"], s2=30000.0, op1=ALU.mult)

        def B(t):
            ot = t - NT_CTX
            qT, qk = qT_[ot % 2], "qT%d" % (ot % 2)
            nk = t + 1
            for kvg in range(4):
                po = k.psum[1][:, 1024:2048].rearrange("p (a b) -> p a b", b=256)

                def sc_stage(kt, kvg=kvg):
                    bk = kt % 2
                    Et, etk = Et_[bk], "Et%d" % bk
                    ps = k.bank(bk)
                    k.mm(ps, KT[:, kvg, kt * 128:(kt + 1) * 128], qT[:, kvg * 4:(kvg + 1) * 4, :], True, False, ["KT", qk], ["b%d" % bk])
                    for hh in range(4):
                        k.mm(ps[:, hh * 128:(hh + 1) * 128], identb, MB[:, kt, :], False, hh == 3, ["identb", "MB"], ["b%d" % bk])
                    k.act(Et, ps.rearrange("p (a b) -> p a b", b=128), AF.Exp, ["b%d" % bk], [etk], scale=1.0 / math.sqrt(128.0))

                def pv_stage(kt, kvg=kvg, po=po):
                    bk = kt % 2
                    Et, etk = Et_[bk], "Et%d" % bk
                    for hh in range(4):
                        k.mm(po[:, hh, 0:129], Et[:, hh, :], V[:, kt, kvg, 0:129], kt == 0 and hh % 2 == 0, kt == nk - 1, [etk, "V"], ["b%d" % (6 + hh // 2)])

                sc_stage(0)
                for kt in range(nk):
                    if kt + 1 < nk:
                        sc_stage(kt + 1)
                    pv_stage(kt)
                k.act(rs, po[:, :, 128], AF.Ln, ["b6", "b7"], ["rs"])
                k.act(rs, rs, AF.Exp, ["rs"], ["rs"], scale=-1.0)
                for hh in range(4):
                    k.act(ao[:, kvg * 4 + hh, :], po[:, hh, 0:128], AF.Copy, ["b%d" % (6 + hh // 2), "rs"], ["ao"], scale=rs[:, hh:hh + 1])
            pTa = k.psum[1][:, 0:1024].bitcast(BF16)
            for h in range(16):
                k.tr(pTa[:, h * 128:(h + 1) * 128], ao[:, h, :], identb, ["ao", "identb"], ["b4", "b5"])
            k.cp("act", aT, pTa.rearrange("p (a b) -> p a b", b=128), ["b4", "b5"], ["aT"])
            k.dma("sp", st_attn[:, :, ot * 128:(ot + 1) * 128], aT, reads=["aT"], writes=["st_attn"])

        sel = [t for t in range(NT_CTX, NT) if (7 if t < NT_CTX + 4 else 8) in gsel]
        prev = None
        for t in sel:
            A1(t)
            TK(t)
            if prev is not None:
                B(prev)
            A2(t)
            prev = t
        if prev is not None:
            B(prev)
        if dbg:
            for ot in range(NT_OWN):
                k.dma("sp", aT, st_attn[:, :, ot * 128:(ot + 1) * 128], reads=["st_attn"], writes=["aT"])
                out_toks.append(k.dma("sp", dbg_out["d_attn"][:, :, ot * 128:(ot + 1) * 128], aT, reads=["aT"]))
    k.aoff = mark0

    h2 = k.alloc([128, 4, D], F32)
    mark4 = k.aoff
    for gi in (7, 8):
        tiles = groups[gi]
        otiles = [t - NT_CTX for t in tiles]
        o0 = otiles[0] * 128
        if 4 in phases:
            k.barrier()
            k.aoff = mark4
            k.dma("sp", nw_fm, nmix, writes=["nw"])
            wb2 = k.alloc([128, 16 * 512], BF16)
            actT = k.alloc([128, 32, 512], BF16)
            mg = k.alloc([128, 16, 512], BF16)
            sga = k.alloc([128, 512], F32)
            t1 = k.alloc([128, 512], F32)
            make_hnT([xin[t * 128:(t + 1) * 128, :] for t in tiles])
            k.dma("sp", actT[:, 0:16, :], st_attn[:, :, o0:o0 + 512], reads=["st_attn"], writes=["actT"])
            for c4 in range(4):
                wg, gkey = load_w(w_in, D, O_G + c4 * 512, 512, buf=0)
                wv, wkey = load_w(w_pa, 2048, c4 * 512, 512, buf=1)
                for cc in range(4):
                    pg = proj_fm(wg, gkey, cc, 512, 0)
                    pa = proj_fm(wv, wkey, cc, 512, 1, rhs=actT, rkey="actT")
                    k.act(sga, pg, AF.Sigmoid, ["b0"], ["sga"])
                    k.tt("dve", mg[:, c4 * 4 + cc, :], sga, pa, ALU.mult, ["sga", "b1"], ["mg"])
            k.dma("sp", actT, st_ssm[:, :, o0:o0 + 512], reads=["st_ssm", "actT"], writes=["actT"])
            for c4 in range(4):
                wg = wb2[:, :].rearrange("p (a b) -> p a b", b=512)
                k.dma("pool", wg, w_in[:, O_G + 2048 + c4 * 512:O_G + 2048 + (c4 + 1) * 512].rearrange("(a p) c -> p a c", p=128), writes=["wb2"])
                for c2 in range(2):
                    wv, wkey = load_w(w_ps, 4096, c4 * 512 + c2 * 256, 256, buf=c2)
                    for cc in range(2):
                        col = c4 * 4 + c2 * 2 + cc
                        pg = proj_fm(wg, "wb2", c2 * 2 + cc, 512, 0)
                        pa = proj_fm(wv, wkey, cc, 512, 1, nk=32, rhs=actT, rkey="actT")
                        k.act(sga, pg, AF.Sigmoid, ["b0"], ["sga"])
                        k.tt("dve", t1, sga, pa, ALU.mult, ["sga", "b1"], ["t1"])
                        k.tt("dve", mg[:, col, :], mg[:, col, :], t1, ALU.add, ["mg", "t1"], ["mg"])
            for c4 in range(4):
                wv, wkey = load_w(w_o, 2048, c4 * 512, 512, buf=c4 % 2)
                for j, t in enumerate(tiles):
                    bk = 2 + j % 2
                    ps = k.bank(bk)
                    for kt in range(16):
                        k.mm(ps, mg[:, kt, j * 128:(j + 1) * 128], wv[:, kt, :], kt == 0, kt == 15, ["mg", wkey], ["b%d" % bk])
                    if c4 == 0:
                        k.dma("sp", h2[:, j, :], xin[t * 128:(t + 1) * 128, :], writes=["h2_%d" % j])
                    k.tt("dve", h2[:, j, c4 * 512:(c4 + 1) * 512], h2[:, j, c4 * 512:(c4 + 1) * 512], ps, ALU.add, ["h2_%d" % j, "b%d" % bk], ["h2_%d" % j])
            if dbg:
                for j, ot in enumerate(otiles):
                    out_toks.append(k.dma("sp", dbg_out["d_h2"][:, ot, :], h2[:, j, :], reads=["h2_%d" % j]))
        if 5 in phases:
            k.barrier()
            k.aoff = mark4
            k.dma("sp", nw_fm, nffn, writes=["nw"])
            nfin_r = xt
            k.dma("sp", nfin_r, nfin.partition_broadcast(128), writes=["xt"])
            keysb = k.alloc([128, 2, 128], BF16)
            k.dma("pool", keysb, keysT, writes=["keysb"])
            q_off = k.aoff
            qpT = k.alloc([128, 16, 512], BF16)
            osb = k.view(q_off, [128, D], F32)
            ssc = k.alloc([128, 4, 16, 128], F32)
            top = k.alloc([128, 16, 16], F32)
            wk2 = k.alloc([128, 128], F32)
            cand = k.alloc([128, 256], F32)
            cand2 = k.alloc([128, 256], F32)
            c8 = k.alloc([128, 8], F32)
            thr5 = k.alloc([128, 4, 8], F32)
            nb5 = k.alloc([128, 4, 8], F32)
            zz = k.alloc([128, 4], F32)
            Gt_ = [k.alloc([128, 512], BF16) for _ in range(2)]
            GT_ = [k.alloc([128, 4, 128], BF16) for _ in range(2)]
            cand3 = k.alloc([128, 256], F32)
            c8b = k.alloc([128, 8], F32)
            tau5 = k.alloc([128, 4, 8], F32)
            tsum = k.alloc([128, 8], F32)
            Ex_ = [k.alloc([128, 512], F32) for _ in range(3)]
            Wh_ = [k.alloc([128, 512], BF16) for _ in range(3)]
            GWT_ = [k.alloc([128, 4, 128], BF16) for _ in range(2)]
            wb5 = [wb[0], wb[1], k.alloc([128, 16 * 512], BF16), k.alloc([128, 16 * 512], BF16)]
            make_hnT([h2[:, j, :] for j in range(4)], keys=["h2_%d" % j for j in range(4)])
            for c4 in range(4):
                wv, wkey = load_w(w_pq, 2048, c4 * 512, 512, buf=c4 % 2)
                for cc in range(4):
                    pa = proj_fm(wv, wkey, cc, 512, 0)
                    k.cp("act", qpT[:, c4 * 4 + cc, :], pa, ["b0"], ["qpT"])
            for j in range(4):
                js = slice(j * 128, (j + 1) * 128)
                for q4 in range(4):
                    bk = 2 + q4 % 2
                    ps = k.bank(bk)
                    for i2 in range(4):
                        hc = q4 * 4 + i2
                        k.mm(ps[:, i2 * 128:(i2 + 1) * 128], qpT[:, hc, js], keysb[:, hc % 2, :], True, True, ["qpT", "keysb"], ["b%d" % bk])
                    k.cp("act", ssc[:, j, q4 * 4:(q4 + 1) * 4, :], ps.rearrange("p (a b) -> p a b", b=128), ["b%d" % bk], ["ssc"])
                for hc in range(16):
                    k.op("dve", lambda e, hc=hc, j=j: e.max(out=top[:, hc, 0:8], in_=ssc[:, j, hc, :]), ["ssc"], ["top"])
                    k.op("dve", lambda e, hc=hc, j=j: e.match_replace(out=wk2, in_to_replace=top[:, hc, 0:8], in_values=ssc[:, j, hc, :], imm_value=NEG), ["ssc", "top", "wk2"], ["wk2"])
                    k.op("dve", lambda e, hc=hc: e.max(out=top[:, hc, 8:16], in_=wk2), ["wk2", "top"], ["top"])
                for h in range(8):
                    c3 = cand.rearrange("p (a b) -> p a b", b=16)
                    k.tt("dve", c3, bc(top[:, 2 * h, :], 2, [128, 16, 16]), bc(top[:, 2 * h + 1, :], 1, [128, 16, 16]), ALU.add, ["top"], ["cand"])
                    k.op("dve", lambda e: e.max(out=c8, in_=cand), ["cand"], ["c8"])
                    k.ts("dve", zz[:, 0:1], c8[:, 0:1], -1.0, ALU.mult, ["c8"], ["zz0"])
                    k.op("dve", lambda e: e.match_replace(out=cand2, in_to_replace=c8, in_values=cand, imm_value=NEG), ["cand", "c8", "cand2"], ["cand2"])
                    k.op("dve", lambda e: e.max(out=c8, in_=cand2), ["cand2", "c8"], ["c8"])
                    k.cp("dve", thr5[:, j, h:h + 1], c8[:, 7:8], ["c8"], ["thr5"])
                    k.op("dve", lambda e: e.match_replace(out=cand3, in_to_replace=c8, in_values=cand2, imm_value=NEG), ["cand2", "c8", "cand3"], ["cand3"])
                    k.op("dve", lambda e: e.max(out=c8b, in_=cand3), ["cand3", "c8b"], ["c8b"])
                    k.tt("dve", tsum[:, h:h + 1], c8[:, 7:8], c8b[:, 0:1], ALU.add, ["c8", "c8b"], ["tsum"])
                    k.act(cand3, cand, AF.Exp, ["cand", "zz0", "cand3"], ["cand3"], bias=zz[:, 0:1])
                    k.stt("dve", cand3, cand, thr5[:, j, h:h + 1], cand3, ALU.is_ge, ALU.mult, ["cand", "thr5", "cand3"], ["cand3"])
                    k.op("dve", lambda e: e.reduce_sum(out=zz[:, 1:2], in_=cand3, axis=mybir.AxisListType.X), ["cand3"], ["zz1"])
                    k.act(zz[:, 2:3], zz[:, 1:2], AF.Ln, ["zz1"], ["zz2"])
                    k.tt("dve", nb5[:, j, h:h + 1], zz[:, 0:1], zz[:, 2:3], ALU.subtract, ["zz0", "zz2"], ["nb5"])
                k.stt("dve", tau5[:, j, :], tsum, 0.5, nb5[:, j, :], ALU.mult, ALU.add, ["tsum", "nb5"], ["tau5"])
                k.act(tau5[:, j, :], tau5[:, j, :], AF.Exp, ["tau5"], ["tau5"])
                for h in range(8):
                    k.act(ssc[:, j, 2 * h, :], ssc[:, j, 2 * h, :], AF.Exp, ["ssc", "nb5"], ["ssc"], bias=nb5[:, j, h:h + 1])
                    k.act(ssc[:, j, 2 * h + 1, :], ssc[:, j, 2 * h + 1, :], AF.Exp, ["ssc"], ["ssc"])
            iters = [(ec, j) for ec in range(NEC) for j in range(4)]

            def bufs(ec):
                uv = wb5[(ec % 2) * 2][:, :].rearrange("p (a b) -> p a b", b=512)
                vv = wb5[(ec % 2) * 2 + 1][:, :].rearrange("p (a b) -> p a b", b=2048)
                ukey = "wb0" if ec % 2 == 0 else "wu1"
                vkey = "wb1" if ec % 2 == 0 else "wv1"
                return uv, vv, ukey, vkey

            def load5(ec):
                uv, vv, ukey, vkey = bufs(ec)
                k.dma("pool", uv, uT[:, ec * 512:(ec + 1) * 512].rearrange("(a p) c -> p a c", p=128), writes=[ukey])
                k.dma("pool", vv, pv[ec * 512:(ec + 1) * 512, :].rearrange("(a p) c -> p a c", p=128), writes=[vkey])

            def stageA(idx):
                ec, j = iters[idx]
                uv, vv, ukey, vkey = bufs(ec)
                js = slice(j * 128, (j + 1) * 128)
                jb = idx % 2
                Gt, GT, GWT = Gt_[jb], GT_[jb], GWT_[jb]
                gk, gtk, gwtk = "Gt%d" % jb, "GT%d" % jb, "GWT%d" % jb
                ps = k.bank(jb)
                for kt in range(16):
                    k.mm(ps, hnT[:, kt, js], uv[:, kt, :], kt == 0, kt == 15, ["hnT", ukey], ["b%d" % jb])
                k.act(Gt, ps, AF.Gelu, ["b%d" % jb], [gk])
                if idx >= 1:
                    stageB_pe(idx - 1)
                pTg = k.bank(jb, BF16)
                for b4 in range(4):
                    k.tr(pTg[:, b4 * 128:(b4 + 1) * 128], Gt[:, b4 * 128:(b4 + 1) * 128], identb, [gk, "identb"], ["b%d" % jb])
                k.cp("act", GT, pTg[:, 0:512].rearrange("p (a b) -> p a b", b=128), ["b%d" % jb], [gtk])
                pW = k.psum[0][:, 1024:2048].rearrange("p (a b) -> p a b", b=256)[:, :, 0:128]
                for h in range(8):
                    hb = (idx * 8 + h) % 3
                    Ex, Wh = Ex_[hb], Wh_[hb]
                    ek, whk = "Ex%d" % hb, "Wh%d" % hb
                    eng = "dve" if h % 4 == 3 else "pool"
                    k.tt(eng, Ex.rearrange("p (a b) -> p a b", b=128), bc(ssc[:, j, 2 * h, ec * 4:(ec + 1) * 4], 2, [128, 4, 128]),
                         bc(ssc[:, j, 2 * h + 1, :], 1, [128, 4, 128]), ALU.mult, ["ssc"], [ek])
                    k.stt("dve", Wh, Ex, tau5[:, j, h:h + 1], Ex, ALU.is_ge, ALU.mult, ["tau5", ek], [whk])
                    for b4 in range(4):
                        k.mm(pW[:, b4, :], Wh[:, b4 * 128:(b4 + 1) * 128], identb, h == 0 and b4 % 2 == 0, h == 7, [whk, "identb"], ["b%d" % (2 + b4 // 2)])
                k.tt("dve", GWT, pW, GT, ALU.mult, ["b2", "b3", gtk], [gwtk])
                if idx >= 1:
                    stageB_dve(idx - 1)

            def stageB_pe(idx):
                ec, j = iters[idx]
                uv, vv, ukey, vkey = bufs(ec)
                jb = idx % 2
                GWT, gwtk = GWT_[jb], "GWT%d" % jb
                po = k.psum[1][:, 0:2048]
                for b4 in range(4):
                    for dc in range(4):
                        k.mm(po[:, dc * 512:(dc + 1) * 512], GWT[:, b4, :], vv[:, b4, dc * 512:(dc + 1) * 512], b4 == 0, b4 == 3, [gwtk, vkey], ["b%d" % (4 + dc)])

            def stageB_dve(idx):
                ec, j = iters[idx]
                po = k.psum[1][:, 0:2048]
                k.tt("dve", h2[:, j, :], h2[:, j, :], po, ALU.add, ["h2_%d" % j, "b4", "b5", "b6", "b7"], ["h2_%d" % j])

            load5(0)
            for idx in range(len(iters)):
                ec, j = iters[idx]
                stageA(idx)
                if j == 1 and ec + 1 < NEC:
                    load5(ec + 1)
            stageB_pe(len(iters) - 1)
            stageB_dve(len(iters) - 1)
            k.barrier()
            for j, ot in enumerate(otiles):
                k.memset("pool", sq[:, 0:1], 0.0, ["sq0"])
                k.act(osb, h2[:, j, :], AF.Square, ["h2_%d" % j, "sq0"], ["osb", "sq0"], accum_out=sq[:, 0:1])
                k.ts("dve", sq[:, 1:2], sq[:, 0:1], 1.0 / D, ALU.mult, ["sq0"], ["sq1"], s2=EPS, op1=ALU.add)
                k.act(sq[:, 2:3], sq[:, 1:2], AF.Sqrt, ["sq1"], ["sq2"])
                k.op("dve", lambda e: e.reciprocal(sq[:, 3:4], sq[:, 2:3]), ["sq2"], ["sq3"])
                k.stt("dve", osb, h2[:, j, :], sq[:, 3:4], nfin_r, ALU.mult, ALU.mult, ["h2_%d" % j, "sq3", "xt", "osb"], ["osb"])
                out_toks.append(k.dma("sp", yout[ot * 128:(ot + 1) * 128, :], osb, reads=["osb"]))


def _rot_cols(w, head_dim):
    half = head_dim // 2
    n = w.shape[1]
    idx = np.arange(n)
    r = (idx // head_dim) * head_dim + (idx % head_dim + half) % head_dim
    return w[:, r]


def _tables(pos):
    res = []
    for hd in (128, 64):
        half = hd // 2
        p = np.arange(128) % hd
        inv = (10000.0 ** (-(np.arange(half, dtype=np.float32)) / np.float32(half))).astype(np.float32)
        ang = pos.astype(np.float32)[None, :] * inv[p % half][:, None]
        c = np.cos(ang).astype(np.float32)
        s = np.sin(ang).astype(np.float32)
        sgn = np.where(p < half, -1.0, 1.0).astype(np.float32)[:, None]
        res += [c, s * sgn]
    return np.stack(res, 0).astype(np.float32)


_NC_CACHE = {}


def kernel(x, meta_tokens, norm_mix_w, w_in, conv_w, conv_b, dt_bias, a_log, d_skip, ssm_norm_w,
           w_branch_attn, w_branch_ssm, w_out, norm_ffn_w, peer_w_query, peer_sub_keys, peer_u, peer_v,
           norm_final_w, _phases=(1, 2, 3, 4, 5), _dbg=False, _gsel=None, _ncores=8, _trace=False):
    f = lambda a: np.ascontiguousarray(np.asarray(a, dtype=np.float32))
    x = f(x)
    meta = f(meta_tokens)
    w_in0 = f(w_in)[0]
    w_rot = np.concatenate([
        _rot_cols(w_in0[:, O_Q:O_Q + 2048], 128), _rot_cols(w_in0[:, O_K:O_K + 512], 128),
        _rot_cols(w_in0[:, O_QI:O_QI + 1024], 64), _rot_cols(w_in0[:, O_KI:O_KI + 64], 64)], axis=1)
    w_rot = np.ascontiguousarray(w_rot)
    cst = np.zeros((128, 5, 128), np.float32)
    ii = np.arange(128)
    cst[:, 0, :] = np.eye(128)
    cst[:, 1, :] = (ii[:, None] <= ii[None, :])
    cst[:, 2, :] = np.where(ii[:, None] > ii[None, :], -30000.0, 0.0)
    cst[:, 3, :] = np.where(ii[None, :] <= ii[:, None], 0.0, NEG)
    cst[:, 4, :] = 1.0
    common = {
        "cst": cst, "w_in": w_in0, "w_rot": w_rot,
        "convw": np.ascontiguousarray(f(conv_w)[0].T.reshape(48, 128, 4).transpose(1, 0, 2)),
        "convb": np.ascontiguousarray(f(conv_b)[0].reshape(48, 128).T),
        "dtb": f(dt_bias)[0], "alog": f(a_log)[0], "dsk": f(d_skip)[0],
        "snw": np.ascontiguousarray(f(ssm_norm_w)[0].reshape(32, 128).T),
        "nmix": np.ascontiguousarray(f(norm_mix_w)[0].reshape(16, 128).T),
        "nffn": np.ascontiguousarray(f(norm_ffn_w)[0].reshape(16, 128).T),
        "nfin": f(norm_final_w),
        "w_pa": f(w_branch_attn)[0], "w_ps": f(w_branch_ssm)[0], "w_o": f(w_out)[0], "w_pq": f(peer_w_query)[0],
        "keysT": np.ascontiguousarray(f(peer_sub_keys)[0].transpose(2, 0, 1)),
        "uT": np.ascontiguousarray(f(peer_u)[0].T), "pv": f(peer_v)[0],
    }
    in_maps = []
    for core in range(8):
        b, c = core // 4, core % 4
        own_start = 16 + 1024 * c
        pos = own_start - CTX + np.arange(NSLOT)
        xin = np.zeros((NSLOT, D), np.float32)
        seq = np.concatenate([meta, x[b]], axis=0)
        ok = (pos >= 0)
        xin[ok] = seq[pos[ok]]
        vmask = ok.astype(np.float32)
        m = dict(common)
        m["xin"] = xin
        m["tabs"] = _tables(pos)
        m["valid"] = np.ascontiguousarray(vmask.reshape(NT, 128).T)
        m["padb"] = np.where(ok[:CTX], 0.0, NEG).astype(np.float32)
        in_maps.append(m)
    key = (tuple(_phases), _dbg, _gsel)
    if key not in _NC_CACHE:
        _NC_CACHE[key] = build_program(_phases, _dbg, _gsel)
    nc, used = _NC_CACHE[key]
    in_maps = [{n: m[n] for n in used} for m in in_maps][:_ncores]
    res = run_bass_kernel_spmd(nc, in_maps, core_ids=list(range(_ncores)), **({'trace': True} if _trace else {}))
    out = np.zeros((2, 4096, D), np.float32)
    for core in range(_ncores):
        b, c = core // 4, core % 4
        out[b, c * 1024:(c + 1) * 1024] = res.results[core]["y"]
    if _dbg:
        return out, res
    return out
```

```python
import math
import numpy as np
from contextlib import ExitStack
import concourse.bass as bass
import concourse.mybir as mybir
from concourse.bass_utils import run_bass_kernel_spmd

F32 = mybir.dt.float32
BF16 = mybir.dt.bfloat16
U8 = mybir.dt.uint8
AF = mybir.ActivationFunctionType
ALU = mybir.AluOpType

N_DMA_SEMS = 24
D = 2048
NT_CTX = 25
NT_OWN = 8
NT = NT_CTX + NT_OWN
NSLOT = NT * 128
CTX = NT_CTX * 128
EPS = 1e-6
O_Q, O_K, O_V, O_QI, O_KI, O_WI, O_Z, O_XS, O_B, O_C, O_DT, O_G = 0, 2048, 2560, 3072, 4096, 4160, 4176, 8272, 12368, 13392, 14416, 14480
R_Q, R_K, R_QI, R_KI = 0, 2048, 2560, 3584
NEG = -1.0e30
NEC = 32


class KB:
    ENGS = ("pe", "act", "dve", "pool", "sp")

    def __init__(self, nc):
        self.nc = nc
        self.es = ExitStack()
        self.ops = {e: [] for e in self.ENGS}
        self.sem = {e: self.es.enter_context(nc.semaphore("s_" + e)) for e in self.ENGS}
        self.cnt = {e: 0 for e in self.ENGS}
        self.dsem = [self.es.enter_context(nc.semaphore("s_dma%d" % i)) for i in range(N_DMA_SEMS)]
        self.dcnt = [0] * N_DMA_SEMS
        self.dnext = 0
        self.seen = {e: {} for e in self.ENGS}
        self.res = {}
        self.semobj = {}
        for e in self.ENGS:
            self.semobj[("e", e)] = self.sem[e]
        for i in range(N_DMA_SEMS):
            self.semobj[("d", i)] = self.dsem[i]
        self.arena = self.es.enter_context(nc.sbuf_tensor("arena", [128, 204 * 1024], U8))
        self.aoff = 0
        self.psum = [self.es.enter_context(nc.psum_tensor("psb%d" % i, [128, 2048], F32)) for i in range(2)]

    def alloc(self, shape, dt):
        esz = 4 if dt == F32 else 2
        n = int(np.prod(shape[1:]))
        nb = (n * esz + 63) // 64 * 64
        o = self.aoff
        self.aoff += nb
        assert self.aoff <= 204 * 1024, "SBUF arena overflow %d" % self.aoff
        v = self.arena[:, o:o + n * esz].bitcast(dt)
        if len(shape) == 3:
            v = v.rearrange("p (a b) -> p a b", b=shape[2])
        elif len(shape) == 4:
            v = v.rearrange("p (a b c) -> p a b c", b=shape[2], c=shape[3])
        if shape[0] < 128:
            v = v[0:shape[0]]
        return v

    def view(self, off, shape, dt):
        save = self.aoff
        self.aoff = off
        v = self.alloc(shape, dt)
        self.aoff = save
        return v

    def barrier(self):
        toks = [(("e", e), self.cnt[e]) for e in self.ENGS if self.cnt[e] > 0]
        toks += [(("d", i), self.dcnt[i]) for i in range(N_DMA_SEMS) if self.dcnt[i] > 0]
        for e in self.ENGS:
            w = []
            for kk, v in toks:
                if kk == ("e", e) or self.seen[e].get(kk, 0) >= v:
                    continue
                self.seen[e][kk] = v
                w.append((kk, v))
            if w:
                self.ops[e].append((w, None, None))

    def bank(self, i, dt=F32):
        t = self.psum[i // 4]
        v = t[:, (i % 4) * 512:(i % 4 + 1) * 512]
        if dt == BF16:
            v = v.bitcast(BF16)
        return v

    def _deps(self, eng, reads, writes):
        need = {}

        def add(tok):
            if tok is None:
                return
            k, v = tok
            if need.get(k, 0) < v:
                need[k] = v

        for r in reads:
            st = self.res.get(r)
            if st is not None:
                add(st[0])
        for w in writes:
            st = self.res.get(w)
            if st is not None:
                add(st[0])
                for k, v in st[1].items():
                    add((k, v))
        waits = []
        for k, v in need.items():
            if k == ("e", "pe") and eng == "pe":
                continue
            if self.seen[eng].get(k, 0) >= v:
                continue
            self.seen[eng][k] = v
            waits.append((k, v))
        return waits

    def _commit(self, tok, reads, writes):
        k, v = tok
        for r in reads:
            st = self.res.setdefault(r, [None, {}])
            if st[1].get(k, 0) < v:
                st[1][k] = v
        for w in writes:
            self.res[w] = [tok, {}]

    @staticmethod
    def _excl(reads, writes):
        ps = [r for r in reads if len(r) == 2 and r[0] == "b" and r[1].isdigit()]
        return (reads, list(writes) + ps) if ps else (reads, writes)

    def op(self, eng, fn, reads=(), writes=()):
        reads, writes = self._excl(reads, writes)
        waits = self._deps(eng, reads, writes)
        self.cnt[eng] += 1
        tok = (("e", eng), self.cnt[eng])
        self.ops[eng].append((waits, fn, (("e", eng), 1)))
        self._commit(tok, reads, writes)
        return tok

    def dma(self, eng, out, in_, reads=(), writes=()):
        i = self.dnext
        self.dnext = (self.dnext + 1) % N_DMA_SEMS
        waits = self._deps(eng, reads, writes)
        prev = self.dcnt[i]
        k = ("d", i)
        if prev > 0 and self.seen[eng].get(k, 0) < prev:
            self.seen[eng][k] = prev
            waits.append((k, prev))
        self.dcnt[i] += 16
        tok = (k, self.dcnt[i])
        self.ops[eng].append((waits, lambda e: e.dma_start(out=out, in_=in_), (k, 16)))
        self._commit(tok, reads, writes)
        return tok

    def wait_all(self, eng, toks):
        self.ops[eng].append((list(toks), None, None))

    def mm(self, out, lhsT, rhs, start, stop, reads, writes):
        return self.op("pe", lambda e: e.matmul(out, lhsT, rhs, start=start, stop=stop), reads, writes)

    def tr(self, out, in_, ident, reads, writes):
        return self.op("pe", lambda e: e.transpose(out, in_, ident), reads, writes)

    def act(self, out, in_, func, reads, writes, bias=None, scale=None, accum_out=None):
        kw = {}
        if bias is not None:
            kw["bias"] = bias
        if scale is not None:
            kw["scale"] = scale
        if accum_out is not None:
            kw["accum_out"] = accum_out
        return self.op("act", lambda e: e.activation(out, in_, func, **kw), reads, writes)

    def tt(self, eng, out, in0, in1, op, reads, writes):
        return self.op(eng, lambda e: e.tensor_tensor(out=out, in0=in0, in1=in1, op=op), reads, writes)

    def ts(self, eng, out, in0, s1, op0, reads, writes, s2=None, op1=None, accum_out=None):
        kw = {}
        if op1 is not None:
            kw["op1"] = op1
        if accum_out is not None:
            kw["accum_out"] = accum_out
        return self.op(eng, lambda e: e.tensor_scalar(out=out, in0=in0, scalar1=s1, scalar2=s2, op0=op0, **kw), reads, writes)

    def stt(self, eng, out, in0, scalar, in1, op0, op1, reads, writes, accum_out=None):
        kw = {}
        if accum_out is not None:
            kw["accum_out"] = accum_out
        return self.op(eng, lambda e: e.scalar_tensor_tensor(out=out, in0=in0, scalar=scalar, in1=in1, op0=op0, op1=op1, **kw), reads, writes)

    def cp(self, eng, out, in_, reads, writes):
        if eng == "act":
            return self.op("act", lambda e: e.copy(out, in_), reads, writes)
        return self.op(eng, lambda e: e.tensor_copy(out, in_), reads, writes)

    def memset(self, eng, ap, val, writes):
        return self.op(eng, lambda e: e.memset(ap, val), (), writes)

    def emit(self):
        nc = self.nc
        engmap = {"pe": "tensor", "act": "scalar", "dve": "vector", "pool": "gpsimd", "sp": "sync"}
        with nc.Block() as block:
            for en in self.ENGS:
                ops = self.ops[en]

                def body(engobj, ops=ops):
                    for waits, fn, inc in ops:
                        for k, v in waits:
                            engobj.wait_ge(self.semobj[k], v)
                        if fn is not None:
                            ins = fn(engobj)
                            ins.then_inc(self.semobj[inc[0]], inc[1])

                getattr(block, engmap[en])(body)
        self.es.close()


DBG_STOP = [None]


class _Stop(Exception):
    pass


def ck(n):
    if DBG_STOP[0] == n:
        raise _Stop()


def bc(ap, axis, shape):
    return ap.unsqueeze(axis).to_broadcast(list(shape))


def build_program(phases=(1, 2, 3, 4, 5), dbg=False, gsel=None):
    nc = bass.Bass("TRN2", target_bir_lowering=False)
    st = {}
    try:
        _build_body(nc, phases, dbg, gsel, st)
    except _Stop:
        pass
    k = st["k"]
    k.wait_all("sp", st["out_toks"])
    k.emit()
    return nc, st["used"]


def _build_body(nc, phases, dbg, gsel, st):
    used_inputs = []
    NEEDS = {"xin": (1, 2, 3, 4), "tabs": (2, 3), "valid": (1,), "padb": (3,), "cst": (1, 2, 3, 4, 5), "w_in": (1, 2, 3, 4),
             "w_rot": (2, 3), "convw": (1,), "convb": (1,), "dtb": (1,), "alog": (1,), "dsk": (1,), "snw": (1,),
             "nmix": (1, 2, 3, 4), "nffn": (5,), "nfin": (5,), "w_pa": (4,), "w_ps": (4,), "w_o": (4,), "w_pq": (5,),
             "keysT": (5,), "uT": (5,), "pv": (5,)}

    def din(name, shape, dt=F32):
        if not any(p in phases for p in NEEDS[name]):
            return None
        used_inputs.append(name)
        return nc.dram_tensor(name, list(shape), dt, kind="ExternalInput").ap()

    xin = din("xin", [NSLOT, D])
    tabs = din("tabs", [4, 128, NSLOT])
    valid = din("valid", [128, NT])
    padb = din("padb", [CTX])
    cst = din("cst", [128, 5, 128])
    w_in = din("w_in", [D, 18576])
    w_rot = din("w_rot", [D, 3648])
    convw = din("convw", [128, 48, 4])
    convb = din("convb", [128, 48])
    dtb = din("dtb", [64])
    alog = din("alog", [64])
    dsk = din("dsk", [64])
    snw = din("snw", [128, 32])
    nmix = din("nmix", [128, 16])
    nffn = din("nffn", [128, 16])
    nfin = din("nfin", [D])
    w_pa = din("w_pa", [2048, 2048])
    w_ps = din("w_ps", [4096, 2048])
    w_o = din("w_o", [2048, 2048])
    w_pq = din("w_pq", [2048, 2048])
    keysT = din("keysT", [128, 2, 128])
    uT = din("uT", [2048, 16384])
    pv = din("pv", [16384, 2048])
    yout = nc.dram_tensor("y", [NT_OWN * 128, D], F32, kind="ExternalOutput").ap()
    st_attn = nc.dram_tensor("st_attn", [128, 16, NT_OWN * 128], BF16).ap()
    st_ssm = nc.dram_tensor("st_ssm", [128, 32, NT_OWN * 128], BF16).ap()
    dbg_out = {}
    if dbg:
        dbg_out["d_ssm"] = nc.dram_tensor("d_ssm", [128, 32, NT_OWN * 128], BF16, kind="ExternalOutput").ap()
        dbg_out["d_attn"] = nc.dram_tensor("d_attn", [128, 16, NT_OWN * 128], BF16, kind="ExternalOutput").ap()
        dbg_out["d_h2"] = nc.dram_tensor("d_h2", [128, NT_OWN, D], F32, kind="ExternalOutput").ap()
        dbg_out["d_score"] = nc.dram_tensor("d_score", [128, NSLOT], F32, kind="ExternalOutput").ap()
        dbg_out["d_mask"] = nc.dram_tensor("d_mask", [128, NSLOT], BF16, kind="ExternalOutput").ap()
        dbg_out["d_thr"] = nc.dram_tensor("d_thr", [128, 8], F32, kind="ExternalOutput").ap()
        dbg_out["d_po"] = nc.dram_tensor("d_po", [128, 1024], F32, kind="ExternalOutput").ap()
        dbg_out["d_q"] = nc.dram_tensor("d_q", [128, 16, 128], BF16, kind="ExternalOutput").ap()
        dbg_out["d_k"] = nc.dram_tensor("d_k", [128, 4, NSLOT], BF16, kind="ExternalOutput").ap()

    k = KB(nc)
    out_toks = []
    st["k"] = k
    st["out_toks"] = out_toks
    st["used"] = used_inputs

    ck(-1)
    cstf = k.alloc([128, 5, 128], F32)
    k.dma("sp", cstf, cst, writes=["cst"])
    identf, tri_le, negtri, causb, onesf = (cstf[:, i, :] for i in range(5))
    identb = k.alloc([128, 128], BF16)
    k.dma("pool", identb, cst[:, 0, :], writes=["identb"])
    ck(-2)
    nw_fm = k.alloc([128, 16], F32)
    xt = k.alloc([128, D], F32)
    hn = k.alloc([128, D], BF16)
    sq = k.alloc([128, 4], F32)
    hnT_off = k.aoff
    hnT = k.alloc([128, 16, 512], BF16)
    wb = [k.alloc([128, 16 * 512], BF16) for _ in range(2)]
    wsel = [0]

    def load_w(dram, K, c0, ncols, buf=None):
        if buf is None:
            i = wsel[0]
            wsel[0] ^= 1
        else:
            i = buf
        kt = K // 128
        v = wb[i][:, 0:kt * ncols].rearrange("p (a b) -> p a b", b=ncols)
        k.dma("pool", v, dram[:, c0:c0 + ncols].rearrange("(a p) c -> p a c", p=128), writes=["wb%d" % i])
        return v, "wb%d" % i

    def make_hnT(src_tiles, keys=None, dst=None, dkey="hnT"):
        dst = hnT if dst is None else dst
        for j, src in enumerate(src_tiles):
            if keys is None:
                k.dma("sp", xt, src, writes=["xt"])
                xsrc, xkey = xt, "xt"
            else:
                xsrc, xkey = src, keys[j]
            k.memset("pool", sq[:, 0:1], 0.0, ["sq0"])
            k.act(hn, xsrc, AF.Square, [xkey, "sq0"], ["hn", "sq0"], accum_out=sq[:, 0:1])
            k.ts("dve", sq[:, 1:2], sq[:, 0:1], 1.0 / D, ALU.mult, ["sq0"], ["sq1"], s2=EPS, op1=ALU.add)
            k.act(sq[:, 2:3], sq[:, 1:2], AF.Sqrt, ["sq1"], ["sq2"])
            k.op("dve", lambda e: e.reciprocal(sq[:, 3:4], sq[:, 2:3]), ["sq2"], ["sq3"])
            k.ts("dve", hn, xsrc, sq[:, 3:4], ALU.mult, [xkey, "sq3", "hn"], ["hn"])
            ck(11)
            pT = k.psum[1][:, 0:1024].bitcast(BF16)
            for kt in range(16):
                k.tr(pT[:, kt * 128:(kt + 1) * 128], hn[:, kt * 128:(kt + 1) * 128], identb, ["hn", "identb"], ["b4", "b5"])
            ck(12)
            k.tt("dve", dst[:, :, j * 128:(j + 1) * 128], pT.rearrange("p (a b) -> p a b", b=128),
                 bc(nw_fm, 2, [128, 16, 128]), ALU.mult, ["b4", "b5", "nw"], [dkey])

    def proj_fm(wv, wkey, cc, N, bank, nk=16, rhs=None, rkey="hnT", start=True, stop=True):
        ps = k.bank(bank)[:, 0:N]
        r = hnT if rhs is None else rhs
        for kt in range(nk):
            k.mm(ps, wv[:, kt, cc * 128:(cc + 1) * 128], r[:, kt, 0:N], start and kt == 0, stop and kt == nk - 1,
                 [wkey, rkey], ["b%d" % bank])
        return ps

    groups = [[0]] + [list(range(1 + 4 * i, 5 + 4 * i)) for i in range(8)]
    gsel = tuple(range(9)) if gsel is None else tuple(gsel)

    mark0 = k.aoff
    if 1 in phases:
        k.dma("sp", nw_fm, nmix, writes=["nw"])
        cw = k.alloc([128, 48, 4], F32)
        cb = k.alloc([128, 48], F32)
        k.dma("sp", cw, convw, writes=["cw"])
        k.dma("sp", cb, convb, writes=["cw"])
        snwf = k.alloc([128, 32], F32)
        k.dma("sp", snwf, snw, writes=["snw"])
        validf = k.alloc([128, NT], F32)
        k.dma("sp", validf, valid, writes=["valid"])
        ck(-3)
        dtb_r = k.alloc([128, 64], F32)
        A_r = k.alloc([128, 64], F32)
        dsk_r = k.alloc([128, 64], F32)
        k.dma("sp", dtb_r, dtb.partition_broadcast(128), writes=["dtb"])
        k.dma("sp", A_r, alog.partition_broadcast(128), writes=["A"])
        k.dma("sp", dsk_r, dsk.partition_broadcast(128), writes=["dsk"])
        k.act(A_r, A_r, AF.Exp, ["A"], ["A"])
        k.ts("dve", A_r, A_r, -1.0, ALU.mult, ["A"], ["A"])
        ck(-4)
        hist = k.alloc([128, 48, 3], F32)
        k.memset("pool", hist, 0.0, ["hist"])
        H = k.alloc([128, 4096], F32)
        k.memset("pool", H, 0.0, ["H"])
        Hb = k.alloc([128, 512], BF16)
        ext_ = [k.alloc([128, 515], F32) for _ in range(2)]
        acc_ = [k.alloc([128, 512], F32) for _ in range(2)]
        xc_ = [k.alloc([128, 512], BF16) for _ in range(2)]
        cvsel = [0]
        Xdt = k.alloc([128, 4, 512], BF16)
        Xdec = k.alloc([128, 4, 512], BF16)
        XD = k.alloc([128, 4, 512], F32)
        Btm = k.alloc([128, 4, 128], BF16)
        BT = k.alloc([128, 512], BF16)
        CT = k.alloc([128, 512], BF16)
        sz = k.alloc([128, 4, 512], BF16)
        dtv = k.alloc([128, 4, 64], F32)
        av = k.alloc([128, 4, 64], F32)
        acs = k.alloc([128, 4, 64], F32)
        nacs = k.alloc([128, 4, 64], F32)
        edec = k.alloc([128, 4, 64], F32)
        wst = k.alloc([128, 4, 64], F32)
        eav = k.alloc([128, 4, 64], F32)
        tmp64 = k.alloc([128, 64], F32)
        AT_ = [k.alloc([128, 8, 128], F32) for _ in range(2)]
        LT_ = [k.alloc([128, 8, 128], BF16) for _ in range(2)]
        MT_ = [k.alloc([128, 8, 128], BF16) for _ in range(2)]
        Hb_ = [k.alloc([128, 512], BF16) for _ in range(4)]
        yv = k.alloc([128, 512], F32)
        ynb = k.alloc([128, 512], BF16)
        ysq = k.alloc([128, 4], F32)
        sst = k.alloc([128, 4, 128], BF16)
        ck(-5)
        wdt = k.alloc([128, 16, 64], BF16)
        k.dma("pool", wdt, w_in[:, O_DT:O_DT + 64].rearrange("(a p) c -> p a c", p=128), writes=["wdt"])
        ck(-6)
        negtri4 = k.alloc([128, 4, 128], F32)
        for i in range(4):
            k.cp("dve", negtri4[:, i, :], negtri, ["cst"], ["negtri4"])

        def conv_chunk(ps, N, ch, outbf, okey, first_tile_reads):
            cv = cvsel[0]
            cvsel[0] ^= 1
            ext, acc, ek, ak = ext_[cv], acc_[cv], "ext%d" % cv, "acc%d" % cv
            k.cp("dve", ext[:, 0:3], hist[:, ch, :], ["hist"], [ek])
            k.cp("act", ext[:, 3:3 + N], ps, first_tile_reads, [ek])
            k.cp("dve", hist[:, ch, :], ext[:, N:N + 3], [ek], ["hist"])
            k.ts("dve", acc[:, 0:N], ext[:, 3:3 + N], cw[:, ch, 3:4], ALU.mult, [ek, "cw"], [ak], s2=cb[:, ch:ch + 1], op1=ALU.add)
            for tap in range(3):
                k.stt("dve", acc[:, 0:N], ext[:, tap:tap + N], cw[:, ch, tap:tap + 1], acc[:, 0:N], ALU.mult, ALU.add, [ek, "cw", ak], [ak])
            k.act(outbf, acc[:, 0:N], AF.Silu, [ak], [okey])

        ck(1)
        for gi, tiles in enumerate(groups):
            if gi not in gsel:
                continue
            own = gi >= 7
            needC = gi >= 6
            nt = len(tiles)
            N = 128 * nt
            make_hnT([xin[t * 128:(t + 1) * 128, :] for t in tiles])
            ck(2)
            for j, t in enumerate(tiles):
                ps = k.bank(6)[:, 0:64]
                for kt in range(16):
                    k.mm(ps, hnT[:, kt, j * 128:(j + 1) * 128], wdt[:, kt, :], kt == 0, kt == 15, ["hnT", "wdt"], ["b6"])
                k.tt("dve", tmp64, ps, dtb_r, ALU.add, ["b6", "dtb"], ["tmp64"])
                ck(31)
                k.act(tmp64, tmp64, AF.Exp, ["tmp64"], ["tmp64"])
                k.act(tmp64, tmp64, AF.Ln, ["tmp64"], ["tmp64"], bias=1.0)
                ck(32)
                k.ts("dve", dtv[:, j, :], tmp64, validf[:, t:t + 1], ALU.mult, ["tmp64", "valid"], ["dtv"])
                k.tt("dve", av[:, j, :], dtv[:, j, :], A_r, ALU.mult, ["dtv", "A"], ["av"])
                ps2 = k.bank(6)[:, 64:128]
                k.mm(ps2, tri_le, av[:, j, :], True, True, ["cst", "av"], ["b6"])
                ps3 = k.bank(6)[:, 128:192]
                k.mm(ps3, onesf, av[:, j, :], True, True, ["cst", "av"], ["b6"])
                ck(33)
                k.cp("dve", acs[:, j, :], ps2, ["b6"], ["acs"])
                ck(331)
                k.ts("dve", nacs[:, j, :], ps2, -1.0, ALU.mult, ["b6"], ["nacs"])
                ck(332)
                k.act(edec[:, j, :], ps3, AF.Exp, ["b6"], ["edec"])
                ck(333)
                k.tt("dve", tmp64, ps3, acs[:, j, :], ALU.subtract, ["b6", "acs"], ["tmp64"])
                ck(334)
                k.act(tmp64, tmp64, AF.Exp, ["tmp64"], ["tmp64"])
                ck(335)
                k.tt("dve", wst[:, j, :], tmp64, dtv[:, j, :], ALU.mult, ["tmp64", "dtv"], ["wst"])
                ck(34)
                if own:
                    k.act(eav[:, j, :], acs[:, j, :], AF.Exp, ["acs"], ["eav"])
            ck(3)
            for g in range(8):
                hs = slice(g * 8, (g + 1) * 8)
                wv, wkey = load_w(w_in, D, O_XS + g * 512, 512, buf=0)
                wvb = wb[1][:, 0:16 * 256].rearrange("p (a b) -> p a b", b=256)
                k.dma("pool", wvb[:, :, 0:128], w_in[:, O_B + g * 128:O_B + (g + 1) * 128].rearrange("(a p) c -> p a c", p=128), writes=["wb1"])
                if needC:
                    k.dma("pool", wvb[:, :, 128:256], w_in[:, O_C + g * 128:O_C + (g + 1) * 128].rearrange("(a p) c -> p a c", p=128), writes=["wb1"])
                pTX = k.psum[0][:, 1024:2048].bitcast(BF16).rearrange("p (a b) -> p a b", b=512)
                pTB = k.bank(2, BF16)
                chunks = [("xs", cc) for cc in range(4)] + [("B", 0)] + ([("C", 1)] if needC else [])

                def do_proj(ch, wv=wv, wkey=wkey, wvb=wvb):
                    kind, i = ch
                    if kind == "xs":
                        return proj_fm(wv, wkey, i, N, i % 2)
                    return proj_fm(wvb, "wb1", i, N, i % 2)

                def do_post(ch, ps, g=g, hs=hs):
                    kind, i = ch
                    if kind == "xs":
                        xc, xck = xc_[i % 2], "xc%d" % (i % 2)
                        conv_chunk(ps, N, g * 4 + i, xc[:, 0:N], xck, ["b%d" % (i % 2)])
                        for j in range(nt):
                            k.tr(pTX[:, j, i * 128:(i + 1) * 128], xc[:, j * 128:(j + 1) * 128], identb, [xck, "identb"], ["b2", "b3"])
                        if i == 3:
                            for j in range(nt):
                                src = pTX[:, j, :].rearrange("p (h d) -> p h d", d=64)
                                k.tt("dve", Xdt[:, j, :].rearrange("p (h d) -> p h d", d=64), src, bc(dtv[:, j, hs], 2, [128, 8, 64]), ALU.mult, ["b2", "b3", "dtv"], ["Xdt"])
                                k.tt("dve", Xdec[:, j, :].rearrange("p (h d) -> p h d", d=64), src, bc(wst[:, j, hs], 2, [128, 8, 64]), ALU.mult, ["b2", "b3", "wst"], ["Xdec"])
                                if own:
                                    k.tt("dve", XD[:, j, :].rearrange("p (h d) -> p h d", d=64), src, bc(dsk_r[:, hs], 2, [128, 8, 64]), ALU.mult, ["b2", "b3", "dsk"], ["XD"])
                    elif kind == "B":
                        conv_chunk(ps, N, 32 + g, BT[:, 0:N], "BT", ["b0"])
                        for j in range(nt):
                            k.tr(pTB[:, j * 128:(j + 1) * 128], BT[:, j * 128:(j + 1) * 128], identb, ["BT", "identb"], ["b2", "b3"])
                        k.cp("act", Btm[:, 0:nt, :], pTB[:, 0:N].rearrange("p (a b) -> p a b", b=128), ["b2", "b3"], ["Btm"])
                    else:
                        conv_chunk(ps, N, 40 + g, CT[:, 0:N], "CT", ["b1"])

                pending = None
                for ch in chunks:
                    ps = do_proj(ch)
                    if pending is not None:
                        do_post(*pending)
                    pending = (ch, ps)
                do_post(*pending)
                if own:
                    wvz, wkeyz = load_w(w_in, D, O_Z + g * 512, 512, buf=0)
                    for j in range(nt):
                        ps = k.bank(j % 2)
                        for kt in range(16):
                            k.mm(ps, hnT[:, kt, j * 128:(j + 1) * 128], wvz[:, kt, :], kt == 0, kt == 15, ["hnT", wkeyz], ["b%d" % (j % 2)])
                        k.act(sz[:, j, :], ps, AF.Silu, ["b%d" % (j % 2)], ["sz"])
                ck(7)
                Hg = H[:, g * 512:(g + 1) * 512]
                Hg3 = Hg.rearrange("p (h d) -> p h d", d=64)

                def upd(j, Hg=Hg, Hg3=Hg3, hs=hs):
                    sb_ = 6 + j % 2
                    ps_S = k.bank(sb_)
                    k.mm(ps_S, Btm[:, j, :], Xdec[:, j, :], True, True, ["Btm", "Xdec"], ["b%d" % sb_])
                    k.tt("dve", Hg3, Hg3, bc(edec[:, j, hs], 2, [128, 8, 64]), ALU.mult, ["H", "edec"], ["H"])
                    k.tt("dve", Hg, Hg, ps_S, ALU.add, ["H", "b%d" % sb_], ["H"])

                if not own:
                    for j in range(nt):
                        upd(j)
                else:
                    for j in range(nt):
                        k.cp("act", Hb_[j], Hg, ["H"], ["Hb%d" % j])
                        upd(j)

                    def front(j, g=g, hs=hs):
                        js = slice(j * 128, (j + 1) * 128)
                        jb = j % 2
                        AT, LT, MT = AT_[jb], LT_[jb], MT_[jb]
                        atk, ltk, mtk = "AT%d" % jb, "LT%d" % jb, "MT%d" % jb
                        ps_yo = k.bank(jb)
                        k.mm(ps_yo, CT[:, js], Hb_[j], True, True, ["CT", "Hb%d" % j], ["b%d" % jb])
                        ps_cb = k.bank(7)[:, 0:128]
                        k.mm(ps_cb, BT[:, js], CT[:, js], True, True, ["BT", "CT"], ["b7"])
                        k.tt("pool", AT, bc(tri_le, 1, [128, 8, 128]), bc(av[:, j, hs], 2, [128, 8, 128]), ALU.mult, ["cst", "av"], [atk])
                        psL = k.psum[1][:, 0:1024].rearrange("p (a b) -> p a b", b=128)
                        for hf in range(2):
                            k.mm(psL[:, hf * 4:(hf + 1) * 4, :], onesf, AT[:, hf * 4:(hf + 1) * 4, :], True, False, ["cst", atk], ["b%d" % (4 + hf)])
                            k.mm(psL[:, hf * 4:(hf + 1) * 4, :], identf, negtri4, False, True, ["cst", "negtri4"], ["b%d" % (4 + hf)])
                        for hh in range(8):
                            k.act(LT[:, hh, :], psL[:, hh, :], AF.Exp, ["b%d" % (4 + hh // 4), "nacs"], [ltk], bias=nacs[:, j, g * 8 + hh:g * 8 + hh + 1])
                        k.tt("dve", MT, LT, bc(ps_cb, 1, [128, 8, 128]), ALU.mult, [ltk, "b7"], [mtk])

                    def back(j, g=g, hs=hs):
                        t = tiles[j]
                        jb = j % 2
                        MT, mtk = MT_[jb], "MT%d" % jb
                        ps_yo = k.bank(jb)
                        ps_y = k.bank(6)[:, 0:512]
                        for hh in range(8):
                            k.mm(ps_y[:, hh * 64:(hh + 1) * 64], MT[:, hh, :], Xdt[:, j, hh * 64:(hh + 1) * 64], True, True, [mtk, "Xdt"], ["b6"])
                        y3 = yv.rearrange("p (h d) -> p h d", d=64)
                        k.tt("dve", y3, ps_yo.rearrange("p (h d) -> p h d", d=64), bc(eav[:, j, hs], 2, [128, 8, 64]), ALU.mult, ["b%d" % jb, "eav"], ["yv"])
                        k.tt("dve", yv, yv, ps_y, ALU.add, ["yv", "b6"], ["yv"])
                        k.tt("dve", yv, yv, XD[:, j, :], ALU.add, ["yv", "XD"], ["yv"])
                        k.tt("dve", yv, yv, sz[:, j, :], ALU.mult, ["yv", "sz"], ["yv"])
                        k.memset("pool", ysq[:, 0:1], 0.0, ["ysq0"])
                        k.act(ynb, yv, AF.Square, ["yv", "ysq0"], ["ynb", "ysq0"], accum_out=ysq[:, 0:1])
                        k.ts("dve", ysq[:, 1:2], ysq[:, 0:1], 1.0 / 512, ALU.mult, ["ysq0"], ["ysq1"], s2=EPS, op1=ALU.add)
                        k.act(ysq[:, 2:3], ysq[:, 1:2], AF.Sqrt, ["ysq1"], ["ysq2"])
                        k.op("dve", lambda e: e.reciprocal(ysq[:, 3:4], ysq[:, 2:3]), ["ysq2"], ["ysq3"])
                        k.ts("dve", ynb, yv, ysq[:, 3:4], ALU.mult, ["yv", "ysq3", "ynb"], ["ynb"])
                        pTy = k.bank(3, BF16)
                        for cc in range(4):
                            k.tr(pTy[:, cc * 128:(cc + 1) * 128], ynb[:, cc * 128:(cc + 1) * 128], identb, ["ynb", "identb"], ["b2", "b3"])
                        for cc in range(4):
                            k.act(sst[:, cc, :], pTy[:, cc * 128:(cc + 1) * 128], AF.Copy, ["b2", "b3", "snw"], ["sst"], scale=snwf[:, g * 4 + cc:g * 4 + cc + 1])
                        ot = t - NT_CTX
                        k.dma("sp", st_ssm[:, g * 4:(g + 1) * 4, ot * 128:(ot + 1) * 128], sst, reads=["sst"], writes=["st_ssm"])

                    front(0)
                    for j in range(nt):
                        if j + 1 < nt:
                            front(j + 1)
                        back(j)
                ck(8)
        if dbg:
            dtile = k.alloc([128, 32, 128], BF16)
            for ot in range(NT_OWN):
                k.dma("sp", dtile, st_ssm[:, :, ot * 128:(ot + 1) * 128], reads=["st_ssm"], writes=["dtile"])
                out_toks.append(k.dma("sp", dbg_out["d_ssm"][:, :, ot * 128:(ot + 1) * 128], dtile, reads=["dtile"]))
    k.aoff = mark0

    def rope_fm(ps_a, ps_b, tbuf, tsel, N, out, okey, akey, bkey, r1, r2):
        k.tt("dve", r1[:, 0:N], ps_a, tbuf[:, tsel, 0:N], ALU.mult, [akey, "tb"], ["r1"])
        k.tt("dve", r2[:, 0:N], ps_b, tbuf[:, tsel + 1, 0:N], ALU.mult, [bkey, "tb"], ["r2"])
        k.tt("dve", out, r1[:, 0:N], r2[:, 0:N], ALU.add, ["r1", "r2"], [okey])

    if 2 in phases:
        k.barrier()
        k.dma("sp", nw_fm, nmix, writes=["nw"])
        KT = k.alloc([128, 4, NSLOT], BF16)
        V = k.alloc([128, NT, 4, 130], BF16)
        kiT = k.alloc([128, NSLOT], BF16)
        mark2 = k.aoff
        k.memset("pool", V, 1.0, ["V"])
        tb = k.alloc([128, 4, 512], F32)
        r1 = k.alloc([128, 512], F32)
        r2 = k.alloc([128, 512], F32)
        for gi, tiles in enumerate(groups):
            if gi not in gsel:
                continue
            nt = len(tiles)
            N = 128 * nt
            s0 = tiles[0] * 128
            make_hnT([xin[t * 128:(t + 1) * 128, :] for t in tiles])
            k.dma("sp", tb[:, :, 0:N], tabs[:, :, s0:s0 + N].rearrange("a p s -> p a s"), writes=["tb"])
            wv, wkey = load_w(w_in, D, O_K, 512, buf=0)
            wr, rkey = load_w(w_rot, D, R_K, 512, buf=1)
            for cc in range(4):
                pa = proj_fm(wv, wkey, cc, N, 0)
                pb = proj_fm(wr, rkey, cc, N, 1)
                rope_fm(pa, pb, tb, 0, N, KT[:, cc, s0:s0 + N], "KT", "b0", "b1", r1, r2)
            wki = wb[0][:, 0:16 * 256].rearrange("p (a b) -> p a b", b=256)
            for half in range(2):
                k.dma("pool", wki[:, :, half * 64:(half + 1) * 64], w_in[:, O_KI:O_KI + 64].rearrange("(a p) c -> p a c", p=128), writes=["wb0"])
                k.dma("pool", wki[:, :, 128 + half * 64:128 + (half + 1) * 64], w_rot[:, R_KI:R_KI + 64].rearrange("(a p) c -> p a c", p=128), writes=["wb0"])
            pa = proj_fm(wki, "wb0", 0, N, 0)
            pb = proj_fm(wki, "wb0", 1, N, 1)
            rope_fm(pa, pb, tb, 2, N, kiT[:, s0:s0 + N], "kiT", "b0", "b1", r1, r2)
            wvv, wkeyv = load_w(w_in, D, O_V, 512, buf=1)
            for j, t in enumerate(tiles):
                ps = k.bank(2 + j % 2)
                for kt in range(16):
                    k.mm(ps, hnT[:, kt, j * 128:(j + 1) * 128], wvv[:, kt, :], kt == 0, kt == 15, ["hnT", wkeyv], ["b%d" % (2 + j % 2)])
                k.cp("act", V[:, t, :, 0:128], ps.rearrange("p (g d) -> p g d", d=128), ["b%d" % (2 + j % 2)], ["V"])
        k.aoff = mark2

    if 3 in phases:
        k.barrier()
        MB = k.view(hnT_off, [128, NT, 128], BF16)
        tb3 = k.view(hnT_off + 8448 + 4096, [128, 4, 128], F32)
        hnT3 = k.alloc([128, 16, 128], BF16)
        padb_r = k.alloc([128, CTX], BF16)
        k.dma("pool", padb_r, padb.partition_broadcast(128), writes=["padb"])
        qT_ = [k.alloc([128, 16, 128], BF16), k.view(hnT_off + 8448, [128, 16, 128], BF16)]
        qiT = k.alloc([128, 8, 128], BF16)
        wis = k.alloc([128, 16], F32)
        score = k.alloc([128, NSLOT], F32)
        wm_off = k.aoff
        work = k.alloc([128, NSLOT], F32)
        maskb = k.view(wm_off, [128, NSLOT], BF16)
        maskT = k.view(wm_off + NSLOT * 2, [128, NT, 128], BF16)
        rl_ = [k.alloc([128, 512], F32) for _ in range(2)]
        m8 = k.alloc([128, 8], F32)
        thr = k.alloc([128, 1], F32)
        Et_ = [k.alloc([128, 4, 128], BF16) for _ in range(2)]
        ao = k.alloc([128, 16, 128], BF16)
        rs = k.alloc([128, 4], F32)
        aT = k.alloc([128, 16, 128], BF16)
        wwi = k.alloc([128, 16, 16], BF16)
        k.dma("pool", wwi, w_in[:, O_WI:O_WI + 16].rearrange("(a p) c -> p a c", p=128), writes=["wwi"])

        def A1(t):
            ot = t - NT_CTX
            qT, qk = qT_[ot % 2], "qT%d" % (ot % 2)
            N = 128
            s0 = t * 128
            make_hnT([xin[t * 128:(t + 1) * 128, :]], dst=hnT3, dkey="hnT3")
            k.dma("sp", tb3, tabs[:, :, s0:s0 + N].rearrange("a p s -> p a s"), writes=["tb"])
            def proj4(wv, wkey, bank):
                pw = k.bank(bank).rearrange("p (a b) -> p a b", b=128)
                for cc in range(4):
                    for kt in range(16):
                        k.mm(pw[:, cc, :], wv[:, kt, cc * 128:(cc + 1) * 128], hnT3[:, kt, 0:128], kt == 0, kt == 15, [wkey, "hnT3"], ["b%d" % bank])
                return pw

            def rope4(pa, pb, tsel, out, okey, ba, bb):
                tC = bc(tb3[:, tsel, :], 1, [128, 4, 128])
                tS = bc(tb3[:, tsel + 1, :], 1, [128, 4, 128])
                r1q = rl_[0].rearrange("p (a b) -> p a b", b=128)
                r2q = rl_[1].rearrange("p (a b) -> p a b", b=128)
                k.tt("dve", r1q, pa, tC, ALU.mult, ["b%d" % ba, "tb"], ["rl0"])
                k.tt("dve", r2q, pb, tS, ALU.mult, ["b%d" % bb, "tb"], ["rl1"])
                k.tt("dve", out, r1q, r2q, ALU.add, ["rl0", "rl1"], [okey])

            nproj = 0
            for c4 in range(4):
                wv, wkey = load_w(w_in, D, O_Q + c4 * 512, 512, buf=0)
                wr, rkey = load_w(w_rot, D, R_Q + c4 * 512, 512, buf=1)
                ba = 0 if nproj % 2 == 0 else 2
                nproj += 1
                pa = proj4(wv, wkey, ba)
                pb = proj4(wr, rkey, ba + 1)
                rope4(pa, pb, 0, qT[:, c4 * 4:(c4 + 1) * 4, :], qk, ba, ba + 1)
            for c4 in range(2):
                wv, wkey = load_w(w_in, D, O_QI + c4 * 512, 512, buf=0)
                wr, rkey = load_w(w_rot, D, R_QI + c4 * 512, 512, buf=1)
                ba = 0 if nproj % 2 == 0 else 2
                nproj += 1
                pa = proj4(wv, wkey, ba)
                pb = proj4(wr, rkey, ba + 1)
                rope4(pa, pb, 2, qiT[:, c4 * 4:(c4 + 1) * 4, :], "qiT", ba, ba + 1)
            ps = k.bank(2)[:, 0:16]
            for kt in range(16):
                k.mm(ps, hnT3[:, kt, 0:128], wwi[:, kt, :], kt == 0, kt == 15, ["hnT3", "wwi"], ["b2"])
            k.ts("dve", wis, ps, 1.0 / 32.0, ALU.mult, ["b2"], ["wis"])
            nk = t + 1
            S = nk * 128
            chunks = [(c0, min(512, CTX - c0)) for c0 in range(0, CTX, 512)] + [(c0, min(512, S - c0)) for c0 in range(CTX, S, 512)]
            for ci, (c0, n) in enumerate(chunks):
                for h in range(16):
                    hp = slice((h % 2) * 64, (h % 2) * 64 + 64)
                    bk = 2 + (h % 2)
                    rl, rlk = rl_[h % 2], "rl%d" % (h % 2)
                    ps = k.bank(bk)[:, 0:n]
                    k.mm(ps, qiT[hp, h // 2, :], kiT[hp, c0:c0 + n], True, True, ["qiT", "kiT"], ["b%d" % bk])
                    k.act(rl[:, 0:n], ps, AF.Relu, ["b%d" % bk], [rlk])
                    if h == 0:
                        if c0 < CTX:
                            k.stt("dve", score[:, c0:c0 + n], rl[:, 0:n], wis[:, 0:1], padb_r[:, c0:c0 + n], ALU.mult, ALU.add, [rlk, "wis", "padb"], ["score"])
                        else:
                            k.ts("dve", score[:, c0:c0 + n], rl[:, 0:n], wis[:, 0:1], ALU.mult, [rlk, "wis"], ["score"])
                    else:
                        k.stt("dve", score[:, c0:c0 + n], rl[:, 0:n], wis[:, h:h + 1], score[:, c0:c0 + n], ALU.mult, ALU.add, [rlk, "wis", "score"], ["score"])
            k.tt("dve", score[:, S - 128:S], score[:, S - 128:S], causb, ALU.add, ["score", "cst"], ["score"])
            if dbg and t == NT_CTX:
                out_toks.append(k.dma("sp", dbg_out["d_score"], score, reads=["score"]))

        def TK(t):
            S = (t + 1) * 128
            cur, ckey = score, "score"
            for r in range(32):
                k.op("dve", lambda e, cur=cur, S=S: e.max(out=m8, in_=cur[:, 0:S]), [ckey], ["m8"])
                if r < 31:
                    k.op("dve", lambda e, cur=cur, S=S: e.match_replace(out=work[:, 0:S], in_to_replace=m8, in_values=cur[:, 0:S], imm_value=NEG),
                         [ckey, "m8", "wm"], ["wm"])
                    cur, ckey = work, "wm"
            k.ts("dve", thr, m8[:, 7:8], -1.0e29, ALU.max, ["m8"], ["thr"])
            k.ts("dve", maskb[:, 0:S], score[:, 0:S], thr, ALU.is_ge, ["score", "thr", "wm"], ["wm"])
            if dbg and t == NT_CTX:
                out_toks.append(k.dma("sp", dbg_out["d_mask"], maskb, reads=["wm"]))
                out_toks.append(k.dma("sp", dbg_out["d_thr"], m8, reads=["m8"]))

        def A2(t):
            nk = t + 1
            for kt0 in range(0, nk, 8):
                nb = min(8, nk - kt0)
                pTm = k.bank(4, BF16)
                for i2 in range(nb):
                    k.tr(pTm[:, i2 * 128:(i2 + 1) * 128], maskb[:, (kt0 + i2) * 128:(kt0 + i2 + 1) * 128], identb, ["wm", "identb"], ["b4"])
                k.cp("act", maskT[:, kt0:kt0 + nb, :], pTm[:, 0:nb * 128].rearrange("p (a b) -> p a b", b=128), ["b4", "wm"], ["wm"])
            k.ts("dve", MB[:, 0:nk, :], maskT[:, 0:nk, :], -1.0, ALU.add, ["wm"], ["MB"], s2=30000.0, op1=ALU.mult)

        def B(t):
            ot = t - NT_CTX
            qT, qk = qT_[ot % 2], "qT%d" % (ot % 2)
            nk = t + 1
            for kvg in range(4):
                po = k.psum[1][:, 1024:2048].rearrange("p (a b) -> p a b", b=256)

                def sc_stage(kt, kvg=kvg):
                    bk = kt % 2
                    Et, etk = Et_[bk], "Et%d" % bk
                    ps = k.bank(bk)
                    k.mm(ps, KT[:, kvg, kt * 128:(kt + 1) * 128], qT[:, kvg * 4:(kvg + 1) * 4, :], True, False, ["KT", qk], ["b%d" % bk])
                    for hh in range(4):
                        k.mm(ps[:, hh * 128:(hh + 1) * 128], identb, MB[:, kt, :], False, hh == 3, ["identb", "MB"], ["b%d" % bk])
                    k.act(Et, ps.rearrange("p (a b) -> p a b", b=128), AF.Exp, ["b%d" % bk], [etk], scale=1.0 / math.sqrt(128.0))

                def pv_stage(kt, kvg=kvg, po=po):
                    bk = kt % 2
                    Et, etk = Et_[bk], "Et%d" % bk
                    for hh in range(4):
                        k.mm(po[:, hh, 0:129], Et[:, hh, :], V[:, kt, kvg, 0:129], kt == 0 and hh % 2 == 0, kt == nk - 1, [etk, "V"], ["b%d" % (6 + hh // 2)])

                sc_stage(0)
                for kt in range(nk):
                    if kt + 1 < nk:
                        sc_stage(kt + 1)
                    pv_stage(kt)
                k.act(rs, po[:, :, 128], AF.Ln, ["b6", "b7"], ["rs"])
                k.act(rs, rs, AF.Exp, ["rs"], ["rs"], scale=-1.0)
                for hh in range(4):
                    k.act(ao[:, kvg * 4 + hh, :], po[:, hh, 0:128], AF.Copy, ["b%d" % (6 + hh // 2), "rs"], ["ao"], scale=rs[:, hh:hh + 1])
            pTa = k.psum[1][:, 0:1024].bitcast(BF16)
            for h in range(16):
                k.tr(pTa[:, h * 128:(h + 1) * 128], ao[:, h, :], identb, ["ao", "identb"], ["b4", "b5"])
            k.cp("act", aT, pTa.rearrange("p (a b) -> p a b", b=128), ["b4", "b5"], ["aT"])
            k.dma("sp", st_attn[:, :, ot * 128:(ot + 1) * 128], aT, reads=["aT"], writes=["st_attn"])

        sel = [t for t in range(NT_CTX, NT) if (7 if t < NT_CTX + 4 else 8) in gsel]
        prev = None
        for t in sel:
            A1(t)
            TK(t)
            if prev is not None:
                B(prev)
            A2(t)
            prev = t
        if prev is not None:
            B(prev)
        if dbg:
            for ot in range(NT_OWN):
                k.dma("sp", aT, st_attn[:, :, ot * 128:(ot + 1) * 128], reads=["st_attn"], writes=["aT"])
                out_toks.append(k.dma("sp", dbg_out["d_attn"][:, :, ot * 128:(ot + 1) * 128], aT, reads=["aT"]))
    k.aoff = mark0

    h2 = k.alloc([128, 4, D], F32)
    mark4 = k.aoff
    for gi in (7, 8):
        tiles = groups[gi]
        otiles = [t - NT_CTX for t in tiles]
        o0 = otiles[0] * 128
        if 4 in phases:
            k.barrier()
            k.aoff = mark4
            k.dma("sp", nw_fm, nmix, writes=["nw"])
            wb2 = k.alloc([128, 16 * 512], BF16)
            actT = k.alloc([128, 32, 512], BF16)
            mg = k.alloc([128, 16, 512], BF16)
            sga = k.alloc([128, 512], F32)
            t1 = k.alloc([128, 512], F32)
            make_hnT([xin[t * 128:(t + 1) * 128, :] for t in tiles])
            k.dma("sp", actT[:, 0:16, :], st_attn[:, :, o0:o0 + 512], reads=["st_attn"], writes=["actT"])
            for c4 in range(4):
                wg, gkey = load_w(w_in, D, O_G + c4 * 512, 512, buf=0)
                wv, wkey = load_w(w_pa, 2048, c4 * 512, 512, buf=1)
                for cc in range(4):
                    pg = proj_fm(wg, gkey, cc, 512, 0)
                    pa = proj_fm(wv, wkey, cc, 512, 1, rhs=actT, rkey="actT")
                    k.act(sga, pg, AF.Sigmoid, ["b0"], ["sga"])
                    k.tt("dve", mg[:, c4 * 4 + cc, :], sga, pa, ALU.mult, ["sga", "b1"], ["mg"])
            k.dma("sp", actT, st_ssm[:, :, o0:o0 + 512], reads=["st_ssm", "actT"], writes=["actT"])
            for c4 in range(4):
                wg = wb2[:, :].rearrange("p (a b) -> p a b", b=512)
                k.dma("pool", wg, w_in[:, O_G + 2048 + c4 * 512:O_G + 2048 + (c4 + 1) * 512].rearrange("(a p) c -> p a c", p=128), writes=["wb2"])
                for c2 in range(2):
                    wv, wkey = load_w(w_ps, 4096, c4 * 512 + c2 * 256, 256, buf=c2)
                    for cc in range(2):
                        col = c4 * 4 + c2 * 2 + cc
                        pg = proj_fm(wg, "wb2", c2 * 2 + cc, 512, 0)
                        pa = proj_fm(wv, wkey, cc, 512, 1, nk=32, rhs=actT, rkey="actT")
                        k.act(sga, pg, AF.Sigmoid, ["b0"], ["sga"])
                        k.tt("dve", t1, sga, pa, ALU.mult, ["sga", "b1"], ["t1"])
                        k.tt("dve", mg[:, col, :], mg[:, col, :], t1, ALU.add, ["mg", "t1"], ["mg"])
            for c4 in range(4):
                wv, wkey = load_w(w_o, 2048, c4 * 512, 512, buf=c4 % 2)
                for j, t in enumerate(tiles):
                    bk = 2 + j % 2
                    ps = k.bank(bk)
                    for kt in range(16):
                        k.mm(ps, mg[:, kt, j * 128:(j + 1) * 128], wv[:, kt, :], kt == 0, kt == 15, ["mg", wkey], ["b%d" % bk])
                    if c4 == 0:
                        k.dma("sp", h2[:, j, :], xin[t * 128:(t + 1) * 128, :], writes=["h2_%d" % j])
                    k.tt("dve", h2[:, j, c4 * 512:(c4 + 1) * 512], h2[:, j, c4 * 512:(c4 + 1) * 512], ps, ALU.add, ["h2_%d" % j, "b%d" % bk], ["h2_%d" % j])
            if dbg:
                for j, ot in enumerate(otiles):
                    out_toks.append(k.dma("sp", dbg_out["d_h2"][:, ot, :], h2[:, j, :], reads=["h2_%d" % j]))
        if 5 in phases:
            k.barrier()
            k.aoff = mark4
            k.dma("sp", nw_fm, nffn, writes=["nw"])
            nfin_r = xt
            k.dma("sp", nfin_r, nfin.partition_broadcast(128), writes=["xt"])
            keysb = k.alloc([128, 2, 128], BF16)
            k.dma("pool", keysb, keysT, writes=["keysb"])
            q_off = k.aoff
            qpT = k.alloc([128, 16, 512], BF16)
            osb = k.view(q_off, [128, D], F32)
            ssc = k.alloc([128, 4, 16, 128], F32)
            top = k.alloc([128, 16, 16], F32)
            wk2 = k.alloc([128, 128], F32)
            cand = k.alloc([128, 256], F32)
            cand2 = k.alloc([128, 256], F32)
            c8 = k.alloc([128, 8], F32)
            thr5 = k.alloc([128, 4, 8], F32)
            nb5 = k.alloc([128, 4, 8], F32)
            zz = k.alloc([128, 4], F32)
            Gt_ = [k.alloc([128, 512], BF16) for _ in range(2)]
            GT_ = [k.alloc([128, 4, 128], BF16) for _ in range(2)]
            cand3 = k.alloc([128, 256], F32)
            c8b = k.alloc([128, 8], F32)
            tau5 = k.alloc([128, 4, 8], F32)
            tsum = k.alloc([128, 8], F32)
            Ex_ = [k.alloc([128, 512], F32) for _ in range(3)]
            Wh_ = [k.alloc([128, 512], BF16) for _ in range(3)]
            GWT_ = [k.alloc([128, 4, 128], BF16) for _ in range(2)]
            wb5 = [wb[0], wb[1], k.alloc([128, 16 * 512], BF16), k.alloc([128, 16 * 512], BF16)]
            make_hnT([h2[:, j, :] for j in range(4)], keys=["h2_%d" % j for j in range(4)])
            for c4 in range(4):
                wv, wkey = load_w(w_pq, 2048, c4 * 512, 512, buf=c4 % 2)
                for cc in range(4):
                    pa = proj_fm(wv, wkey, cc, 512, 0)
                    k.cp("act", qpT[:, c4 * 4 + cc, :], pa, ["b0"], ["qpT"])
            for j in range(4):
                js = slice(j * 128, (j + 1) * 128)
                for q4 in range(4):
                    bk = 2 + q4 % 2
                    ps = k.bank(bk)
                    for i2 in range(4):
                        hc = q4 * 4 + i2
                        k.mm(ps[:, i2 * 128:(i2 + 1) * 128], qpT[:, hc, js], keysb[:, hc % 2, :], True, True, ["qpT", "keysb"], ["b%d" % bk])
                    k.cp("act", ssc[:, j, q4 * 4:(q4 + 1) * 4, :], ps.rearrange("p (a b) -> p a b", b=128), ["b%d" % bk], ["ssc"])
                for hc in range(16):
                    k.op("dve", lambda e, hc=hc, j=j: e.max(out=top[:, hc, 0:8], in_=ssc[:, j, hc, :]), ["ssc"], ["top"])
                    k.op("dve", lambda e, hc=hc, j=j: e.match_replace(out=wk2, in_to_replace=top[:, hc, 0:8], in_values=ssc[:, j, hc, :], imm_value=NEG), ["ssc", "top", "wk2"], ["wk2"])
                    k.op("dve", lambda e, hc=hc: e.max(out=top[:, hc, 8:16], in_=wk2), ["wk2", "top"], ["top"])
                for h in range(8):
                    c3 = cand.rearrange("p (a b) -> p a b", b=16)
                    k.tt("dve", c3, bc(top[:, 2 * h, :], 2, [128, 16, 16]), bc(top[:, 2 * h + 1, :], 1, [128, 16, 16]), ALU.add, ["top"], ["cand"])
                    k.op("dve", lambda e: e.max(out=c8, in_=cand), ["cand"], ["c8"])
                    k.ts("dve", zz[:, 0:1], c8[:, 0:1], -1.0, ALU.mult, ["c8"], ["zz0"])
                    k.op("dve", lambda e: e.match_replace(out=cand2, in_to_replace=c8, in_values=cand, imm_value=NEG), ["cand", "c8", "cand2"], ["cand2"])
                    k.op("dve", lambda e: e.max(out=c8, in_=cand2), ["cand2", "c8"], ["c8"])
                    k.cp("dve", thr5[:, j, h:h + 1], c8[:, 7:8], ["c8"], ["thr5"])
                    k.op("dve", lambda e: e.match_replace(out=cand3, in_to_replace=c8, in_values=cand2, imm_value=NEG), ["cand2", "c8", "cand3"], ["cand3"])
                    k.op("dve", lambda e: e.max(out=c8b, in_=cand3), ["cand3", "c8b"], ["c8b"])
                    k.tt("dve", tsum[:, h:h + 1], c8[:, 7:8], c8b[:, 0:1], ALU.add, ["c8", "c8b"], ["tsum"])
                    k.act(cand3, cand, AF.Exp, ["cand", "zz0", "cand3"], ["cand3"], bias=zz[:, 0:1])
                    k.stt("dve", cand3, cand, thr5[:, j, h:h + 1], cand3, ALU.is_ge, ALU.mult, ["cand", "thr5", "cand3"], ["cand3"])
                    k.op("dve", lambda e: e.reduce_sum(out=zz[:, 1:2], in_=cand3, axis=mybir.AxisListType.X), ["cand3"], ["zz1"])
                    k.act(zz[:, 2:3], zz[:, 1:2], AF.Ln, ["zz1"], ["zz2"])
                    k.tt("dve", nb5[:, j, h:h + 1], zz[:, 0:1], zz[:, 2:3], ALU.subtract, ["zz0", "zz2"], ["nb5"])
                k.stt("dve", tau5[:, j, :], tsum, 0.5, nb5[:, j, :], ALU.mult, ALU.add, ["tsum", "nb5"], ["tau5"])
                k.act(tau5[:, j, :], tau5[:, j, :], AF.Exp, ["tau5"], ["tau5"])
                for h in range(8):
                    k.act(ssc[:, j, 2 * h, :], ssc[:, j, 2 * h, :], AF.Exp, ["ssc", "nb5"], ["ssc"], bias=nb5[:, j, h:h + 1])
                    k.act(ssc[:, j, 2 * h + 1, :], ssc[:, j, 2 * h + 1, :], AF.Exp, ["ssc"], ["ssc"])
            iters = [(ec, j) for ec in range(NEC) for j in range(4)]

            def bufs(ec):
                uv = wb5[(ec % 2) * 2][:, :].rearrange("p (a b) -> p a b", b=512)
                vv = wb5[(ec % 2) * 2 + 1][:, :].rearrange("p (a b) -> p a b", b=2048)
                ukey = "wb0" if ec % 2 == 0 else "wu1"
                vkey = "wb1" if ec % 2 == 0 else "wv1"
                return uv, vv, ukey, vkey

            def load5(ec):
                uv, vv, ukey, vkey = bufs(ec)
                k.dma("pool", uv, uT[:, ec * 512:(ec + 1) * 512].rearrange("(a p) c -> p a c", p=128), writes=[ukey])
                k.dma("pool", vv, pv[ec * 512:(ec + 1) * 512, :].rearrange("(a p) c -> p a c", p=128), writes=[vkey])

            def stageA(idx):
                ec, j = iters[idx]
                uv, vv, ukey, vkey = bufs(ec)
                js = slice(j * 128, (j + 1) * 128)
                jb = idx % 2
                Gt, GT, GWT = Gt_[jb], GT_[jb], GWT_[jb]
                gk, gtk, gwtk = "Gt%d" % jb, "GT%d" % jb, "GWT%d" % jb
                ps = k.bank(jb)
                for kt in range(16):
                    k.mm(ps, hnT[:, kt, js], uv[:, kt, :], kt == 0, kt == 15, ["hnT", ukey], ["b%d" % jb])
                k.act(Gt, ps, AF.Gelu, ["b%d" % jb], [gk])
                if idx >= 1:
                    stageB_pe(idx - 1)
                pTg = k.bank(jb, BF16)
                for b4 in range(4):
                    k.tr(pTg[:, b4 * 128:(b4 + 1) * 128], Gt[:, b4 * 128:(b4 + 1) * 128], identb, [gk, "identb"], ["b%d" % jb])
                k.cp("act", GT, pTg[:, 0:512].rearrange("p (a b) -> p a b", b=128), ["b%d" % jb], [gtk])
                pW = k.psum[0][:, 1024:2048].rearrange("p (a b) -> p a b", b=256)[:, :, 0:128]
                for h in range(8):
                    hb = (idx * 8 + h) % 3
                    Ex, Wh = Ex_[hb], Wh_[hb]
                    ek, whk = "Ex%d" % hb, "Wh%d" % hb
                    eng = "dve" if h % 4 == 3 else "pool"
                    k.tt(eng, Ex.rearrange("p (a b) -> p a b", b=128), bc(ssc[:, j, 2 * h, ec * 4:(ec + 1) * 4], 2, [128, 4, 128]),
                         bc(ssc[:, j, 2 * h + 1, :], 1, [128, 4, 128]), ALU.mult, ["ssc"], [ek])
                    k.stt("dve", Wh, Ex, tau5[:, j, h:h + 1], Ex, ALU.is_ge, ALU.mult, ["tau5", ek], [whk])
                    for b4 in range(4):
                        k.mm(pW[:, b4, :], Wh[:, b4 * 128:(b4 + 1) * 128], identb, h == 0 and b4 % 2 == 0, h == 7, [whk, "identb"], ["b%d" % (2 + b4 // 2)])
                k.tt("dve", GWT, pW, GT, ALU.mult, ["b2", "b3", gtk], [gwtk])
                if idx >= 1:
                    stageB_dve(idx - 1)

            def stageB_pe(idx):
                ec, j = iters[idx]
                uv, vv, ukey, vkey = bufs(ec)
                jb = idx % 2
                GWT, gwtk = GWT_[jb], "GWT%d" % jb
                po = k.psum[1][:, 0:2048]
                for b4 in range(4):
                    for dc in range(4):
                        k.mm(po[:, dc * 512:(dc + 1) * 512], GWT[:, b4, :], vv[:, b4, dc * 512:(dc + 1) * 512], b4 == 0, b4 == 3, [gwtk, vkey], ["b%d" % (4 + dc)])

            def stageB_dve(idx):
                ec, j = iters[idx]
                po = k.psum[1][:, 0:2048]
                k.tt("dve", h2[:, j, :], h2[:, j, :], po, ALU.add, ["h2_%d" % j, "b4", "b5", "b6", "b7"], ["h2_%d" % j])

            load5(0)
            for idx in range(len(iters)):
                ec, j = iters[idx]
                stageA(idx)
                if j == 1 and ec + 1 < NEC:
                    load5(ec + 1)
            stageB_pe(len(iters) - 1)
            stageB_dve(len(iters) - 1)
            k.barrier()
            for j, ot in enumerate(otiles):
                k.memset("pool", sq[:, 0:1], 0.0, ["sq0"])
                k.act(osb, h2[:, j, :], AF.Square, ["h2_%d" % j, "sq0"], ["osb", "sq0"], accum_out=sq[:, 0:1])
                k.ts("dve", sq[:, 1:2], sq[:, 0:1], 1.0 / D, ALU.mult, ["sq0"], ["sq1"], s2=EPS, op1=ALU.add)
                k.act(sq[:, 2:3], sq[:, 1:2], AF.Sqrt, ["sq1"], ["sq2"])
                k.op("dve", lambda e: e.reciprocal(sq[:, 3:4], sq[:, 2:3]), ["sq2"], ["sq3"])
                k.stt("dve", osb, h2[:, j, :], sq[:, 3:4], nfin_r, ALU.mult, ALU.mult, ["h2_%d" % j, "sq3", "xt", "osb"], ["osb"])
                out_toks.append(k.dma("sp", yout[ot * 128:(ot + 1) * 128, :], osb, reads=["osb"]))


def _rot_cols(w, head_dim):
    half = head_dim // 2
    n = w.shape[1]
    idx = np.arange(n)
    r = (idx // head_dim) * head_dim + (idx % head_dim + half) % head_dim
    return w[:, r]


def _tables(pos):
    res = []
    for hd in (128, 64):
        half = hd // 2
        p = np.arange(128) % hd
        inv = (10000.0 ** (-(np.arange(half, dtype=np.float32)) / np.float32(half))).astype(np.float32)
        ang = pos.astype(np.float32)[None, :] * inv[p % half][:, None]
        c = np.cos(ang).astype(np.float32)
        s = np.sin(ang).astype(np.float32)
        sgn = np.where(p < half, -1.0, 1.0).astype(np.float32)[:, None]
        res += [c, s * sgn]
    return np.stack(res, 0).astype(np.float32)


_NC_CACHE = {}


def kernel(x, meta_tokens, norm_mix_w, w_in, conv_w, conv_b, dt_bias, a_log, d_skip, ssm_norm_w,
           w_branch_attn, w_branch_ssm, w_out, norm_ffn_w, peer_w_query, peer_sub_keys, peer_u, peer_v,
           norm_final_w, _phases=(1, 2, 3, 4, 5), _dbg=False, _gsel=None, _ncores=8, _trace=False):
    f = lambda a: np.ascontiguousarray(np.asarray(a, dtype=np.float32))
    x = f(x)
    meta = f(meta_tokens)
    w_in0 = f(w_in)[0]
    w_rot = np.concatenate([
        _rot_cols(w_in0[:, O_Q:O_Q + 2048], 128), _rot_cols(w_in0[:, O_K:O_K + 512], 128),
        _rot_cols(w_in0[:, O_QI:O_QI + 1024], 64), _rot_cols(w_in0[:, O_KI:O_KI + 64], 64)], axis=1)
    w_rot = np.ascontiguousarray(w_rot)
    cst = np.zeros((128, 5, 128), np.float32)
    ii = np.arange(128)
    cst[:, 0, :] = np.eye(128)
    cst[:, 1, :] = (ii[:, None] <= ii[None, :])
    cst[:, 2, :] = np.where(ii[:, None] > ii[None, :], -30000.0, 0.0)
    cst[:, 3, :] = np.where(ii[None, :] <= ii[:, None], 0.0, NEG)
    cst[:, 4, :] = 1.0
    common = {
        "cst": cst, "w_in": w_in0, "w_rot": w_rot,
        "convw": np.ascontiguousarray(f(conv_w)[0].T.reshape(48, 128, 4).transpose(1, 0, 2)),
        "convb": np.ascontiguousarray(f(conv_b)[0].reshape(48, 128).T),
        "dtb": f(dt_bias)[0], "alog": f(a_log)[0], "dsk": f(d_skip)[0],
        "snw": np.ascontiguousarray(f(ssm_norm_w)[0].reshape(32, 128).T),
        "nmix": np.ascontiguousarray(f(norm_mix_w)[0].reshape(16, 128).T),
        "nffn": np.ascontiguousarray(f(norm_ffn_w)[0].reshape(16, 128).T),
        "nfin": f(norm_final_w),
        "w_pa": f(w_branch_attn)[0], "w_ps": f(w_branch_ssm)[0], "w_o": f(w_out)[0], "w_pq": f(peer_w_query)[0],
        "keysT": np.ascontiguousarray(f(peer_sub_keys)[0].transpose(2, 0, 1)),
        "uT": np.ascontiguousarray(f(peer_u)[0].T), "pv": f(peer_v)[0],
    }
    in_maps = []
    for core in range(8):
        b, c = core // 4, core % 4
        own_start = 16 + 1024 * c
        pos = own_start - CTX + np.arange(NSLOT)
        xin = np.zeros((NSLOT, D), np.float32)
        seq = np.concatenate([meta, x[b]], axis=0)
        ok = (pos >= 0)
        xin[ok] = seq[pos[ok]]
        vmask = ok.astype(np.float32)
        m = dict(common)
        m["xin"] = xin
        m["tabs"] = _tables(pos)
        m["valid"] = np.ascontiguousarray(vmask.reshape(NT, 128).T)
        m["padb"] = np.where(ok[:CTX], 0.0, NEG).astype(np.float32)
        in_maps.append(m)
    key = (tuple(_phases), _dbg, _gsel)
    if key not in _NC_CACHE:
        _NC_CACHE[key] = build_program(_phases, _dbg, _gsel)
    nc, used = _NC_CACHE[key]
    in_maps = [{n: m[n] for n in used} for m in in_maps][:_ncores]
    res = run_bass_kernel_spmd(nc, in_maps, core_ids=list(range(_ncores)), **({'trace': True} if _trace else {}))
    out = np.zeros((2, 4096, D), np.float32)
    for core in range(_ncores):
        b, c = core // 4, core % 4
        out[b, c * 1024:(c + 1) * 1024] = res.results[core]["y"]
    if _dbg:
        return out, res
    return out
```

```python
import math
import numpy as np
from contextlib import ExitStack
import concourse.bass as bass
import concourse.mybir as mybir
from concourse.bass_utils import run_bass_kernel_spmd

F32 = mybir.dt.float32
BF16 = mybir.dt.bfloat16
U8 = mybir.dt.uint8
AF = mybir.ActivationFunctionType
ALU = mybir.AluOpType

N_DMA_SEMS = 24
D = 2048
NT_CTX = 25
NT_OWN = 8
NT = NT_CTX + NT_OWN
NSLOT = NT * 128
CTX = NT_CTX * 128
EPS = 1e-6
O_Q, O_K, O_V, O_QI, O_KI, O_WI, O_Z, O_XS, O_B, O_C, O_DT, O_G = 0, 2048, 2560, 3072, 4096, 4160, 4176, 8272, 12368, 13392, 14416, 14480
R_Q, R_K, R_QI, R_KI = 0, 2048, 2560, 3584
NEG = -1.0e30
NEC = 32


class KB:
    ENGS = ("pe", "act", "dve", "pool", "sp")

    def __init__(self, nc):
        self.nc = nc
        self.es = ExitStack()
        self.ops = {e: [] for e in self.ENGS}
        self.sem = {e: self.es.enter_context(nc.semaphore("s_" + e)) for e in self.ENGS}
        self.cnt = {e: 0 for e in self.ENGS}
        self.dsem = [self.es.enter_context(nc.semaphore("s_dma%d" % i)) for i in range(N_DMA_SEMS)]
        self.dcnt = [0] * N_DMA_SEMS
        self.dnext = 0
        self.seen = {e: {} for e in self.ENGS}
        self.res = {}
        self.semobj = {}
        for e in self.ENGS:
            self.semobj[("e", e)] = self.sem[e]
        for i in range(N_DMA_SEMS):
            self.semobj[("d", i)] = self.dsem[i]
        self.arena = self.es.enter_context(nc.sbuf_tensor("arena", [128, 204 * 1024], U8))
        self.aoff = 0
        self.psum = [self.es.enter_context(nc.psum_tensor("psb%d" % i, [128, 2048], F32)) for i in range(2)]

    def alloc(self, shape, dt):
        esz = 4 if dt == F32 else 2
        n = int(np.prod(shape[1:]))
        nb = (n * esz + 63) // 64 * 64
        o = self.aoff
        self.aoff += nb
        assert self.aoff <= 204 * 1024, "SBUF arena overflow %d" % self.aoff
        v = self.arena[:, o:o + n * esz].bitcast(dt)
        if len(shape) == 3:
            v = v.rearrange("p (a b) -> p a b", b=shape[2])
        elif len(shape) == 4:
            v = v.rearrange("p (a b c) -> p a b c", b=shape[2], c=shape[3])
        if shape[0] < 128:
            v = v[0:shape[0]]
        return v

    def view(self, off, shape, dt):
        save = self.aoff
        self.aoff = off
        v = self.alloc(shape, dt)
        self.aoff = save
        return v

    def barrier(self):
        toks = [(("e", e), self.cnt[e]) for e in self.ENGS if self.cnt[e] > 0]
        toks += [(("d", i), self.dcnt[i]) for i in range(N_DMA_SEMS) if self.dcnt[i] > 0]
        for e in self.ENGS:
            w = []
            for kk, v in toks:
                if kk == ("e", e) or self.seen[e].get(kk, 0) >= v:
                    continue
                self.seen[e][kk] = v
                w.append((kk, v))
            if w:
                self.ops[e].append((w, None, None))

    def bank(self, i, dt=F32):
        t = self.psum[i // 4]
        v = t[:, (i % 4) * 512:(i % 4 + 1) * 512]
        if dt == BF16:
            v = v.bitcast(BF16)
        return v

    def _deps(self, eng, reads, writes):
        need = {}

        def add(tok):
            if tok is None:
                return
            k, v = tok
            if need.get(k, 0) < v:
                need[k] = v

        for r in reads:
            st = self.res.get(r)
            if st is not None:
                add(st[0])
        for w in writes:
            st = self.res.get(w)
            if st is not None:
                add(st[0])
                for k, v in st[1].items():
                    add((k, v))
        waits = []
        for k, v in need.items():
            if k == ("e", "pe") and eng == "pe":
                continue
            if self.seen[eng].get(k, 0) >= v:
                continue
            self.seen[eng][k] = v
            waits.append((k, v))
        return waits

    def _commit(self, tok, reads, writes):
        k, v = tok
        for r in reads:
            st = self.res.setdefault(r, [None, {}])
            if st[1].get(k, 0) < v:
                st[1][k] = v
        for w in writes:
            self.res[w] = [tok, {}]

    @staticmethod
    def _excl(reads, writes):
        ps = [r for r in reads if len(r) == 2 and r[0] == "b" and r[1].isdigit()]
        return (reads, list(writes) + ps) if ps else (reads, writes)

    def op(self, eng, fn, reads=(), writes=()):
        reads, writes = self._excl(reads, writes)
        waits = self._deps(eng, reads, writes)
        self.cnt[eng] += 1
        tok = (("e", eng), self.cnt[eng])
        self.ops[eng].append((waits, fn, (("e", eng), 1)))
        self._commit(tok, reads, writes)
        return tok

    def dma(self, eng, out, in_, reads=(), writes=()):
        i = self.dnext
        self.dnext = (self.dnext + 1) % N_DMA_SEMS
        waits = self._deps(eng, reads, writes)
        prev = self.dcnt[i]
        k = ("d", i)
        if prev > 0 and self.seen[eng].get(k, 0) < prev:
            self.seen[eng][k] = prev
            waits.append((k, prev))
        self.dcnt[i] += 16
        tok = (k, self.dcnt[i])
        self.ops[eng].append((waits, lambda e: e.dma_start(out=out, in_=in_), (k, 16)))
        self._commit(tok, reads, writes)
        return tok

    def wait_all(self, eng, toks):
        self.ops[eng].append((list(toks), None, None))

    def mm(self, out, lhsT, rhs, start, stop, reads, writes):
        return self.op("pe", lambda e: e.matmul(out, lhsT, rhs, start=start, stop=stop), reads, writes)

    def tr(self, out, in_, ident, reads, writes):
        return self.op("pe", lambda e: e.transpose(out, in_, ident), reads, writes)

    def act(self, out, in_, func, reads, writes, bias=None, scale=None, accum_out=None):
        kw = {}
        if bias is not None:
            kw["bias"] = bias
        if scale is not None:
            kw["scale"] = scale
        if accum_out is not None:
            kw["accum_out"] = accum_out
        return self.op("act", lambda e: e.activation(out, in_, func, **kw), reads, writes)

    def tt(self, eng, out, in0, in1, op, reads, writes):
        return self.op(eng, lambda e: e.tensor_tensor(out=out, in0=in0, in1=in1, op=op), reads, writes)

    def ts(self, eng, out, in0, s1, op0, reads, writes, s2=None, op1=None, accum_out=None):
        kw = {}
        if op1 is not None:
            kw["op1"] = op1
        if accum_out is not None:
            kw["accum_out"] = accum_out
        return self.op(eng, lambda e: e.tensor_scalar(out=out, in0=in0, scalar1=s1, scalar2=s2, op0=op0, **kw), reads, writes)

    def stt(self, eng, out, in0, scalar, in1, op0, op1, reads, writes, accum_out=None):
        kw = {}
        if accum_out is not None:
            kw["accum_out"] = accum_out
        return self.op(eng, lambda e: e.scalar_tensor_tensor(out=out, in0=in0, scalar=scalar, in1=in1, op0=op0, op1=op1, **kw), reads, writes)

    def cp(self, eng, out, in_, reads, writes):
        if eng == "act":
            return self.op("act", lambda e: e.copy(out, in_), reads, writes)
        return self.op(eng, lambda e: e.tensor_copy(out, in_), reads, writes)

    def memset(self, eng, ap, val, writes):
        return self.op(eng, lambda e: e.memset(ap, val), (), writes)

    def emit(self):
        nc = self.nc
        engmap = {"pe": "tensor", "act": "scalar", "dve": "vector", "pool": "gpsimd", "sp": "sync"}
        with nc.Block() as block:
            for en in self.ENGS:
                ops = self.ops[en]

                def body(engobj, ops=ops):
                    for waits, fn, inc in ops:
                        for k, v in waits:
                            engobj.wait_ge(self.semobj[k], v)
                        if fn is not None:
                            ins = fn(engobj)
                            ins.then_inc(self.semobj[inc[0]], inc[1])

                getattr(block, engmap[en])(body)
        self.es.close()


DBG_STOP = [None]


class _Stop(Exception):
    pass


def ck(n):
    if DBG_STOP[0] == n:
        raise _Stop()


def bc(ap, axis, shape):
    return ap.unsqueeze(axis).to_broadcast(list(shape))


def build_program(phases=(1, 2, 3, 4, 5), dbg=False, gsel=None):
    nc = bass.Bass("TRN2", target_bir_lowering=False)
    st = {}
    try:
        _build_body(nc, phases, dbg, gsel, st)
    except _Stop:
        pass
    k = st["k"]
    k.wait_all("sp", st["out_toks"])
    k.emit()
    return nc, st["used"]


def _build_body(nc, phases, dbg, gsel, st):
    used_inputs = []
    NEEDS = {"xin": (1, 2, 3, 4), "tabs": (2, 3), "valid": (1,), "padb": (3,), "cst": (1, 2, 3, 4, 5), "w_in": (1, 2, 3, 4),
             "w_rot": (2, 3), "convw": (1,), "convb": (1,), "dtb": (1,), "alog": (1,), "dsk": (1,), "snw": (1,),
             "nmix": (1, 2, 3, 4), "nffn": (5,), "nfin": (5,), "w_pa": (4,), "w_ps": (4,), "w_o": (4,), "w_pq": (5,),
             "keysT": (5,), "uT": (5,), "pv": (5,)}

    def din(name, shape, dt=F32):
        if not any(p in phases for p in NEEDS[name]):
            return None
        used_inputs.append(name)
        return nc.dram_tensor(name, list(shape), dt, kind="ExternalInput").ap()

    xin = din("xin", [NSLOT, D])
    tabs = din("tabs", [4, 128, NSLOT])
    valid = din("valid", [128, NT])
    padb = din("padb", [CTX])
    cst = din("cst", [128, 5, 128])
    w_in = din("w_in", [D, 18576])
    w_rot = din("w_rot", [D, 3648])
    convw = din("convw", [128, 48, 4])
    convb = din("convb", [128, 48])
    dtb = din("dtb", [64])
    alog = din("alog", [64])
    dsk = din("dsk", [64])
    snw = din("snw", [128, 32])
    nmix = din("nmix", [128, 16])
    nffn = din("nffn", [128, 16])
    nfin = din("nfin", [D])
    w_pa = din("w_pa", [2048, 2048])
    w_ps = din("w_ps", [4096, 2048])
    w_o = din("w_o", [2048, 2048])
    w_pq = din("w_pq", [2048, 2048])
    keysT = din("keysT", [128, 2, 128])
    uT = din("uT", [2048, 16384])
    pv = din("pv", [16384, 2048])
    yout = nc.dram_tensor("y", [NT_OWN * 128, D], F32, kind="ExternalOutput").ap()
    st_attn = nc.dram_tensor("st_attn", [128, 16, NT_OWN * 128], BF16).ap()
    st_ssm = nc.dram_tensor("st_ssm", [128, 32, NT_OWN * 128], BF16).ap()
    dbg_out = {}
    if dbg:
        dbg_out["d_ssm"] = nc.dram_tensor("d_ssm", [128, 32, NT_OWN * 128], BF16, kind="ExternalOutput").ap()
        dbg_out["d_attn"] = nc.dram_tensor("d_attn", [128, 16, NT_OWN * 128], BF16, kind="ExternalOutput").ap()
        dbg_out["d_h2"] = nc.dram_tensor("d_h2", [128, NT_OWN, D], F32, kind="ExternalOutput").ap()
        dbg_out["d_score"] = nc.dram_tensor("d_score", [128, NSLOT], F32, kind="ExternalOutput").ap()
        dbg_out["d_mask"] = nc.dram_tensor("d_mask", [128, NSLOT], BF16, kind="ExternalOutput").ap()
        dbg_out["d_thr"] = nc.dram_tensor("d_thr", [128, 8], F32, kind="ExternalOutput").ap()
        dbg_out["d_po"] = nc.dram_tensor("d_po", [128, 1024], F32, kind="ExternalOutput").ap()
        dbg_out["d_q"] = nc.dram_tensor("d_q", [128, 16, 128], BF16, kind="ExternalOutput").ap()
        dbg_out["d_k"] = nc.dram_tensor("d_k", [128, 4, NSLOT], BF16, kind="ExternalOutput").ap()

    k = KB(nc)
    out_toks = []
    st["k"] = k
    st["out_toks"] = out_toks
    st["used"] = used_inputs

    ck(-1)
    cstf = k.alloc([128, 5, 128], F32)
    k.dma("sp", cstf, cst, writes=["cst"])
    identf, tri_le, negtri, causb, onesf = (cstf[:, i, :] for i in range(5))
    identb = k.alloc([128, 128], BF16)
    k.dma("pool", identb, cst[:, 0, :], writes=["identb"])
    ck(-2)
    nw_fm = k.alloc([128, 16], F32)
    xt = k.alloc([128, D], F32)
    hn = k.alloc([128, D], BF16)
    sq = k.alloc([128, 4], F32)
    hnT_off = k.aoff
    hnT = k.alloc([128, 16, 512], BF16)
    wb = [k.alloc([128, 16 * 512], BF16) for _ in range(2)]
    wsel = [0]

    def load_w(dram, K, c0, ncols, buf=None):
        if buf is None:
            i = wsel[0]
            wsel[0] ^= 1
        else:
            i = buf
        kt = K // 128
        v = wb[i][:, 0:kt * ncols].rearrange("p (a b) -> p a b", b=ncols)
        k.dma("pool", v, dram[:, c0:c0 + ncols].rearrange("(a p) c -> p a c", p=128), writes=["wb%d" % i])
        return v, "wb%d" % i

    def make_hnT(src_tiles, keys=None, dst=None, dkey="hnT"):
        dst = hnT if dst is None else dst
        for j, src in enumerate(src_tiles):
            if keys is None:
                k.dma("sp", xt, src, writes=["xt"])
                xsrc, xkey = xt, "xt"
            else:
                xsrc, xkey = src, keys[j]
            k.memset("pool", sq[:, 0:1], 0.0, ["sq0"])
            k.act(hn, xsrc, AF.Square, [xkey, "sq0"], ["hn", "sq0"], accum_out=sq[:, 0:1])
            k.act(sq[:, 2:3], sq[:, 0:1], AF.Ln, ["sq0"], ["sq2"], bias=EPS, scale=1.0 / D)
            k.act(sq[:, 3:4], sq[:, 2:3], AF.Exp, ["sq2"], ["sq3"], scale=-0.5)
            k.ts("dve", hn, xsrc, sq[:, 3:4], ALU.mult, [xkey, "sq3", "hn"], ["hn"])
            ck(11)
            pT = k.psum[1][:, 0:1024].bitcast(BF16)
            for kt in range(16):
                k.tr(pT[:, kt * 128:(kt + 1) * 128], hn[:, kt * 128:(kt + 1) * 128], identb, ["hn", "identb"], ["b4", "b5"])
            ck(12)
            k.tt("dve", dst[:, :, j * 128:(j + 1) * 128], pT.rearrange("p (a b) -> p a b", b=128),
                 bc(nw_fm, 2, [128, 16, 128]), ALU.mult, ["b4", "b5", "nw"], [dkey])

    def proj_fm(wv, wkey, cc, N, bank, nk=16, rhs=None, rkey="hnT", start=True, stop=True):
        ps = k.bank(bank)[:, 0:N]
        r = hnT if rhs is None else rhs
        for kt in range(nk):
            k.mm(ps, wv[:, kt, cc * 128:(cc + 1) * 128], r[:, kt, 0:N], start and kt == 0, stop and kt == nk - 1,
                 [wkey, rkey], ["b%d" % bank])
        return ps

    groups = [[0]] + [list(range(1 + 4 * i, 5 + 4 * i)) for i in range(8)]
    gsel = tuple(range(9)) if gsel is None else tuple(gsel)

    mark0 = k.aoff
    if 1 in phases:
        k.dma("sp", nw_fm, nmix, writes=["nw"])
        cw = k.alloc([128, 48, 4], F32)
        cb = k.alloc([128, 48], F32)
        k.dma("sp", cw, convw, writes=["cw"])
        k.dma("sp", cb, convb, writes=["cw"])
        snwf = k.alloc([128, 32], F32)
        k.dma("sp", snwf, snw, writes=["snw"])
        validf = k.alloc([128, NT], F32)
        k.dma("sp", validf, valid, writes=["valid"])
        ck(-3)
        dtb_r = k.alloc([128, 64], F32)
        A_r = k.alloc([128, 64], F32)
        dsk_r = k.alloc([128, 64], F32)
        k.dma("sp", dtb_r, dtb.partition_broadcast(128), writes=["dtb"])
        k.dma("sp", A_r, alog.partition_broadcast(128), writes=["A"])
        k.dma("sp", dsk_r, dsk.partition_broadcast(128), writes=["dsk"])
        k.act(A_r, A_r, AF.Exp, ["A"], ["A"])
        k.ts("dve", A_r, A_r, -1.0, ALU.mult, ["A"], ["A"])
        ck(-4)
        hist = k.alloc([128, 48, 3], F32)
        k.memset("pool", hist, 0.0, ["hist"])
        H = k.alloc([128, 4096], F32)
        k.memset("pool", H, 0.0, ["H"])
        Hb = k.alloc([128, 512], BF16)
        ext_ = [k.alloc([128, 515], F32) for _ in range(2)]
        acc_ = [k.alloc([128, 512], F32) for _ in range(2)]
        xc_ = [k.alloc([128, 512], BF16) for _ in range(2)]
        cvsel = [0]
        Xdt = k.alloc([128, 4, 512], BF16)
        Xdec = k.alloc([128, 4, 512], BF16)
        XD = k.alloc([128, 4, 512], F32)
        Btm = k.alloc([128, 4, 128], BF16)
        BT = k.alloc([128, 512], BF16)
        CT = k.alloc([128, 512], BF16)
        sz = k.alloc([128, 4, 512], BF16)
        dtv = k.alloc([128, 4, 64], F32)
        av = k.alloc([128, 4, 64], F32)
        acs = k.alloc([128, 4, 64], F32)
        nacs = k.alloc([128, 4, 64], F32)
        edec = k.alloc([128, 4, 64], F32)
        wst = k.alloc([128, 4, 64], F32)
        eav = k.alloc([128, 4, 64], F32)
        tmp64 = k.alloc([128, 64], F32)
        AT_ = [k.alloc([128, 8, 128], F32) for _ in range(2)]
        LT_ = [k.alloc([128, 8, 128], BF16) for _ in range(2)]
        MT_ = [k.alloc([128, 8, 128], BF16) for _ in range(2)]
        Hb_ = [k.alloc([128, 512], BF16) for _ in range(4)]
        yv = k.alloc([128, 512], F32)
        ynb = k.alloc([128, 512], BF16)
        ysq = k.alloc([128, 4], F32)
        sst = k.alloc([128, 4, 128], BF16)
        ck(-5)
        wdt = k.alloc([128, 16, 64], BF16)
        k.dma("pool", wdt, w_in[:, O_DT:O_DT + 64].rearrange("(a p) c -> p a c", p=128), writes=["wdt"])
        ck(-6)
        negtri4 = k.alloc([128, 4, 128], F32)
        for i in range(4):
            k.cp("dve", negtri4[:, i, :], negtri, ["cst"], ["negtri4"])

        def conv_chunk(ps, N, ch, outbf, okey, first_tile_reads):
            cv = cvsel[0]
            cvsel[0] ^= 1
            ext, acc, ek, ak = ext_[cv], acc_[cv], "ext%d" % cv, "acc%d" % cv
            k.cp("dve", ext[:, 0:3], hist[:, ch, :], ["hist"], [ek])
            k.cp("act", ext[:, 3:3 + N], ps, first_tile_reads, [ek])
            k.cp("dve", hist[:, ch, :], ext[:, N:N + 3], [ek], ["hist"])
            k.ts("dve", acc[:, 0:N], ext[:, 3:3 + N], cw[:, ch, 3:4], ALU.mult, [ek, "cw"], [ak], s2=cb[:, ch:ch + 1], op1=ALU.add)
            for tap in range(3):
                k.stt("dve", acc[:, 0:N], ext[:, tap:tap + N], cw[:, ch, tap:tap + 1], acc[:, 0:N], ALU.mult, ALU.add, [ek, "cw", ak], [ak])
            k.act(outbf, acc[:, 0:N], AF.Silu, [ak], [okey])

        ck(1)
        for gi, tiles in enumerate(groups):
            if gi not in gsel:
                continue
            own = gi >= 7
            needC = gi >= 6
            nt = len(tiles)
            N = 128 * nt
            make_hnT([xin[t * 128:(t + 1) * 128, :] for t in tiles])
            ck(2)
            for j, t in enumerate(tiles):
                ps = k.bank(6)[:, 0:64]
                for kt in range(16):
                    k.mm(ps, hnT[:, kt, j * 128:(j + 1) * 128], wdt[:, kt, :], kt == 0, kt == 15, ["hnT", "wdt"], ["b6"])
                k.tt("dve", tmp64, ps, dtb_r, ALU.add, ["b6", "dtb"], ["tmp64"])
                ck(31)
                k.act(tmp64, tmp64, AF.Exp, ["tmp64"], ["tmp64"])
                k.act(tmp64, tmp64, AF.Ln, ["tmp64"], ["tmp64"], bias=1.0)
                ck(32)
                k.ts("dve", dtv[:, j, :], tmp64, validf[:, t:t + 1], ALU.mult, ["tmp64", "valid"], ["dtv"])
                k.tt("dve", av[:, j, :], dtv[:, j, :], A_r, ALU.mult, ["dtv", "A"], ["av"])
                ps2 = k.bank(6)[:, 64:128]
                k.mm(ps2, tri_le, av[:, j, :], True, True, ["cst", "av"], ["b6"])
                ps3 = k.bank(6)[:, 128:192]
                k.mm(ps3, onesf, av[:, j, :], True, True, ["cst", "av"], ["b6"])
                ck(33)
                k.cp("dve", acs[:, j, :], ps2, ["b6"], ["acs"])
                ck(331)
                k.ts("dve", nacs[:, j, :], ps2, -1.0, ALU.mult, ["b6"], ["nacs"])
                ck(332)
                k.act(edec[:, j, :], ps3, AF.Exp, ["b6"], ["edec"])
                ck(333)
                k.tt("dve", tmp64, ps3, acs[:, j, :], ALU.subtract, ["b6", "acs"], ["tmp64"])
                ck(334)
                k.act(tmp64, tmp64, AF.Exp, ["tmp64"], ["tmp64"])
                ck(335)
                k.tt("dve", wst[:, j, :], tmp64, dtv[:, j, :], ALU.mult, ["tmp64", "dtv"], ["wst"])
                ck(34)
                if own:
                    k.act(eav[:, j, :], acs[:, j, :], AF.Exp, ["acs"], ["eav"])
            ck(3)
            for g in range(8):
                hs = slice(g * 8, (g + 1) * 8)
                wv, wkey = load_w(w_in, D, O_XS + g * 512, 512, buf=0)
                wvb = wb[1][:, 0:16 * 256].rearrange("p (a b) -> p a b", b=256)
                k.dma("pool", wvb[:, :, 0:128], w_in[:, O_B + g * 128:O_B + (g + 1) * 128].rearrange("(a p) c -> p a c", p=128), writes=["wb1"])
                if needC:
                    k.dma("pool", wvb[:, :, 128:256], w_in[:, O_C + g * 128:O_C + (g + 1) * 128].rearrange("(a p) c -> p a c", p=128), writes=["wb1"])
                pTX = k.psum[0][:, 1024:2048].bitcast(BF16).rearrange("p (a b) -> p a b", b=512)
                pTB = k.bank(2, BF16)
                chunks = [("xs", cc) for cc in range(4)] + [("B", 0)] + ([("C", 1)] if needC else [])

                def do_proj(ch, wv=wv, wkey=wkey, wvb=wvb):
                    kind, i = ch
                    if kind == "xs":
                        return proj_fm(wv, wkey, i, N, i % 2)
                    return proj_fm(wvb, "wb1", i, N, i % 2)

                def do_post(ch, ps, g=g, hs=hs):
                    kind, i = ch
                    if kind == "xs":
                        xc, xck = xc_[i % 2], "xc%d" % (i % 2)
                        conv_chunk(ps, N, g * 4 + i, xc[:, 0:N], xck, ["b%d" % (i % 2)])
                        for j in range(nt):
                            k.tr(pTX[:, j, i * 128:(i + 1) * 128], xc[:, j * 128:(j + 1) * 128], identb, [xck, "identb"], ["b2", "b3"])
                        if i == 3:
                            for j in range(nt):
                                src = pTX[:, j, :].rearrange("p (h d) -> p h d", d=64)
                                k.tt("dve", Xdt[:, j, :].rearrange("p (h d) -> p h d", d=64), src, bc(dtv[:, j, hs], 2, [128, 8, 64]), ALU.mult, ["b2", "b3", "dtv"], ["Xdt"])
                                k.tt("dve", Xdec[:, j, :].rearrange("p (h d) -> p h d", d=64), src, bc(wst[:, j, hs], 2, [128, 8, 64]), ALU.mult, ["b2", "b3", "wst"], ["Xdec"])
                                if own:
                                    k.tt("dve", XD[:, j, :].rearrange("p (h d) -> p h d", d=64), src, bc(dsk_r[:, hs], 2, [128, 8, 64]), ALU.mult, ["b2", "b3", "dsk"], ["XD"])
                    elif kind == "B":
                        conv_chunk(ps, N, 32 + g, BT[:, 0:N], "BT", ["b0"])
                        for j in range(nt):
                            k.tr(pTB[:, j * 128:(j + 1) * 128], BT[:, j * 128:(j + 1) * 128], identb, ["BT", "identb"], ["b2", "b3"])
                        k.cp("act", Btm[:, 0:nt, :], pTB[:, 0:N].rearrange("p (a b) -> p a b", b=128), ["b2", "b3"], ["Btm"])
                    else:
                        conv_chunk(ps, N, 40 + g, CT[:, 0:N], "CT", ["b1"])

                pending = None
                for ch in chunks:
                    ps = do_proj(ch)
                    if pending is not None:
                        do_post(*pending)
                    pending = (ch, ps)
                do_post(*pending)
                if own:
                    wvz, wkeyz = load_w(w_in, D, O_Z + g * 512, 512, buf=0)
                    for j in range(nt):
                        ps = k.bank(j % 2)
                        for kt in range(16):
                            k.mm(ps, hnT[:, kt, j * 128:(j + 1) * 128], wvz[:, kt, :], kt == 0, kt == 15, ["hnT", wkeyz], ["b%d" % (j % 2)])
                        k.act(sz[:, j, :], ps, AF.Silu, ["b%d" % (j % 2)], ["sz"])
                ck(7)
                Hg = H[:, g * 512:(g + 1) * 512]
                Hg3 = Hg.rearrange("p (h d) -> p h d", d=64)

                def upd(j, Hg=Hg, Hg3=Hg3, hs=hs):
                    sb_ = 6 + j % 2
                    ps_S = k.bank(sb_)
                    k.mm(ps_S, Btm[:, j, :], Xdec[:, j, :], True, True, ["Btm", "Xdec"], ["b%d" % sb_])
                    k.tt("dve", Hg3, Hg3, bc(edec[:, j, hs], 2, [128, 8, 64]), ALU.mult, ["H", "edec"], ["H"])
                    k.tt("dve", Hg, Hg, ps_S, ALU.add, ["H", "b%d" % sb_], ["H"])

                if not own:
                    for j in range(nt):
                        upd(j)
                else:
                    for j in range(nt):
                        k.cp("act", Hb_[j], Hg, ["H"], ["Hb%d" % j])
                        upd(j)

                    def front(j, g=g, hs=hs):
                        js = slice(j * 128, (j + 1) * 128)
                        jb = j % 2
                        AT, LT, MT = AT_[jb], LT_[jb], MT_[jb]
                        atk, ltk, mtk = "AT%d" % jb, "LT%d" % jb, "MT%d" % jb
                        ps_yo = k.bank(jb)
                        k.mm(ps_yo, CT[:, js], Hb_[j], True, True, ["CT", "Hb%d" % j], ["b%d" % jb])
                        ps_cb = k.bank(7)[:, 0:128]
                        k.mm(ps_cb, BT[:, js], CT[:, js], True, True, ["BT", "CT"], ["b7"])
                        k.tt("pool", AT, bc(tri_le, 1, [128, 8, 128]), bc(av[:, j, hs], 2, [128, 8, 128]), ALU.mult, ["cst", "av"], [atk])
                        psL = k.psum[1][:, 0:1024].rearrange("p (a b) -> p a b", b=128)
                        for hf in range(2):
                            k.mm(psL[:, hf * 4:(hf + 1) * 4, :], onesf, AT[:, hf * 4:(hf + 1) * 4, :], True, False, ["cst", atk], ["b%d" % (4 + hf)])
                            k.mm(psL[:, hf * 4:(hf + 1) * 4, :], identf, negtri4, False, True, ["cst", "negtri4"], ["b%d" % (4 + hf)])
                        for hh in range(8):
                            k.act(LT[:, hh, :], psL[:, hh, :], AF.Exp, ["b%d" % (4 + hh // 4), "nacs"], [ltk], bias=nacs[:, j, g * 8 + hh:g * 8 + hh + 1])
                        k.tt("dve", MT, LT, bc(ps_cb, 1, [128, 8, 128]), ALU.mult, [ltk, "b7"], [mtk])

                    def back(j, g=g, hs=hs):
                        t = tiles[j]
                        jb = j % 2
                        MT, mtk = MT_[jb], "MT%d" % jb
                        ps_yo = k.bank(jb)
                        ps_y = k.bank(6)[:, 0:512]
                        for hh in range(8):
                            k.mm(ps_y[:, hh * 64:(hh + 1) * 64], MT[:, hh, :], Xdt[:, j, hh * 64:(hh + 1) * 64], True, True, [mtk, "Xdt"], ["b6"])
                        y3 = yv.rearrange("p (h d) -> p h d", d=64)
                        k.tt("dve", y3, ps_yo.rearrange("p (h d) -> p h d", d=64), bc(eav[:, j, hs], 2, [128, 8, 64]), ALU.mult, ["b%d" % jb, "eav"], ["yv"])
                        k.tt("dve", yv, yv, ps_y, ALU.add, ["yv", "b6"], ["yv"])
                        k.tt("dve", yv, yv, XD[:, j, :], ALU.add, ["yv", "XD"], ["yv"])
                        k.tt("dve", yv, yv, sz[:, j, :], ALU.mult, ["yv", "sz"], ["yv"])
                        k.memset("pool", ysq[:, 0:1], 0.0, ["ysq0"])
                        k.act(ynb, yv, AF.Square, ["yv", "ysq0"], ["ynb", "ysq0"], accum_out=ysq[:, 0:1])
                        k.act(ysq[:, 2:3], ysq[:, 0:1], AF.Ln, ["ysq0"], ["ysq2"], bias=EPS, scale=1.0 / 512)
                        k.act(ysq[:, 3:4], ysq[:, 2:3], AF.Exp, ["ysq2"], ["ysq3"], scale=-0.5)
                        k.ts("dve", ynb, yv, ysq[:, 3:4], ALU.mult, ["yv", "ysq3", "ynb"], ["ynb"])
                        pTy = k.bank(3, BF16)
                        for cc in range(4):
                            k.tr(pTy[:, cc * 128:(cc + 1) * 128], ynb[:, cc * 128:(cc + 1) * 128], identb, ["ynb", "identb"], ["b2", "b3"])
                        for cc in range(4):
                            k.act(sst[:, cc, :], pTy[:, cc * 128:(cc + 1) * 128], AF.Copy, ["b2", "b3", "snw"], ["sst"], scale=snwf[:, g * 4 + cc:g * 4 + cc + 1])
                        ot = t - NT_CTX
                        k.dma("sp", st_ssm[:, g * 4:(g + 1) * 4, ot * 128:(ot + 1) * 128], sst, reads=["sst"], writes=["st_ssm"])

                    front(0)
                    for j in range(nt):
                        if j + 1 < nt:
                            front(j + 1)
                        back(j)
                ck(8)
        if dbg:
            dtile = k.alloc([128, 32, 128], BF16)
            for ot in range(NT_OWN):
                k.dma("sp", dtile, st_ssm[:, :, ot * 128:(ot + 1) * 128], reads=["st_ssm"], writes=["dtile"])
                out_toks.append(k.dma("sp", dbg_out["d_ssm"][:, :, ot * 128:(ot + 1) * 128], dtile, reads=["dtile"]))
    k.aoff = mark0

    def rope_fm(ps_a, ps_b, tbuf, tsel, N, out, okey, akey, bkey, r1, r2):
        k.tt("dve", r1[:, 0:N], ps_a, tbuf[:, tsel, 0:N], ALU.mult, [akey, "tb"], ["r1"])
        k.tt("dve", r2[:, 0:N], ps_b, tbuf[:, tsel + 1, 0:N], ALU.mult, [bkey, "tb"], ["r2"])
        k.tt("dve", out, r1[:, 0:N], r2[:, 0:N], ALU.add, ["r1", "r2"], [okey])

    if 2 in phases:
        k.barrier()
        k.dma("sp", nw_fm, nmix, writes=["nw"])
        KT = k.alloc([128, 4, NSLOT], BF16)
        V = k.alloc([128, NT, 4, 130], BF16)
        kiT = k.alloc([128, NSLOT], BF16)
        mark2 = k.aoff
        k.memset("pool", V, 1.0, ["V"])
        tb = k.alloc([128, 4, 512], F32)
        r1 = k.alloc([128, 512], F32)
        r2 = k.alloc([128, 512], F32)
        for gi, tiles in enumerate(groups):
            if gi not in gsel:
                continue
            nt = len(tiles)
            N = 128 * nt
            s0 = tiles[0] * 128
            make_hnT([xin[t * 128:(t + 1) * 128, :] for t in tiles])
            k.dma("sp", tb[:, :, 0:N], tabs[:, :, s0:s0 + N].rearrange("a p s -> p a s"), writes=["tb"])
            wv, wkey = load_w(w_in, D, O_K, 512, buf=0)
            wr, rkey = load_w(w_rot, D, R_K, 512, buf=1)
            for cc in range(4):
                pa = proj_fm(wv, wkey, cc, N, 0)
                pb = proj_fm(wr, rkey, cc, N, 1)
                rope_fm(pa, pb, tb, 0, N, KT[:, cc, s0:s0 + N], "KT", "b0", "b1", r1, r2)
            wki = wb[0][:, 0:16 * 256].rearrange("p (a b) -> p a b", b=256)
            for half in range(2):
                k.dma("pool", wki[:, :, half * 64:(half + 1) * 64], w_in[:, O_KI:O_KI + 64].rearrange("(a p) c -> p a c", p=128), writes=["wb0"])
                k.dma("pool", wki[:, :, 128 + half * 64:128 + (half + 1) * 64], w_rot[:, R_KI:R_KI + 64].rearrange("(a p) c -> p a c", p=128), writes=["wb0"])
            pa = proj_fm(wki, "wb0", 0, N, 0)
            pb = proj_fm(wki, "wb0", 1, N, 1)
            rope_fm(pa, pb, tb, 2, N, kiT[:, s0:s0 + N], "kiT", "b0", "b1", r1, r2)
            wvv, wkeyv = load_w(w_in, D, O_V, 512, buf=1)
            for j, t in enumerate(tiles):
                ps = k.bank(2 + j % 2)
                for kt in range(16):
                    k.mm(ps, hnT[:, kt, j * 128:(j + 1) * 128], wvv[:, kt, :], kt == 0, kt == 15, ["hnT", wkeyv], ["b%d" % (2 + j % 2)])
                k.cp("act", V[:, t, :, 0:128], ps.rearrange("p (g d) -> p g d", d=128), ["b%d" % (2 + j % 2)], ["V"])
        k.aoff = mark2

    if 3 in phases:
        k.barrier()
        MB = k.view(hnT_off, [128, NT, 128], BF16)
        tb3 = k.view(hnT_off + 8448 + 4096, [128, 4, 128], F32)
        hnT3 = k.alloc([128, 16, 128], BF16)
        padb_r = k.alloc([128, CTX], BF16)
        k.dma("pool", padb_r, padb.partition_broadcast(128), writes=["padb"])
        qT_ = [k.alloc([128, 16, 128], BF16), k.view(hnT_off + 8448, [128, 16, 128], BF16)]
        qiT = k.alloc([128, 8, 128], BF16)
        wis = k.alloc([128, 16], F32)
        score = k.alloc([128, NSLOT], F32)
        wm_off = k.aoff
        work = k.alloc([128, NSLOT], F32)
        maskb = k.view(wm_off, [128, NSLOT], BF16)
        maskT = k.view(wm_off + NSLOT * 2, [128, NT, 128], BF16)
        rl_ = [k.alloc([128, 512], F32) for _ in range(2)]
        m8 = k.alloc([128, 8], F32)
        thr = k.alloc([128, 1], F32)
        Et_ = [k.alloc([128, 4, 128], BF16) for _ in range(2)]
        ao = k.alloc([128, 16, 128], BF16)
        rs = k.alloc([128, 4], F32)
        aT = k.alloc([128, 16, 128], BF16)
        wwi = k.alloc([128, 16, 16], BF16)
        k.dma("pool", wwi, w_in[:, O_WI:O_WI + 16].rearrange("(a p) c -> p a c", p=128), writes=["wwi"])

        def A1(t):
            ot = t - NT_CTX
            qT, qk = qT_[ot % 2], "qT%d" % (ot % 2)
            N = 128
            s0 = t * 128
            make_hnT([xin[t * 128:(t + 1) * 128, :]], dst=hnT3, dkey="hnT3")
            k.dma("sp", tb3, tabs[:, :, s0:s0 + N].rearrange("a p s -> p a s"), writes=["tb"])
            def proj4(wv, wkey, bank):
                pw = k.bank(bank).rearrange("p (a b) -> p a b", b=128)
                for cc in range(4):
                    for kt in range(16):
                        k.mm(pw[:, cc, :], wv[:, kt, cc * 128:(cc + 1) * 128], hnT3[:, kt, 0:128], kt == 0, kt == 15, [wkey, "hnT3"], ["b%d" % bank])
                return pw

            def rope4(pa, pb, tsel, out, okey, ba, bb):
                tC = bc(tb3[:, tsel, :], 1, [128, 4, 128])
                tS = bc(tb3[:, tsel + 1, :], 1, [128, 4, 128])
                r1q = rl_[0].rearrange("p (a b) -> p a b", b=128)
                r2q = rl_[1].rearrange("p (a b) -> p a b", b=128)
                k.tt("dve", r1q, pa, tC, ALU.mult, ["b%d" % ba, "tb"], ["rl0"])
                k.tt("dve", r2q, pb, tS, ALU.mult, ["b%d" % bb, "tb"], ["rl1"])
                k.tt("dve", out, r1q, r2q, ALU.add, ["rl0", "rl1"], [okey])

            nproj = 0
            for c4 in range(4):
                wv, wkey = load_w(w_in, D, O_Q + c4 * 512, 512, buf=0)
                wr, rkey = load_w(w_rot, D, R_Q + c4 * 512, 512, buf=1)
                ba = 0 if nproj % 2 == 0 else 2
                nproj += 1
                pa = proj4(wv, wkey, ba)
                pb = proj4(wr, rkey, ba + 1)
                rope4(pa, pb, 0, qT[:, c4 * 4:(c4 + 1) * 4, :], qk, ba, ba + 1)
            for c4 in range(2):
                wv, wkey = load_w(w_in, D, O_QI + c4 * 512, 512, buf=0)
                wr, rkey = load_w(w_rot, D, R_QI + c4 * 512, 512, buf=1)
                ba = 0 if nproj % 2 == 0 else 2
                nproj += 1
                pa = proj4(wv, wkey, ba)
                pb = proj4(wr, rkey, ba + 1)
                rope4(pa, pb, 2, qiT[:, c4 * 4:(c4 + 1) * 4, :], "qiT", ba, ba + 1)
            ps = k.bank(2)[:, 0:16]
            for kt in range(16):
                k.mm(ps, hnT3[:, kt, 0:128], wwi[:, kt, :], kt == 0, kt == 15, ["hnT3", "wwi"], ["b2"])
            k.ts("dve", wis, ps, 1.0 / 32.0, ALU.mult, ["b2"], ["wis"])
            nk = t + 1
            S = nk * 128
            chunks = [(c0, min(512, CTX - c0)) for c0 in range(0, CTX, 512)] + [(c0, min(512, S - c0)) for c0 in range(CTX, S, 512)]
            for ci, (c0, n) in enumerate(chunks):
                for h in range(16):
                    hp = slice((h % 2) * 64, (h % 2) * 64 + 64)
                    bk = 2 + (h % 2)
                    rl, rlk = rl_[h % 2], "rl%d" % (h % 2)
                    ps = k.bank(bk)[:, 0:n]
                    k.mm(ps, qiT[hp, h // 2, :], kiT[hp, c0:c0 + n], True, True, ["qiT", "kiT"], ["b%d" % bk])
                    k.act(rl[:, 0:n], ps, AF.Relu, ["b%d" % bk], [rlk])
                    if h == 0:
                        if c0 < CTX:
                            k.stt("dve", score[:, c0:c0 + n], rl[:, 0:n], wis[:, 0:1], padb_r[:, c0:c0 + n], ALU.mult, ALU.add, [rlk, "wis", "padb"], ["score"])
                        else:
                            k.ts("dve", score[:, c0:c0 + n], rl[:, 0:n], wis[:, 0:1], ALU.mult, [rlk, "wis"], ["score"])
                    else:
                        k.stt("dve", score[:, c0:c0 + n], rl[:, 0:n], wis[:, h:h + 1], score[:, c0:c0 + n], ALU.mult, ALU.add, [rlk, "wis", "score"], ["score"])
            k.tt("dve", score[:, S - 128:S], score[:, S - 128:S], causb, ALU.add, ["score", "cst"], ["score"])
            if dbg and t == NT_CTX:
                out_toks.append(k.dma("sp", dbg_out["d_score"], score, reads=["score"]))

        def TK(t):
            S = (t + 1) * 128
            cur, ckey = score, "score"
            for r in range(32):
                k.op("dve", lambda e, cur=cur, S=S: e.max(out=m8, in_=cur[:, 0:S]), [ckey], ["m8"])
                if r < 31:
                    k.op("dve", lambda e, cur=cur, S=S: e.match_replace(out=work[:, 0:S], in_to_replace=m8, in_values=cur[:, 0:S], imm_value=NEG),
                         [ckey, "m8", "wm"], ["wm"])
                    cur, ckey = work, "wm"
            k.ts("dve", thr, m8[:, 7:8], -1.0e29, ALU.max, ["m8"], ["thr"])
            k.ts("dve", maskb[:, 0:S], score[:, 0:S], thr, ALU.is_ge, ["score", "thr", "wm"], ["wm"])
            if dbg and t == NT_CTX:
                out_toks.append(k.dma("sp", dbg_out["d_mask"], maskb, reads=["wm"]))
                out_toks.append(k.dma("sp", dbg_out["d_thr"], m8, reads=["m8"]))

        def A2(t):
            nk = t + 1
            for kt0 in range(0, nk, 8):
                nb = min(8, nk - kt0)
                pTm = k.bank(4, BF16)
                for i2 in range(nb):
                    k.tr(pTm[:, i2 * 128:(i2 + 1) * 128], maskb[:, (kt0 + i2) * 128:(kt0 + i2 + 1) * 128], identb, ["wm", "identb"], ["b4"])
                k.cp("act", maskT[:, kt0:kt0 + nb, :], pTm[:, 0:nb * 128].rearrange("p (a b) -> p a b", b=128), ["b4", "wm"], ["wm"])
            k.ts("dve", MB[:, 0:nk, :], maskT[:, 0:nk, :], -1.0, ALU.add, ["wm"], ["MB"], s2=30000.0, op1=ALU.mult)

        def B(t):
            ot = t - NT_CTX
            qT, qk = qT_[ot % 2], "qT%d" % (ot % 2)
            nk = t + 1
            for kvg in range(4):
                po = k.psum[1][:, 1024:2048].rearrange("p (a b) -> p a b", b=256)

                def sc_stage(kt, kvg=kvg):
                    bk = kt % 2
                    Et, etk = Et_[bk], "Et%d" % bk
                    ps = k.bank(bk)
                    k.mm(ps, KT[:, kvg, kt * 128:(kt + 1) * 128], qT[:, kvg * 4:(kvg + 1) * 4, :], True, False, ["KT", qk], ["b%d" % bk])
                    for hh in range(4):
                        k.mm(ps[:, hh * 128:(hh + 1) * 128], identb, MB[:, kt, :], False, hh == 3, ["identb", "MB"], ["b%d" % bk])
                    k.act(Et, ps.rearrange("p (a b) -> p a b", b=128), AF.Exp, ["b%d" % bk], [etk], scale=1.0 / math.sqrt(128.0))

                def pv_stage(kt, kvg=kvg, po=po):
                    bk = kt % 2
                    Et, etk = Et_[bk], "Et%d" % bk
                    for hh in range(4):
                        k.mm(po[:, hh, 0:129], Et[:, hh, :], V[:, kt, kvg, 0:129], kt == 0 and hh % 2 == 0, kt == nk - 1, [etk, "V"], ["b%d" % (6 + hh // 2)])

                sc_stage(0)
                for kt in range(nk):
                    if kt + 1 < nk:
                        sc_stage(kt + 1)
                    pv_stage(kt)
                k.act(rs, po[:, :, 128], AF.Ln, ["b6", "b7"], ["rs"])
                k.act(rs, rs, AF.Exp, ["rs"], ["rs"], scale=-1.0)
                for hh in range(4):
                    k.act(ao[:, kvg * 4 + hh, :], po[:, hh, 0:128], AF.Copy, ["b%d" % (6 + hh // 2), "rs"], ["ao"], scale=rs[:, hh:hh + 1])
            pTa = k.psum[1][:, 0:1024].bitcast(BF16)
            for h in range(16):
                k.tr(pTa[:, h * 128:(h + 1) * 128], ao[:, h, :], identb, ["ao", "identb"], ["b4", "b5"])
            k.cp("act", aT, pTa.rearrange("p (a b) -> p a b", b=128), ["b4", "b5"], ["aT"])
            k.dma("sp", st_attn[:, :, ot * 128:(ot + 1) * 128], aT, reads=["aT"], writes=["st_attn"])

        sel = [t for t in range(NT_CTX, NT) if (7 if t < NT_CTX + 4 else 8) in gsel]
        prev = None
        for t in sel:
            A1(t)
            TK(t)
            if prev is not None:
                B(prev)
            A2(t)
            prev = t
        if prev is not None:
            B(prev)
        if dbg:
            for ot in range(NT_OWN):
                k.dma("sp", aT, st_attn[:, :, ot * 128:(ot + 1) * 128], reads=["st_attn"], writes=["aT"])
                out_toks.append(k.dma("sp", dbg_out["d_attn"][:, :, ot * 128:(ot + 1) * 128], aT, reads=["aT"]))
    k.aoff = mark0

    h2 = k.alloc([128, 4, D], F32)
    mark4 = k.aoff
    for gi in (7, 8):
        tiles = groups[gi]
        otiles = [t - NT_CTX for t in tiles]
        o0 = otiles[0] * 128
        if 4 in phases:
            k.barrier()
            k.aoff = mark4
            k.dma("sp", nw_fm, nmix, writes=["nw"])
            wb2 = k.alloc([128, 16 * 512], BF16)
            actT = k.alloc([128, 32, 512], BF16)
            mg = k.alloc([128, 16, 512], BF16)
            sga = k.alloc([128, 512], F32)
            t1 = k.alloc([128, 512], F32)
            make_hnT([xin[t * 128:(t + 1) * 128, :] for t in tiles])
            k.dma("sp", actT[:, 0:16, :], st_attn[:, :, o0:o0 + 512], reads=["st_attn"], writes=["actT"])
            for c4 in range(4):
                wg, gkey = load_w(w_in, D, O_G + c4 * 512, 512, buf=0)
                wv, wkey = load_w(w_pa, 2048, c4 * 512, 512, buf=1)
                for cc in range(4):
                    pg = proj_fm(wg, gkey, cc, 512, 0)
                    pa = proj_fm(wv, wkey, cc, 512, 1, rhs=actT, rkey="actT")
                    k.act(sga, pg, AF.Sigmoid, ["b0"], ["sga"])
                    k.tt("dve", mg[:, c4 * 4 + cc, :], sga, pa, ALU.mult, ["sga", "b1"], ["mg"])
            k.dma("sp", actT, st_ssm[:, :, o0:o0 + 512], reads=["st_ssm", "actT"], writes=["actT"])
            for c4 in range(4):
                wg = wb2[:, :].rearrange("p (a b) -> p a b", b=512)
                k.dma("pool", wg, w_in[:, O_G + 2048 + c4 * 512:O_G + 2048 + (c4 + 1) * 512].rearrange("(a p) c -> p a c", p=128), writes=["wb2"])
                for c2 in range(2):
                    wv, wkey = load_w(w_ps, 4096, c4 * 512 + c2 * 256, 256, buf=c2)
                    for cc in range(2):
                        col = c4 * 4 + c2 * 2 + cc
                        pg = proj_fm(wg, "wb2", c2 * 2 + cc, 512, 0)
                        pa = proj_fm(wv, wkey, cc, 512, 1, nk=32, rhs=actT, rkey="actT")
                        k.act(sga, pg, AF.Sigmoid, ["b0"], ["sga"])
                        k.tt("dve", t1, sga, pa, ALU.mult, ["sga", "b1"], ["t1"])
                        k.tt("dve", mg[:, col, :], mg[:, col, :], t1, ALU.add, ["mg", "t1"], ["mg"])
            for c4 in range(4):
                wv, wkey = load_w(w_o, 2048, c4 * 512, 512, buf=c4 % 2)
                for j, t in enumerate(tiles):
                    bk = 2 + j % 2
                    ps = k.bank(bk)
                    for kt in range(16):
                        k.mm(ps, mg[:, kt, j * 128:(j + 1) * 128], wv[:, kt, :], kt == 0, kt == 15, ["mg", wkey], ["b%d" % bk])
                    if c4 == 0:
                        k.dma("sp", h2[:, j, :], xin[t * 128:(t + 1) * 128, :], writes=["h2_%d" % j])
                    k.tt("dve", h2[:, j, c4 * 512:(c4 + 1) * 512], h2[:, j, c4 * 512:(c4 + 1) * 512], ps, ALU.add, ["h2_%d" % j, "b%d" % bk], ["h2_%d" % j])
            if dbg:
                for j, ot in enumerate(otiles):
                    out_toks.append(k.dma("sp", dbg_out["d_h2"][:, ot, :], h2[:, j, :], reads=["h2_%d" % j]))
        if 5 in phases:
            k.barrier()
            k.aoff = mark4
            k.dma("sp", nw_fm, nffn, writes=["nw"])
            nfin_r = xt
            k.dma("sp", nfin_r, nfin.partition_broadcast(128), writes=["xt"])
            keysb = k.alloc([128, 2, 128], BF16)
            k.dma("pool", keysb, keysT, writes=["keysb"])
            q_off = k.aoff
            qpT = k.alloc([128, 16, 512], BF16)
            osb = k.view(q_off, [128, D], F32)
            ssc = k.alloc([128, 4, 16, 128], F32)
            top = k.alloc([128, 16, 16], F32)
            wk2 = k.alloc([128, 128], F32)
            cand = k.alloc([128, 256], F32)
            cand2 = k.alloc([128, 256], F32)
            c8 = k.alloc([128, 8], F32)
            thr5 = k.alloc([128, 4, 8], F32)
            nb5 = k.alloc([128, 4, 8], F32)
            zz = k.alloc([128, 4], F32)
            Gt_ = [k.alloc([128, 512], BF16) for _ in range(2)]
            GT_ = [k.alloc([128, 4, 128], BF16) for _ in range(2)]
            cand3 = k.alloc([128, 256], F32)
            c8b = k.alloc([128, 8], F32)
            tau5 = k.alloc([128, 4, 8], F32)
            tsum = k.alloc([128, 8], F32)
            Ex_ = [k.alloc([128, 512], F32) for _ in range(3)]
            Wh_ = [k.alloc([128, 512], BF16) for _ in range(3)]
            GWT_ = [k.alloc([128, 4, 128], BF16) for _ in range(2)]
            wb5 = [wb[0], wb[1], k.alloc([128, 16 * 512], BF16), k.alloc([128, 16 * 512], BF16)]
            make_hnT([h2[:, j, :] for j in range(4)], keys=["h2_%d" % j for j in range(4)])
            for c4 in range(4):
                wv, wkey = load_w(w_pq, 2048, c4 * 512, 512, buf=c4 % 2)
                for cc in range(4):
                    pa = proj_fm(wv, wkey, cc, 512, 0)
                    k.cp("act", qpT[:, c4 * 4 + cc, :], pa, ["b0"], ["qpT"])
            for j in range(4):
                js = slice(j * 128, (j + 1) * 128)
                for q4 in range(4):
                    bk = 2 + q4 % 2
                    ps = k.bank(bk)
                    for i2 in range(4):
                        hc = q4 * 4 + i2
                        k.mm(ps[:, i2 * 128:(i2 + 1) * 128], qpT[:, hc, js], keysb[:, hc % 2, :], True, True, ["qpT", "keysb"], ["b%d" % bk])
                    k.cp("act", ssc[:, j, q4 * 4:(q4 + 1) * 4, :], ps.rearrange("p (a b) -> p a b", b=128), ["b%d" % bk], ["ssc"])
                for hc in range(16):
                    k.op("dve", lambda e, hc=hc, j=j: e.max(out=top[:, hc, 0:8], in_=ssc[:, j, hc, :]), ["ssc"], ["top"])
                    k.op("dve", lambda e, hc=hc, j=j: e.match_replace(out=wk2, in_to_replace=top[:, hc, 0:8], in_values=ssc[:, j, hc, :], imm_value=NEG), ["ssc", "top", "wk2"], ["wk2"])
                    k.op("dve", lambda e, hc=hc: e.max(out=top[:, hc, 8:16], in_=wk2), ["wk2", "top"], ["top"])
                for h in range(8):
                    c3 = cand.rearrange("p (a b) -> p a b", b=16)
                    k.tt("dve", c3, bc(top[:, 2 * h, :], 2, [128, 16, 16]), bc(top[:, 2 * h + 1, :], 1, [128, 16, 16]), ALU.add, ["top"], ["cand"])
                    k.op("dve", lambda e: e.max(out=c8, in_=cand), ["cand"], ["c8"])
                    k.ts("dve", zz[:, 0:1], c8[:, 0:1], -1.0, ALU.mult, ["c8"], ["zz0"])
                    k.op("dve", lambda e: e.match_replace(out=cand2, in_to_replace=c8, in_values=cand, imm_value=NEG), ["cand", "c8", "cand2"], ["cand2"])
                    k.op("dve", lambda e: e.max(out=c8, in_=cand2), ["cand2", "c8"], ["c8"])
                    k.cp("dve", thr5[:, j, h:h + 1], c8[:, 7:8], ["c8"], ["thr5"])
                    k.op("dve", lambda e: e.match_replace(out=cand3, in_to_replace=c8, in_values=cand2, imm_value=NEG), ["cand2", "c8", "cand3"], ["cand3"])
                    k.op("dve", lambda e: e.max(out=c8b, in_=cand3), ["cand3", "c8b"], ["c8b"])
                    k.tt("dve", tsum[:, h:h + 1], c8[:, 7:8], c8b[:, 0:1], ALU.add, ["c8", "c8b"], ["tsum"])
                    k.act(cand3, cand, AF.Exp, ["cand", "zz0", "cand3"], ["cand3"], bias=zz[:, 0:1])
                    k.stt("dve", cand3, cand, thr5[:, j, h:h + 1], cand3, ALU.is_ge, ALU.mult, ["cand", "thr5", "cand3"], ["cand3"])
                    k.op("dve", lambda e: e.reduce_sum(out=zz[:, 1:2], in_=cand3, axis=mybir.AxisListType.X), ["cand3"], ["zz1"])
                    k.act(zz[:, 2:3], zz[:, 1:2], AF.Ln, ["zz1"], ["zz2"])
                    k.tt("dve", nb5[:, j, h:h + 1], zz[:, 0:1], zz[:, 2:3], ALU.subtract, ["zz0", "zz2"], ["nb5"])
                k.stt("dve", tau5[:, j, :], tsum, 0.5, nb5[:, j, :], ALU.mult, ALU.add, ["tsum", "nb5"], ["tau5"])
                k.act(tau5[:, j, :], tau5[:, j, :], AF.Exp, ["tau5"], ["tau5"])
                for h in range(8):
                    k.act(ssc[:, j, 2 * h, :], ssc[:, j, 2 * h, :], AF.Exp, ["ssc", "nb5"], ["ssc"], bias=nb5[:, j, h:h + 1])
                    k.act(ssc[:, j, 2 * h + 1, :], ssc[:, j, 2 * h + 1, :], AF.Exp, ["ssc"], ["ssc"])
            iters = [(ec, j) for ec in range(NEC) for j in range(4)]

            def bufs(ec):
                uv = wb5[(ec % 2) * 2][:, :].rearrange("p (a b) -> p a b", b=512)
                vv = wb5[(ec % 2) * 2 + 1][:, :].rearrange("p (a b) -> p a b", b=2048)
                ukey = "wb0" if ec % 2 == 0 else "wu1"
                vkey = "wb1" if ec % 2 == 0 else "wv1"
                return uv, vv, ukey, vkey

            def load5(ec):
                uv, vv, ukey, vkey = bufs(ec)
                k.dma("pool", uv, uT[:, ec * 512:(ec + 1) * 512].rearrange("(a p) c -> p a c", p=128), writes=[ukey])
                k.dma("pool", vv, pv[ec * 512:(ec + 1) * 512, :].rearrange("(a p) c -> p a c", p=128), writes=[vkey])

            def stageA(idx):
                ec, j = iters[idx]
                uv, vv, ukey, vkey = bufs(ec)
                js = slice(j * 128, (j + 1) * 128)
                jb = idx % 2
                Gt, GT, GWT = Gt_[jb], GT_[jb], GWT_[jb]
                gk, gtk, gwtk = "Gt%d" % jb, "GT%d" % jb, "GWT%d" % jb
                ps = k.bank(jb)
                for kt in range(16):
                    k.mm(ps, hnT[:, kt, js], uv[:, kt, :], kt == 0, kt == 15, ["hnT", ukey], ["b%d" % jb])
                k.act(Gt, ps, AF.Gelu, ["b%d" % jb], [gk])
                if idx >= 1:
                    stageB_pe(idx - 1)
                pTg = k.bank(jb, BF16)
                for b4 in range(4):
                    k.tr(pTg[:, b4 * 128:(b4 + 1) * 128], Gt[:, b4 * 128:(b4 + 1) * 128], identb, [gk, "identb"], ["b%d" % jb])
                k.cp("act", GT, pTg[:, 0:512].rearrange("p (a b) -> p a b", b=128), ["b%d" % jb], [gtk])
                pW = k.psum[0][:, 1024:2048].rearrange("p (a b) -> p a b", b=256)[:, :, 0:128]
                for h in range(8):
                    hb = (idx * 8 + h) % 3
                    Ex, Wh = Ex_[hb], Wh_[hb]
                    ek, whk = "Ex%d" % hb, "Wh%d" % hb
                    eng = "dve" if h % 4 == 3 else "pool"
                    k.tt(eng, Ex.rearrange("p (a b) -> p a b", b=128), bc(ssc[:, j, 2 * h, ec * 4:(ec + 1) * 4], 2, [128, 4, 128]),
                         bc(ssc[:, j, 2 * h + 1, :], 1, [128, 4, 128]), ALU.mult, ["ssc"], [ek])
                    k.stt("dve", Wh, Ex, tau5[:, j, h:h + 1], Ex, ALU.is_ge, ALU.mult, ["tau5", ek], [whk])
                    for b4 in range(4):
                        k.mm(pW[:, b4, :], Wh[:, b4 * 128:(b4 + 1) * 128], identb, h == 0 and b4 % 2 == 0, h == 7, [whk, "identb"], ["b%d" % (2 + b4 // 2)])
                k.tt("dve", GWT, pW, GT, ALU.mult, ["b2", "b3", gtk], [gwtk])
                if idx >= 1:
                    stageB_dve(idx - 1)

            def stageB_pe(idx):
                ec, j = iters[idx]
                uv, vv, ukey, vkey = bufs(ec)
                jb = idx % 2
                GWT, gwtk = GWT_[jb], "GWT%d" % jb
                po = k.psum[1][:, 0:2048]
                for b4 in range(4):
                    for dc in range(4):
                        k.mm(po[:, dc * 512:(dc + 1) * 512], GWT[:, b4, :], vv[:, b4, dc * 512:(dc + 1) * 512], b4 == 0, b4 == 3, [gwtk, vkey], ["b%d" % (4 + dc)])

            def stageB_dve(idx):
                ec, j = iters[idx]
                po = k.psum[1][:, 0:2048]
                k.tt("dve", h2[:, j, :], h2[:, j, :], po, ALU.add, ["h2_%d" % j, "b4", "b5", "b6", "b7"], ["h2_%d" % j])

            load5(0)
            for idx in range(len(iters)):
                ec, j = iters[idx]
                stageA(idx)
                if j == 1 and ec + 1 < NEC:
                    load5(ec + 1)
            stageB_pe(len(iters) - 1)
            stageB_dve(len(iters) - 1)
            k.barrier()
            for j, ot in enumerate(otiles):
                k.memset("pool", sq[:, 0:1], 0.0, ["sq0"])
                k.act(osb, h2[:, j, :], AF.Square, ["h2_%d" % j, "sq0"], ["osb", "sq0"], accum_out=sq[:, 0:1])
                k.act(sq[:, 2:3], sq[:, 0:1], AF.Ln, ["sq0"], ["sq2"], bias=EPS, scale=1.0 / D)
                k.act(sq[:, 3:4], sq[:, 2:3], AF.Exp, ["sq2"], ["sq3"], scale=-0.5)
                k.stt("dve", osb, h2[:, j, :], sq[:, 3:4], nfin_r, ALU.mult, ALU.mult, ["h2_%d" % j, "sq3", "xt", "osb"], ["osb"])
                out_toks.append(k.dma("sp", yout[ot * 128:(ot + 1) * 128, :], osb, reads=["osb"]))


def _rot_cols(w, head_dim):
    half = head_dim // 2
    n = w.shape[1]
    idx = np.arange(n)
    r = (idx // head_dim) * head_dim + (idx % head_dim + half) % head_dim
    return w[:, r]


def _tables(pos):
    res = []
    for hd in (128, 64):
        half = hd // 2
        p = np.arange(128) % hd
        inv = (10000.0 ** (-(np.arange(half, dtype=np.float32)) / np.float32(half))).astype(np.float32)
        ang = pos.astype(np.float32)[None, :] * inv[p % half][:, None]
        c = np.cos(ang).astype(np.float32)
        s = np.sin(ang).astype(np.float32)
        sgn = np.where(p < half, -1.0, 1.0).astype(np.float32)[:, None]
        res += [c, s * sgn]
    return np.stack(res, 0).astype(np.float32)


_NC_CACHE = {}


def kernel(x, meta_tokens, norm_mix_w, w_in, conv_w, conv_b, dt_bias, a_log, d_skip, ssm_norm_w,
           w_branch_attn, w_branch_ssm, w_out, norm_ffn_w, peer_w_query, peer_sub_keys, peer_u, peer_v,
           norm_final_w, _phases=(1, 2, 3, 4, 5), _dbg=False, _gsel=None, _ncores=8, _trace=False):
    f = lambda a: np.ascontiguousarray(np.asarray(a, dtype=np.float32))
    x = f(x)
    meta = f(meta_tokens)
    w_in0 = f(w_in)[0]
    w_rot = np.concatenate([
        _rot_cols(w_in0[:, O_Q:O_Q + 2048], 128), _rot_cols(w_in0[:, O_K:O_K + 512], 128),
        _rot_cols(w_in0[:, O_QI:O_QI + 1024], 64), _rot_cols(w_in0[:, O_KI:O_KI + 64], 64)], axis=1)
    w_rot = np.ascontiguousarray(w_rot)
    cst = np.zeros((128, 5, 128), np.float32)
    ii = np.arange(128)
    cst[:, 0, :] = np.eye(128)
    cst[:, 1, :] = (ii[:, None] <= ii[None, :])
    cst[:, 2, :] = np.where(ii[:, None] > ii[None, :], -30000.0, 0.0)
    cst[:, 3, :] = np.where(ii[None, :] <= ii[:, None], 0.0, NEG)
    cst[:, 4, :] = 1.0
    common = {
        "cst": cst, "w_in": w_in0, "w_rot": w_rot,
        "convw": np.ascontiguousarray(f(conv_w)[0].T.reshape(48, 128, 4).transpose(1, 0, 2)),
        "convb": np.ascontiguousarray(f(conv_b)[0].reshape(48, 128).T),
        "dtb": f(dt_bias)[0], "alog": f(a_log)[0], "dsk": f(d_skip)[0],
        "snw": np.ascontiguousarray(f(ssm_norm_w)[0].reshape(32, 128).T),
        "nmix": np.ascontiguousarray(f(norm_mix_w)[0].reshape(16, 128).T),
        "nffn": np.ascontiguousarray(f(norm_ffn_w)[0].reshape(16, 128).T),
        "nfin": f(norm_final_w),
        "w_pa": f(w_branch_attn)[0], "w_ps": f(w_branch_ssm)[0], "w_o": f(w_out)[0], "w_pq": f(peer_w_query)[0],
        "keysT": np.ascontiguousarray(f(peer_sub_keys)[0].transpose(2, 0, 1)),
        "uT": np.ascontiguousarray(f(peer_u)[0].T), "pv": f(peer_v)[0],
    }
    in_maps = []
    for core in range(8):
        b, c = core // 4, core % 4
        own_start = 16 + 1024 * c
        pos = own_start - CTX + np.arange(NSLOT)
        xin = np.zeros((NSLOT, D), np.float32)
        seq = np.concatenate([meta, x[b]], axis=0)
        ok = (pos >= 0)
        xin[ok] = seq[pos[ok]]
        vmask = ok.astype(np.float32)
        m = dict(common)
        m["xin"] = xin
        m["tabs"] = _tables(pos)
        m["valid"] = np.ascontiguousarray(vmask.reshape(NT, 128).T)
        m["padb"] = np.where(ok[:CTX], 0.0, NEG).astype(np.float32)
        in_maps.append(m)
    key = (tuple(_phases), _dbg, _gsel)
    if key not in _NC_CACHE:
        _NC_CACHE[key] = build_program(_phases, _dbg, _gsel)
    nc, used = _NC_CACHE[key]
    in_maps = [{n: m[n] for n in used} for m in in_maps][:_ncores]
    res = run_bass_kernel_spmd(nc, in_maps, core_ids=list(range(_ncores)), **({'trace': True} if _trace else {}))
    out = np.zeros((2, 4096, D), np.float32)
    for core in range(_ncores):
        b, c = core // 4, core % 4
        out[b, c * 1024:(c + 1) * 1024] = res.results[core]["y"]
    if _dbg:
        return out, res
    return out
```
